# Optimizing a Trainium2 kernel written in Bass

```python
import numpy as np
import jax
import jax.numpy as jnp
from jax import lax

D_MODEL = 1024
BATCH = 32
SEQ = 2048
DEPTH = 2

CHUNK = 64
D_MIX = D_MODEL
N_GROUPS = 4
GROUP_W = D_MIX // N_GROUPS
CONV_W = 3
S5_CH = GROUP_W
S5_GROUP = 16
S5_NG = S5_CH // S5_GROUP
S5_STATE = 64
S5_DT_MIN = 1e-3
S5_DT_MAX = 1e-1
ATT_HD = 64
ATT_QH = GROUP_W // ATT_HD
ATT_KVH = 2
ATT_REP = ATT_QH // ATT_KVH
ATT_SCALE = ATT_HD ** -0.5
WINDOW = 128
WIN_CHUNKS = WINDOW // CHUNK
HG_HEADS = 4
HG_DK = GROUP_W // HG_HEADS
HG_DV = GROUP_W // HG_HEADS
HG_BLOCK = 16
D_FF = 4 * D_MODEL
EPS = 1e-6
SPLIT_SIZES = (GROUP_W, GROUP_W, GROUP_W,
               S5_CH,
               ATT_QH * ATT_HD, ATT_KVH * ATT_HD, ATT_KVH * ATT_HD,
               HG_HEADS * HG_DK, HG_HEADS * HG_DK,
               HG_HEADS * HG_DV, HG_HEADS * HG_DV)
D_IN = sum(SPLIT_SIZES)

kernel_name = "hybrid_parallel_group_stream_encoder"


def rmsnorm(x, gain):
    xf = x.astype(jnp.float32)
    y = xf * lax.rsqrt(jnp.mean(xf * xf, axis=-1, keepdims=True) + EPS)
    return (y * gain.astype(jnp.float32)).astype(x.dtype)


def short_conv_mixer(h, gate_b, gate_c, conv_w):
    seq = h.shape[1]
    z = gate_c * h
    zp = jnp.pad(z, ((0, 0), (CONV_W - 1, 0), (0, 0)))
    acc = conv_w[0] * zp[:, 0:seq]
    for j in range(1, CONV_W):
        acc = acc + conv_w[j] * zp[:, j:j + seq]
    return gate_b * acc


def _complex_affine_combine(e1, e2):
    a1r, a1i, b1r, b1i = e1
    a2r, a2i, b2r, b2i = e2
    return (a2r * a1r - a2i * a1i,
            a2r * a1i + a2i * a1r,
            a2r * b1r - a2i * b1i + b2r,
            a2r * b1i + a2i * b1r + b2i)


def s5_mixer(u, lam_re, lam_im, b_re, b_im, c_re, c_im, d_skip, log_dt, w_glu):
    bsz, seq, _ = u.shape
    uf = u.astype(jnp.float32)
    ug = uf.reshape(bsz, seq, S5_NG, S5_GROUP)
    lr = jnp.minimum(lam_re.astype(jnp.float32), -1e-4)
    li = lam_im.astype(jnp.float32)
    dt = jnp.exp(log_dt.astype(jnp.float32))[:, None]
    mag = jnp.exp(lr * dt)
    ar = mag * jnp.cos(li * dt)
    ai = mag * jnp.sin(li * dt)
    den = lr * lr + li * li
    zr = ((ar - 1.0) * lr + ai * li) / den
    zi = (ai * lr - (ar - 1.0) * li) / den
    bre = b_re.astype(jnp.float32)
    bim = b_im.astype(jnp.float32)
    bbr = zr[..., None] * bre - zi[..., None] * bim
    bbi = zr[..., None] * bim + zi[..., None] * bre
    xr = jnp.einsum('blgh,gph->blgp', ug, bbr)
    xi = jnp.einsum('blgh,gph->blgp', ug, bbi)
    a_r = jnp.broadcast_to(ar, (1, seq, S5_NG, S5_STATE))
    a_i = jnp.broadcast_to(ai, (1, seq, S5_NG, S5_STATE))
    _, _, sr, si = lax.associative_scan(_complex_affine_combine, (a_r, a_i, xr, xi), axis=1)
    y = (jnp.einsum('blgp,ghp->blgh', sr, c_re.astype(jnp.float32))
         - jnp.einsum('blgp,ghp->blgh', si, c_im.astype(jnp.float32)))
    y = y.reshape(bsz, seq, S5_CH) + d_skip.astype(jnp.float32) * uf
    y = jax.nn.gelu(y)
    val, gate = jnp.split(y @ w_glu.astype(jnp.float32), 2, axis=-1)
    return (val * jax.nn.sigmoid(gate)).astype(u.dtype)


def swa_sink_attention(q, k, v, q_gain, k_gain, sinks):
    bsz, seq, _ = q.shape
    nc = seq // CHUNK
    band = (WIN_CHUNKS + 1) * CHUNK
    q = rmsnorm(q.reshape(bsz, seq, ATT_QH, ATT_HD), q_gain)
    k = rmsnorm(k.reshape(bsz, seq, ATT_KVH, ATT_HD), k_gain)
    v = v.reshape(bsz, seq, ATT_KVH, ATT_HD)
    qc = q.reshape(bsz, nc, CHUNK, ATT_KVH, ATT_REP, ATT_HD)
    pad = ((0, 0), (WIN_CHUNKS * CHUNK, 0), (0, 0), (0, 0))
    kp = jnp.pad(k, pad).reshape(bsz, nc + WIN_CHUNKS, CHUNK, ATT_KVH, ATT_HD)
    vp = jnp.pad(v, pad).reshape(bsz, nc + WIN_CHUNKS, CHUNK, ATT_KVH, ATT_HD)
    kb = jnp.concatenate([kp[:, j:j + nc] for j in range(WIN_CHUNKS + 1)], axis=2)
    vb = jnp.concatenate([vp[:, j:j + nc] for j in range(WIN_CHUNKS + 1)], axis=2)
    chunk_idx = jnp.arange(nc)[:, None] + jnp.arange(WIN_CHUNKS + 1)[None, :] - WIN_CHUNKS
    valid = jnp.repeat(chunk_idx >= 0, CHUNK, axis=1)
    s = jnp.einsum('bnqkrd,bnskd->bnkrqs', qc, kb).astype(jnp.float32) * ATT_SCALE
    s = jnp.where(valid[None, :, None, None, None, :], s, -jnp.inf)
    sink = sinks.astype(jnp.float32).reshape(1, 1, ATT_KVH, ATT_REP, 1, 1)
    m = jnp.maximum(jnp.max(s, axis=-1, keepdims=True), sink)
    p = jnp.exp(s - m)
    p = p / (jnp.sum(p, axis=-1, keepdims=True) + jnp.exp(sink - m))
    o = jnp.einsum('bnkrqs,bnskd->bnqkrd', p.astype(vb.dtype), vb)
    del band
    return o.reshape(bsz, seq, ATT_QH * ATT_HD)


def hgrn2_mixer(q, f, i, g, lower_bound, o_gain):
    bsz, seq, _ = q.shape
    nb = seq // HG_BLOCK
    shp_k = (bsz, nb, HG_BLOCK, HG_HEADS, HG_DK)
    shp_v = (bsz, nb, HG_BLOCK, HG_HEADS, HG_DV)
    qf = q.astype(jnp.float32).reshape(shp_k)
    lb = lower_bound.astype(jnp.float32).reshape(HG_HEADS, HG_DK)
    fg = lb + (1.0 - lb) * jax.nn.sigmoid(f.astype(jnp.float32).reshape(shp_k))
    kk = 1.0 - fg
    vv = i.astype(jnp.float32).reshape(shp_v)
    bcum = jnp.cumsum(jnp.log(fg), axis=2)
    causal = jnp.tril(jnp.ones((HG_BLOCK, HG_BLOCK), dtype=bool))
    decay = jnp.exp(jnp.where(causal[:, :, None, None],
                              bcum[:, :, :, None] - bcum[:, :, None, :], -jnp.inf))
    scores = jnp.einsum('bnthk,bnshk,bntshk->bnhts', qf, kk, decay)
    o_intra = jnp.einsum('bnhts,bnshv->bnthv', scores, vv)
    b_last = bcum[:, :, -1]
    k_end = kk * jnp.exp(b_last[:, :, None] - bcum)
    upd = jnp.einsum('bnshk,bnshv->bnhkv', k_end, vv)

    def step(state, xs):
        dec, u = xs
        return dec[..., None] * state + u, state

    init = jnp.zeros((bsz, HG_HEADS, HG_DK, HG_DV), jnp.float32)
    _, s_prev = lax.scan(step, init, (jnp.moveaxis(jnp.exp(b_last), 1, 0), jnp.moveaxis(upd, 1, 0)))
    s_prev = jnp.moveaxis(s_prev, 0, 1)
    o_inter = jnp.einsum('bnthk,bnhkv->bnthv', qf * jnp.exp(bcum), s_prev)
    o = (o_intra + o_inter).reshape(bsz, seq, HG_HEADS, HG_DV)
    gate = jax.nn.silu(g.astype(jnp.float32).reshape(bsz, seq, HG_HEADS, HG_DV))
    o = rmsnorm(o, o_gain) * gate
    return o.reshape(bsz, seq, HG_HEADS * HG_DV).astype(q.dtype)


def setup_inputs(seed: int = 0) -> dict:
    key = jax.random.key(seed)
    ks = jax.random.split(key, 24)
    f32 = jnp.float32
    nrm = lambda k, shape, scale: jax.random.normal(k, shape, f32) * scale
    lam_im0 = jnp.pi * jnp.arange(S5_STATE, dtype=f32)
    return {
        'x': jax.random.normal(ks[0], (BATCH, SEQ, D_MODEL), f32),
        'w_in': nrm(ks[1], (DEPTH, D_MODEL, D_IN), D_MODEL ** -0.5),
        'w_out': nrm(ks[2], (DEPTH, D_MIX, D_MODEL), D_MIX ** -0.5),
        'norm_mix': 1.0 + nrm(ks[3], (DEPTH, D_MODEL), 0.05),
        'norm_ffn': 1.0 + nrm(ks[4], (DEPTH, D_MODEL), 0.05),
        'conv_w': nrm(ks[5], (DEPTH, CONV_W, GROUP_W), CONV_W ** -0.5),
        's5_lam_re': -0.5 + nrm(ks[6], (DEPTH, S5_NG, S5_STATE), 0.01),
        's5_lam_im': lam_im0 + nrm(ks[7], (DEPTH, S5_NG, S5_STATE), 0.01),
        's5_b_re': nrm(ks[8], (DEPTH, S5_NG, S5_STATE, S5_GROUP), (2 * S5_GROUP) ** -0.5),
        's5_b_im': nrm(ks[9], (DEPTH, S5_NG, S5_STATE, S5_GROUP), (2 * S5_GROUP) ** -0.5),
        's5_c_re': nrm(ks[10], (DEPTH, S5_NG, S5_GROUP, S5_STATE), (2 * S5_STATE) ** -0.5),
        's5_c_im': nrm(ks[11], (DEPTH, S5_NG, S5_GROUP, S5_STATE), (2 * S5_STATE) ** -0.5),
        's5_d': nrm(ks[12], (DEPTH, S5_CH), 1.0),
        's5_log_dt': jax.random.uniform(ks[13], (DEPTH, S5_NG), f32,
                                        np.log(S5_DT_MIN).astype(np.float32),
                                        np.log(S5_DT_MAX).astype(np.float32)),
        's5_w_glu': nrm(ks[14], (DEPTH, S5_CH, 2 * S5_CH), S5_CH ** -0.5),
        'attn_q_norm': 1.0 + nrm(ks[15], (DEPTH, ATT_HD), 0.05),
        'attn_k_norm': 1.0 + nrm(ks[16], (DEPTH, ATT_HD), 0.05),
        'attn_sinks': nrm(ks[17], (DEPTH, ATT_QH), 0.5),
        'hg_lower_bounds': 1.0 + nrm(ks[18], (DEPTH, HG_HEADS * HG_DK), 0.1),
        'hg_out_norm': 1.0 + nrm(ks[19], (DEPTH, HG_DV), 0.05),
        'group_norm': 1.0 + nrm(ks[20], (DEPTH, D_MIX), 0.05),
        'w_ff1': nrm(ks[21], (DEPTH, D_MODEL, D_FF), D_MODEL ** -0.5),
        'w_ff2': nrm(ks[22], (DEPTH, D_FF, D_MODEL), D_FF ** -0.5),
    }


def reference(x, w_in, w_out, norm_mix, norm_ffn, conv_w, s5_lam_re, s5_lam_im,
              s5_b_re, s5_b_im, s5_c_re, s5_c_im, s5_d, s5_log_dt, s5_w_glu,
              attn_q_norm, attn_k_norm, attn_sinks, hg_lower_bounds, hg_out_norm,
              group_norm, w_ff1, w_ff2):
    bsz, seq, _ = x.shape
    split_idx = [int(v) for v in np.cumsum(SPLIT_SIZES)[:-1]]
    lb_p = jax.nn.softmax(hg_lower_bounds.astype(jnp.float32), axis=0)
    lb_table = jnp.cumsum(lb_p, axis=0) - lb_p[0]
    for l in range(DEPTH):
        h = rmsnorm(x, norm_mix[l])
        proj = h @ w_in[l]
        (cv_h, cv_b, cv_c, s5_u, a_q, a_k, a_v,
         hg_q, hg_f, hg_i, hg_g) = jnp.split(proj, split_idx, axis=-1)
        y_a = short_conv_mixer(cv_h, cv_b, cv_c, conv_w[l])
        y_b = s5_mixer(s5_u, s5_lam_re[l], s5_lam_im[l], s5_b_re[l], s5_b_im[l],
                       s5_c_re[l], s5_c_im[l], s5_d[l], s5_log_dt[l], s5_w_glu[l])
        y_c = swa_sink_attention(a_q, a_k, a_v, attn_q_norm[l], attn_k_norm[l], attn_sinks[l])
        y_d = hgrn2_mixer(hg_q, hg_f, hg_i, hg_g, lb_table[l], hg_out_norm[l])
        y = jnp.stack([y_a.astype(x.dtype), y_b.astype(x.dtype),
                       y_c.astype(x.dtype), y_d.astype(x.dtype)], axis=2)
        y = rmsnorm(y, group_norm[l].reshape(N_GROUPS, GROUP_W)).reshape(bsz, seq, D_MIX)
        x = x + (y @ w_out[l]).astype(x.dtype)
        h = rmsnorm(x, norm_ffn[l])
        x = x + (jnp.square(jax.nn.relu(h @ w_ff1[l])) @ w_ff2[l]).astype(x.dtype)
    return x
```

```python
import contextlib
import numpy as np
import concourse.bass as bass
import concourse.mybir as mybir
from concourse.bass_utils import run_bass_kernel_spmd

F32 = mybir.dt.float32
BF16 = mybir.dt.bfloat16
I32 = mybir.dt.int32
ALU = mybir.AluOpType
AF = mybir.ActivationFunctionType
AX = mybir.AxisListType

NCORES = 8
SEQ_PER_CORE = 4
SEQ = 2048
NT = 512
TILES_PER_SEQ = SEQ // NT
DM = 1024
DEPTH = 2
NCHUNK = 23
EPS = 1e-6
TS5 = 8
NC5 = NT // TS5
TWO_PI = 6.283185307179586

ENGS = ("pe", "act", "dve", "pool", "sp")
SEM_CAP = 1000


class Op:
    __slots__ = ("eng", "fn", "deps", "is_dma", "chan", "needs_inc", "sem", "val")

    def __init__(self, eng, fn, is_dma=False, chan=None):
        self.eng = eng
        self.fn = fn
        self.deps = []
        self.is_dma = is_dma
        self.chan = chan
        self.needs_inc = is_dma
        self.sem = None
        self.val = None


def _keys(items):
    out = []
    for it in items:
        if hasattr(it, "keys_"):
            out.extend(it.keys_)
        else:
            out.append(it)
    return out


class Prog:
    def __init__(self, nc, same_engine_sync=True):
        self.nc = nc
        self.ops = {e: [] for e in ENGS}
        self.last_writer = {}
        self.readers = {}
        self.same_engine_sync = same_engine_sync

    def op(self, eng, fn, reads=(), writes=(), is_dma=False, chan=None, force=False):
        reads = _keys(reads)
        writes = _keys(writes)
        o = Op(eng, fn, is_dma, chan)
        deps = {}
        for k in reads:
            w = self.last_writer.get(k)
            if w is not None:
                deps[id(w)] = w
        for k in writes:
            w = self.last_writer.get(k)
            if w is not None:
                deps[id(w)] = w
            for r in self.readers.get(k, ()):
                deps[id(r)] = r
        for d in deps.values():
            if (not d.is_dma) and d.eng == eng and ((eng == "pe" and not force) or not self.same_engine_sync):
                continue
            d.needs_inc = True
            o.deps.append(d)
        for k in reads:
            self.readers.setdefault(k, []).append(o)
        for k in writes:
            self.last_writer[k] = o
            self.readers[k] = []
        self.ops[eng].append(o)
        return o

    def dma(self, eng, out, in_, reads=(), writes=(), chan=None, **kw):
        return self.op(eng, lambda e: e.dma_start(out=out, in_=in_, **kw), reads, list(writes) + [("chan", chan)], is_dma=True, chan=chan)

    def emit(self, final_deps=()):
        nc = self.nc
        stack = contextlib.ExitStack()
        eng_sems = {e: [] for e in ENGS}
        eng_cnt = {e: 0 for e in ENGS}
        chan_sem, chan_cnt = {}, {}
        nsem = 0
        for e in ENGS:
            for o in self.ops[e]:
                if o.is_dma:
                    if o.chan not in chan_sem or chan_cnt[o.chan] + 16 > SEM_CAP:
                        chan_sem[o.chan] = stack.enter_context(nc.semaphore(f"c{nsem}"))
                        nsem += 1
                        chan_cnt[o.chan] = 0
                    chan_cnt[o.chan] += 16
                    o.sem, o.val = chan_sem[o.chan], chan_cnt[o.chan]
                elif o.needs_inc:
                    if eng_cnt[e] % SEM_CAP == 0:
                        eng_sems[e].append(stack.enter_context(nc.semaphore(f"e{nsem}")))
                        nsem += 1
                    eng_cnt[e] += 1
                    o.sem, o.val = eng_sems[e][-1], (eng_cnt[e] - 1) % SEM_CAP + 1
        self.nsem = nsem
        final_deps = list(final_deps)

        def run_engine(ename, eng):
            waited = {}

            def wait_for(d):
                key = id(d.sem)
                if waited.get(key, 0) >= d.val:
                    return
                eng.wait_ge(d.sem, d.val)
                waited[key] = d.val

            for o in self.ops[ename]:
                for d in o.deps:
                    wait_for(d)
                ins = o.fn(eng)
                if o.needs_inc:
                    ins.then_inc(o.sem, 16 if o.is_dma else 1)
            if ename == "sp":
                for d in final_deps:
                    wait_for(d)

        with nc.Block() as block:
            @block.tensor
            def _(e):
                run_engine("pe", e)

            @block.scalar
            def _(e):
                run_engine("act", e)

            @block.vector
            def _(e):
                run_engine("dve", e)

            @block.gpsimd
            def _(e):
                run_engine("pool", e)

            @block.sync
            def _(e):
                run_engine("sp", e)
        stack.close()


def _col8(v):
    return np.ascontiguousarray(v.reshape(8, 128).T)


def _col2(v):
    return np.ascontiguousarray(v.reshape(2, 128).T)


def _qperm():
    idx = np.zeros(256, np.int64)
    for r in range(2):
        for kh in range(2):
            for d in range(64):
                idx[r * 128 + kh * 64 + d] = (kh * 2 + r) * 64 + d
    return idx


_PRM_FIELDS = [
    ("gmix", 8), ("gffn", 8), ("ggn", 8), ("convw", 6), ("s5d", 2), ("qg", 1), ("kg", 1), ("sink", 2),
    ("hlb0", 2), ("hlb1", 2), ("og", 1),
    ("A_lre", 128), ("A_lim", 128), ("A_ldt", 2), ("A_bre", 128), ("A_bim", 128),
    ("A_cre", 2048), ("A_cim", 2048),
    ("B_lre", 8), ("B_lim", 8), ("B_ldt", 8), ("B_cre", 128), ("B_cim", 128),
]
_PRM_OFF = {}
_o = 0
for _n, _w in _PRM_FIELDS:
    _PRM_OFF[_n] = (_o, _w)
    _o += _w
NPRM = _o

_CST_FIELDS = [
    ("ident", 128), ("bones", 128),
    ("mgp0", 1), ("mgp1", 1), ("mB0", 1), ("mB1", 1),
    ("mg8", 8), ("tauA", 8), ("tauB", 8), ("cidx", 64),
    ("segm", 512), ("cmask", 64),
]
_CST_OFF = {}
_o = 0
for _n, _w in _CST_FIELDS:
    _CST_OFF[_n] = (_o, _w)
    _o += _w
NCST = _o


def _build_consts():
    c = np.zeros((128, NCST), np.float32)

    def put(name, arr):
        o, w = _CST_OFF[name]
        c[:, o:o + w] = np.asarray(arr, np.float32).reshape(128, w)

    p = np.arange(128)
    put("ident", np.eye(128))
    bo = np.zeros((128, 128))
    bo[:64, :64] = 1
    bo[64:, 64:] = 1
    put("bones", bo)
    put("mgp0", ((p // 16) % 2 == 0)[:, None])
    put("mgp1", ((p // 16) % 2 == 1)[:, None])
    put("mB0", (p < 64)[:, None])
    put("mB1", (p >= 64)[:, None])
    put("mg8", (p[:, None] // 16) == np.arange(8)[None, :])
    put("tauA", np.tile(np.arange(8.0)[None], (128, 1)))
    put("tauB", np.tile(np.arange(1.0, 9.0)[None], (128, 1)))
    put("cidx", np.tile(np.arange(1.0, 65.0)[None], (128, 1)))
    seg = np.ones((8, 64))
    seg[:, 0] = 0
    put("segm", np.tile(seg.reshape(1, 512), (128, 1)))
    s = (p % 64)[:, None]
    t = np.arange(64)[None, :]
    put("cmask", (s <= t))
    return c


def _build_params(inp, l):
    prm = np.zeros((128, NPRM), np.float32)

    def put(name, arr):
        o, w = _PRM_OFF[name]
        prm[:, o:o + w] = np.asarray(arr, np.float32).reshape(128, w)

    qp = _qperm()
    put("gmix", _col8(inp["norm_mix"][l]))
    put("gffn", _col8(inp["norm_ffn"][l]))
    gn = np.array(inp["group_norm"][l])
    gn[512:768] = np.array(inp["group_norm"][l])[512:768][qp]
    put("ggn", _col8(gn))
    cw = np.asarray(inp["conv_w"][l])
    put("convw", np.stack([_col2(cw[i]) for i in range(3)], axis=2).reshape(128, 6))
    put("s5d", _col2(np.asarray(inp["s5_d"][l])))
    put("qg", np.tile(np.asarray(inp["attn_q_norm"][l]), 2)[:, None])
    put("kg", np.tile(np.asarray(inp["attn_k_norm"][l]), 2)[:, None])
    sk = np.asarray(inp["attn_sinks"][l])
    put("sink", np.stack([np.repeat(sk[[0 * 2 + r, 1 * 2 + r]], 64) for r in range(2)], axis=1))
    put("hlb0", _col2(np.asarray(inp["hg_lower_bounds"][0])))
    put("hlb1", _col2(np.asarray(inp["hg_lower_bounds"][1])))
    put("og", np.tile(np.asarray(inp["hg_out_norm"][l]), 2)[:, None])
    lre, lim, ldt = (np.asarray(inp[k][l]) for k in ("s5_lam_re", "s5_lam_im", "s5_log_dt"))
    bre, bim = np.asarray(inp["s5_b_re"][l]), np.asarray(inp["s5_b_im"][l])
    cre, cim = np.asarray(inp["s5_c_re"][l]), np.asarray(inp["s5_c_im"][l])
    put("A_lre", np.repeat(lre.reshape(2, 8, 1, 64), 16, axis=2).transpose(1, 2, 0, 3).reshape(128, 128))
    put("A_lim", np.repeat(lim.reshape(2, 8, 1, 64), 16, axis=2).transpose(1, 2, 0, 3).reshape(128, 128))
    put("A_ldt", np.repeat(ldt.reshape(2, 8, 1), 16, axis=2).transpose(1, 2, 0).reshape(128, 2))
    put("A_bre", bre.reshape(2, 8, 64, 16).transpose(1, 3, 0, 2).reshape(128, 128))
    put("A_bim", bim.reshape(2, 8, 64, 16).transpose(1, 3, 0, 2).reshape(128, 128))
    put("A_cre", np.repeat(cre.reshape(2, 8, 1, 16, 64), 16, axis=2).transpose(1, 2, 0, 3, 4).reshape(128, 2048))
    put("A_cim", np.repeat(cim.reshape(2, 8, 1, 16, 64), 16, axis=2).transpose(1, 2, 0, 3, 4).reshape(128, 2048))
    put("B_lre", lre.reshape(8, 2, 64).transpose(1, 2, 0).reshape(128, 8))
    put("B_lim", lim.reshape(8, 2, 64).transpose(1, 2, 0).reshape(128, 8))
    put("B_ldt", np.repeat(ldt.reshape(8, 2, 1), 64, axis=2).transpose(1, 2, 0).reshape(128, 8))
    put("B_cre", cre.reshape(8, 2, 16, 64).transpose(1, 3, 0, 2).reshape(128, 128))
    put("B_cim", cim.reshape(8, 2, 16, 64).transpose(1, 3, 0, 2).reshape(128, 128))
    return prm


def _chunks_kn(W):
    K, N = W.shape
    out = []
    for cg in range(N // 512):
        for kg in range(K // 1024):
            blk = W[kg * 1024:(kg + 1) * 1024, cg * 512:(cg + 1) * 512]
            out.append(np.ascontiguousarray(blk.reshape(8, 128, 512).transpose(1, 0, 2)).reshape(128, 4096))
    return out


def _build_weights(inp):
    qp = _qperm()
    chunks = []
    for l in range(DEPTH):
        w_in = np.asarray(inp["w_in"][l])
        win = np.array(w_in)
        win[:, 1024:1280] = w_in[:, 1024:1280][:, qp]
        w_out = np.asarray(inp["w_out"][l])
        wout = np.array(w_out)
        wout[512:768, :] = w_out[512:768, :][qp, :]
        chunks += _chunks_kn(win)
        chunks += _chunks_kn(wout)
        chunks += _chunks_kn(np.asarray(inp["w_ff1"][l]))
        chunks += _chunks_kn(np.asarray(inp["w_ff2"][l]))
    wch = np.stack(chunks, axis=0).astype(np.float32)
    glu = np.stack([np.asarray(inp["s5_w_glu"][l]).reshape(2, 128, 512).transpose(1, 0, 2) for l in range(DEPTH)], axis=0)
    return wch, np.ascontiguousarray(glu, np.float32)


class Buf:
    def __init__(self, ap2d, keys):
        self.a = ap2d
        self.keys_ = keys

    def __getitem__(self, k):
        return self.a[k]

    def v3(self, b):
        return self.a.rearrange("p (a b) -> p a b", b=b)


def bc(ap, shape):
    return ap.broadcast_to(list(shape))


def build_program(n_seq=SEQ_PER_CORE, n_tiles=TILES_PER_SEQ, layers=(0, 1), taps=None, same_engine_sync=True,
                  stop_after=None, skip_prologue=False):
    taps = taps or {}

    class StopBuild(Exception):
        pass

    def CHK(name):
        if stop_after == name:
            raise StopBuild()
    nc = bass.Bass("TRN2", target_bir_lowering=False)
    P = Prog(nc, same_engine_sync=same_engine_sync)
    NL = DEPTH

    xT = nc.dram_tensor("xT", [SEQ_PER_CORE, DM, SEQ], F32, kind="ExternalInput").ap()
    wch = nc.dram_tensor("wch", [NL * NCHUNK, 128, 4096], F32, kind="ExternalInput").ap()
    glu_d = nc.dram_tensor("glu", [NL, 128, 2, 512], F32, kind="ExternalInput").ap()
    prm_d = nc.dram_tensor("prm", [NL, 128, NPRM], F32, kind="ExternalInput").ap()
    cst_d = nc.dram_tensor("cst", [128, NCST], F32, kind="ExternalInput").ap()
    oT = nc.dram_tensor("oT", [SEQ_PER_CORE, DM, SEQ], F32, kind="ExternalOutput").ap()
    wsc = nc.dram_tensor("wsc", [NL * NCHUNK, 128, 4096], BF16).ap()
    tap_out = {}
    for name, shape in taps.items():
        if name.startswith("_"):
            continue
        tap_out[name] = nc.dram_tensor("tap_" + name, list(shape), F32, kind="ExternalOutput").ap()
    s5m_d = nc.dram_tensor("s5m_d", [NL, 128, 10240], BF16).ap()
    tap_dmas = []

    def sb(name, shape, dt=F32):
        return nc.alloc_sbuf_tensor("s_" + name, list(shape), dt)

    NRING = 4
    ring = [sb(f"ring{i}", [128, 8, 512], BF16) for i in range(NRING)]
    xs = sb("xs", [128, 8, NT], F32)
    hbuf = sb("hbuf", [128, 8, NT], BF16)
    cst = sb("cst", [128, NCST], F32)
    NF, NB = 20, 32
    farena = sb("farena", [128, NF * NT], F32)
    barena = sb("barena", [128, NB * NT], BF16)
    fmap = [False] * NF
    bmap = [False] * NB

    def _alloc(arena, amap, tag, n):
        for i in range(len(amap) - n + 1):
            if not any(amap[i:i + n]):
                for j in range(i, i + n):
                    amap[j] = True
                b = Buf(arena[:, i * NT:(i + n) * NT], [(tag, j) for j in range(i, i + n)])
                b.rng = (i, n)
                return b
        raise RuntimeError(f"arena {tag} exhausted")

    def falloc(n=1):
        return _alloc(farena, fmap, "fp", n)

    def balloc(n=1):
        return _alloc(barena, bmap, "bp", n)

    def free(b):
        i, n = b.rng
        amap = fmap if b.keys_[0][0] == "fp" else bmap
        for j in range(i, i + n):
            assert amap[j]
            amap[j] = False

    banks = [nc.alloc_psum_tensor(f"bank{i}", [128, 512], F32) for i in range(8)]
    BK = [("bank", i) for i in range(8)]

    def cs(name):
        o, w = _CST_OFF[name]
        return cst[:, o:o + w]

    ident_bf = sb("ident_bf", [128, 128], BF16)
    bones_bf = sb("bones_bf", [128, 128], BF16)
    ones_bf = sb("ones_bf", [128, 128], BF16)
    sb_int = sb("sb_int", [128, 512], I32)
    Amz = sb("Amz", [128, 2048], BF16)

    s5m = sb("s5m", [128, 10240], BF16)
    L1v = s5m[:, 0:4096].rearrange("p (j r t c) -> p j r t c", j=2, r=2, t=8)
    L3v = s5m[:, 4096:8192].rearrange("p (r q t c) -> p r q t c", r=2, q=8, t=8)
    KLv = s5m[:, 8192:10240].rearrange("p (j t c) -> p j t c", j=2, t=8)
    L = []
    for l in range(NL):
        d = dict(
            sp=sb(f"sp{l}", [128, 48], F32),
            glu=sb(f"glu{l}", [128, 2, 512], BF16),
            L1=L1v,
            L3=L3v,
            KL=KLv,
            cosR=sb(f"cosR{l}", [128, 512], F32),
            sinR=sb(f"sinR{l}", [128, 512], F32),
            rtab=sb(f"rtab{l}", [128, 512], F32),
            r8=sb(f"r8_{l}", [128, 8], F32),
            zbuf=sb(f"zbuf{l}", [128, 2, NT + 2], F32),
            Er=sb(f"Er{l}", [128, 8, NC5 + 1], F32),
            Ei=sb(f"Ei{l}", [128, 8, NC5 + 1], F32),
            kbuf=sb(f"kbuf{l}", [128, 128 + NT], BF16),
            vbuf=sb(f"vbuf{l}", [128, 5, 128], BF16),
            S=[sb(f"S{l}_{j}", [128, 64], F32) for j in range(2)],
        )
        L.append(d)
    SPC = dict(gmix=0, gffn=8, ggn=16, convw=24, s5d=30, qgs=32, kg=33, esink=34, lb=36, oml=38, og=40)

    def ACT(out, in_, func, reads, writes, scale=1.0, bias=None):
        if bias is None:
            return P.op("act", lambda e: e.activation(out=out, in_=in_, func=func, scale=scale), reads, writes)
        return P.op("act", lambda e: e.activation(out=out, in_=in_, func=func, scale=scale, bias=bias), reads, writes)

    def TT(eng, out, a, b, op, reads, writes):
        return P.op(eng, lambda e: e.tensor_tensor(out=out, in0=a, in1=b, op=op), reads, writes)

    def TS(eng, out, a, s1, op0, reads, writes, s2=None, op1=None):
        if op1 is None:
            return P.op(eng, lambda e: e.tensor_scalar(out=out, in0=a, scalar1=s1, scalar2=None, op0=op0), reads, writes)
        return P.op(eng, lambda e: e.tensor_scalar(out=out, in0=a, scalar1=s1, scalar2=s2, op0=op0, op1=op1), reads, writes)

    def STT(out, in0, scalar, in1, op0, op1, reads, writes):
        return P.op("dve", lambda e: e.scalar_tensor_tensor(out=out, in0=in0, scalar=scalar, in1=in1, op0=op0, op1=op1), reads, writes)

    def CP(eng, out, in_, reads, writes):
        if eng == "act":
            return P.op("act", lambda e: e.activation(out=out, in_=in_, func=AF.Copy), reads, writes)
        return P.op(eng, lambda e: e.tensor_copy(out=out, in_=in_), reads, writes)

    def RECIP(out, in_, reads, writes):
        return P.op("dve", lambda e: e.reciprocal(out=out, in_=in_), reads, writes)

    def MM(mms, reads, writes, force=False):
        def fn(e):
            ins = None
            for m in mms:
                if m.get("tp") is not None:
                    ins = e.matmul(m["out"], lhsT=m["lhsT"], rhs=m["rhs"], start=m["start"], stop=m["stop"], skip_group_check=True,
                                   tile_position=m["tp"])
                else:
                    ins = e.matmul(m["out"], lhsT=m["lhsT"], rhs=m["rhs"], start=m["start"], stop=m["stop"], skip_group_check=True)
            return ins
        return P.op("pe", fn, reads, writes, force=force)

    def mm(out, lhsT, rhs, start=True, stop=True, tp=None):
        return dict(out=out, lhsT=lhsT, rhs=rhs, start=start, stop=stop, tp=tp)

    def TAP(name, idx, src_ap, reads):
        if name in tap_out:
            o = P.dma("sp", tap_out[name][idx], src_ap, reads=reads, writes=[("tap", name, idx)], chan=("tap", name))
            tap_dmas.append(o)

    P.dma("sp", cst[:], cst_d, writes=["cst"], chan="cst")
    CP("dve", ident_bf[:], cs("ident"), ["cst"], ["ident_bf"])
    CP("dve", bones_bf[:], cs("bones"), ["cst"], ["bones_bf"])
    P.op("pool", lambda e: e.memset(ones_bf[:], 1.0), writes=["ones_bf"])
    P.op("pool", lambda e: e.memset(Amz[:, :], 0.0), writes=["Amz"])

    for l in layers:
        for c in range(NCHUNK):
            ci = l * NCHUNK + c
            P.dma("pool", wsc[ci].rearrange("p (a b) -> p a b", b=2048), wch[ci].rearrange("p (a b) -> p a b", b=2048),
                  writes=[("wsc", ci)], chan=("wcast", ci % 4))

    prm_sb = falloc(2)
    _ac_lo = _PRM_OFF["A_cre"][0]
    _ac_hi = _PRM_OFF["A_cim"][0] + 2048
    NSMALL = NPRM - 4096
    assert NSMALL <= 2 * NT

    def prologue_layer(l):
        d = L[l]
        sp = d["sp"]
        SPK = ("sp", l)
        P.dma("sp", prm_sb[:, 0:_ac_lo], prm_d[l, :, 0:_ac_lo], writes=[prm_sb], chan="prm")
        P.dma("sp", prm_sb[:, _ac_lo:NSMALL], prm_d[l, :, _ac_hi:NPRM], writes=[prm_sb], chan="prm2")

        def pf(name, a=0, b=None):
            o, w = _PRM_OFF[name]
            if o >= _ac_hi:
                o -= 4096
            b = w if b is None else b
            return prm_sb[:, o + a:o + b]

        RD = [prm_sb, "cst"]
        for nm in ("gmix", "gffn", "ggn", "convw", "s5d", "kg", "og"):
            w = _PRM_OFF[nm][1]
            CP("dve", sp[:, SPC[nm]:SPC[nm] + w], pf(nm), RD, [SPK])
        TS("dve", sp[:, SPC["qgs"]:SPC["qgs"] + 1], pf("qg"), 0.125, ALU.mult, RD, [SPK])
        ACT(sp[:, SPC["esink"]:SPC["esink"] + 2], pf("sink"), AF.Exp, RD, [SPK])
        if l == 0:
            P.op("pool", lambda e: e.memset(sp[:, SPC["lb"]:SPC["lb"] + 2], 0.0), writes=[SPK])
        else:
            tmp = falloc()
            TT("dve", tmp[:, 0:2], pf("hlb1"), pf("hlb0"), ALU.subtract, RD, [tmp])
            ACT(sp[:, SPC["lb"]:SPC["lb"] + 2], tmp[:, 0:2], AF.Sigmoid, [tmp], [SPK])
            free(tmp)
        TS("dve", sp[:, SPC["oml"]:SPC["oml"] + 2], sp[:, SPC["lb"]:SPC["lb"] + 2], -1.0, ALU.mult, [SPK], [SPK], s2=1.0, op1=ALU.add)
        gl32 = falloc(2)
        P.dma("sp", gl32[:, 0:1024].rearrange("p (k n) -> p k n", n=512), glu_d[l], writes=[gl32], chan="glu")
        CP("dve", d["glu"][:, 0, :], gl32[:, 0:512], [gl32], [("glu", l)])
        CP("dve", d["glu"][:, 1, :], gl32[:, 512:1024], [gl32], [("glu", l)])
        free(gl32)

        def trig(u, n, cos_out, sin_out, ub, cb, sbk, shape3=None):
            t1 = falloc(); t2 = falloc()
            for (shift, outv, ob) in ((0.0, sin_out, sbk), (0.25, cos_out, cb)):
                TS("dve", t1[:, 0:n], u, shift + 64.0, ALU.add, [ub], [t1])
                CP("dve", sb_int[:, 0:n], t1[:, 0:n], [t1], ["sb_int"])
                CP("dve", t2[:, 0:n], sb_int[:, 0:n], ["sb_int"], [t2])
                TT("dve", t1[:, 0:n], t1[:, 0:n], t2[:, 0:n], ALU.subtract, [t1, t2], [t1])
                TS("dve", t2[:, 0:n], t1[:, 0:n], 0.5, ALU.is_gt, [t1], [t2])
                TT("dve", t1[:, 0:n], t1[:, 0:n], t2[:, 0:n], ALU.subtract, [t1, t2], [t1])
                TS("dve", t2[:, 0:n], t1[:, 0:n], -0.5, ALU.is_lt, [t1], [t2])
                TT("dve", t1[:, 0:n], t1[:, 0:n], t2[:, 0:n], ALU.add, [t1, t2], [t1])
                ACT(outv, t1[:, 0:n], AF.Sin, [t1], [ob], scale=TWO_PI)
            free(t1); free(t2)
            return None

        a_lr = falloc(); a_dt = falloc(); a_e1 = falloc(); a_th = falloc()
        TS("dve", a_lr[:, 0:128], pf("A_lre"), -1e-4, ALU.min, RD, [a_lr])
        ACT(a_dt[:, 0:2], pf("A_ldt"), AF.Exp, RD, [a_dt])
        for j in range(2):
            sl = slice(j * 64, (j + 1) * 64)
            TS("dve", a_e1[:, sl], a_lr[:, sl], a_dt[:, j:j + 1], ALU.mult, [a_lr, a_dt], [a_e1])
            TS("dve", a_th[:, sl], pf("A_lim")[:, sl], a_dt[:, j:j + 1], ALU.mult, RD + [a_dt], [a_th], s2=1.0 / TWO_PI, op1=ALU.mult)
        for j in range(2):
            sl = slice(j * 64, (j + 1) * 64)
            u = falloc(); me = falloc(); cc = falloc(); ss = falloc(); mr = falloc(); mi = falloc()
            u3, me3 = u.v3(64), me.v3(64)
            tau_b = bc(cs("tauA").unsqueeze(2), [128, 8, 64])
            TT("dve", u3, bc(a_th[:, sl].unsqueeze(1), [128, 8, 64]), tau_b, ALU.mult, [a_th, "cst"], [u])
            TT("dve", me3, bc(a_e1[:, sl].unsqueeze(1), [128, 8, 64]), tau_b, ALU.mult, [a_e1, "cst"], [me])
            ACT(me[:, :], me[:, :], AF.Exp, [me], [me])
            trig(u[:, :], 512, cc[:, :], ss[:, :], u, cc, ss)
            TT("dve", mr[:, :], me[:, :], cc[:, :], ALU.mult, [me, cc], [mr])
            TT("dve", mi[:, :], me[:, :], ss[:, :], ALU.mult, [me, ss], [mi])
            free(u); free(me); free(cc); free(ss)
            w = falloc()
            W = lambda i: w[:, i * 64:(i + 1) * 64]
            lr_, li_ = a_lr[:, sl], pf("A_lim")[:, sl]
            TT("dve", W(0), lr_, lr_, ALU.mult, [a_lr], [w])
            TT("dve", W(2), li_, li_, ALU.mult, RD, [w])
            TT("dve", W(0), W(0), W(2), ALU.add, [w], [w])
            RECIP(W(0), W(0), [w], [w])
            TS("dve", W(1), mr[:, 64:128], -1.0, ALU.add, [mr], [w])
            TT("dve", W(2), W(1), lr_, ALU.mult, [w, a_lr], [w])
            TT("dve", W(7), mi[:, 64:128], li_, ALU.mult, [mi] + RD, [w])
            TT("dve", W(2), W(2), W(7), ALU.add, [w], [w])
            TT("dve", W(3), W(2), W(0), ALU.mult, [w], [w])
            TT("dve", W(2), mi[:, 64:128], lr_, ALU.mult, [mi, a_lr], [w])
            TT("dve", W(7), W(1), li_, ALU.mult, [w] + RD, [w])
            TT("dve", W(2), W(2), W(7), ALU.subtract, [w], [w])
            TT("dve", W(4), W(2), W(0), ALU.mult, [w], [w])
            bre_, bim_ = pf("A_bre")[:, sl], pf("A_bim")[:, sl]
            TT("dve", W(5), W(3), bre_, ALU.mult, [w] + RD, [w])
            TT("dve", W(7), W(4), bim_, ALU.mult, [w] + RD, [w])
            TT("dve", W(5), W(5), W(7), ALU.subtract, [w], [w])
            TT("dve", W(6), W(3), bim_, ALU.mult, [w] + RD, [w])
            TT("dve", W(7), W(4), bre_, ALU.mult, [w] + RD, [w])
            TT("dve", W(6), W(6), W(7), ALU.add, [w], [w])
            p1r = falloc(); p1i = falloc(); t = falloc()
            bbr_b = bc(W(5).unsqueeze(1), [128, 8, 64]); bbi_b = bc(W(6).unsqueeze(1), [128, 8, 64])
            TT("dve", p1r.v3(64), mr.v3(64), bbr_b, ALU.mult, [mr, w], [p1r])
            TT("dve", t.v3(64), mi.v3(64), bbi_b, ALU.mult, [mi, w], [t])
            TT("dve", p1r[:, :], p1r[:, :], t[:, :], ALU.subtract, [p1r, t], [p1r])
            TT("dve", p1i.v3(64), mr.v3(64), bbi_b, ALU.mult, [mr, w], [p1i])
            TT("dve", t.v3(64), mi.v3(64), bbr_b, ALU.mult, [mi, w], [t])
            TT("dve", p1i[:, :], p1i[:, :], t[:, :], ALU.add, [p1i, t], [p1i])
            free(w); free(mr); free(mi)
            for ri, src in ((0, p1r), (1, p1i)):
                for gp in range(2):
                    TS("dve", d["L1"][:, j, ri, :, gp * 64:(gp + 1) * 64], src.v3(64), cs("mgp%d" % gp), ALU.mult, [src, "cst"], ["s5m"])
            kc = falloc()
            t2 = falloc()
            acr = falloc(2); aci = falloc(2)
            ocr, oci = _PRM_OFF["A_cre"][0], _PRM_OFF["A_cim"][0]
            P.dma("sp", acr[:, 0:1024], prm_d[l, :, ocr + j * 1024:ocr + (j + 1) * 1024], writes=[acr], chan="acr")
            P.dma("sp", aci[:, 0:1024], prm_d[l, :, oci + j * 1024:oci + (j + 1) * 1024], writes=[aci], chan="aci")
            for ho in range(16):
                cr_b = bc(acr[:, ho * 64:(ho + 1) * 64].unsqueeze(1), [128, 8, 64])
                ci_b = bc(aci[:, ho * 64:(ho + 1) * 64].unsqueeze(1), [128, 8, 64])
                TT("dve", t.v3(64), p1r.v3(64), cr_b, ALU.mult, [p1r, acr], [t])
                TT("dve", t2.v3(64), p1i.v3(64), ci_b, ALU.mult, [p1i, aci], [t2])
                TT("dve", t[:, :], t[:, :], t2[:, :], ALU.subtract, [t, t2], [t])
                P.op("dve", lambda e, ho=ho: e.tensor_reduce(out=kc[:, 0:128].rearrange("p (a b) -> p a b", b=16)[:, :, ho], in_=t.v3(64),
                                                             axis=AX.X, op=ALU.add), [t], [kc])
            free(t2); free(p1r); free(p1i); free(acr); free(aci)
            klf = falloc(2)
            for g in range(8):
                o_, _w = _CST_OFF["mg8"]
                TS("dve", klf[:, 0:1024].rearrange("p (a b) -> p a b", b=128)[:, :, g * 16:(g + 1) * 16],
                   kc[:, 0:128].rearrange("p (a b) -> p a b", b=16), cst[:, o_ + g:o_ + g + 1], ALU.mult, [kc, "cst"], [klf])
            STT(klf[:, 0:128], cs("ident"), sp[:, SPC["s5d"] + j:SPC["s5d"] + j + 1], klf[:, 0:128], ALU.mult, ALU.add, ["cst", SPK, klf], [klf])
            CP("dve", d["KL"][:, j, :, :], klf[:, 0:1024].rearrange("p (a b) -> p a b", b=128), [klf], ["s5m"])
            free(kc); free(klf); free(t)
        free(a_lr); free(a_dt); free(a_e1); free(a_th)

        b_ = falloc()
        Bc = lambda i: b_[:, i * 8:(i + 1) * 8]
        TS("dve", Bc(0), pf("B_lre"), -1e-4, ALU.min, RD, [b_])
        ACT(Bc(1), pf("B_ldt"), AF.Exp, RD, [b_])
        TT("dve", Bc(2), Bc(0), Bc(1), ALU.mult, [b_], [b_])
        TT("dve", Bc(3), pf("B_lim"), Bc(1), ALU.mult, RD + [b_], [b_])
        TS("dve", Bc(3), Bc(3), 1.0 / TWO_PI, ALU.mult, [b_], [b_])
        u = falloc(); me = falloc(); cc = falloc(); ss = falloc()
        tauB_b = bc(cs("tauB").unsqueeze(1), [128, 8, 8])
        TT("dve", u[:, 0:64].rearrange("p (a b) -> p a b", b=8), bc(Bc(3).unsqueeze(2), [128, 8, 8]), tauB_b, ALU.mult, [b_, "cst"], [u])
        TT("dve", me[:, 0:64].rearrange("p (a b) -> p a b", b=8), bc(Bc(2).unsqueeze(2), [128, 8, 8]), tauB_b, ALU.mult, [b_, "cst"], [me])
        ACT(me[:, 0:64], me[:, 0:64], AF.Exp, [me], [me])
        trig(u[:, 0:64], 64, cc[:, 0:64], ss[:, 0:64], u, cc, ss)
        TT("dve", cc[:, 0:64], cc[:, 0:64], me[:, 0:64], ALU.mult, [cc, me], [cc])
        TT("dve", ss[:, 0:64], ss[:, 0:64], me[:, 0:64], ALU.mult, [ss, me], [ss])
        for qh in range(2):
            cr_b = bc(pf("B_cre")[:, qh * 64:(qh + 1) * 64].rearrange("p (q h) -> p q h", h=16).unsqueeze(2), [128, 4, 8, 16])
            ci_b = bc(pf("B_cim")[:, qh * 64:(qh + 1) * 64].rearrange("p (q h) -> p q h", h=16).unsqueeze(2), [128, 4, 8, 16])
            mr_b = bc(cc[:, qh * 32:(qh + 1) * 32].rearrange("p (q t) -> p q t", t=8).unsqueeze(3), [128, 4, 8, 16])
            mi_b = bc(ss[:, qh * 32:(qh + 1) * 32].rearrange("p (q t) -> p q t", t=8).unsqueeze(3), [128, 4, 8, 16])
            c1 = falloc(); c2 = falloc()
            v4 = lambda b: b[:, :].rearrange("p (q t h) -> p q t h", t=8, h=16)
            TT("dve", v4(c1), cr_b, mr_b, ALU.mult, RD + [cc], [c1])
            TT("dve", v4(c2), ci_b, mi_b, ALU.mult, RD + [ss], [c2])
            TT("dve", c1[:, :], c1[:, :], c2[:, :], ALU.subtract, [c1, c2], [c1])
            for gp in range(2):
                TS("dve", d["L3"][:, 0, qh * 4:(qh + 1) * 4, :, gp * 16:(gp + 1) * 16], v4(c1), cs("mB%d" % gp), ALU.mult, [c1, "cst"], ["s5m"])
            TT("dve", v4(c1), cr_b, mi_b, ALU.mult, RD + [ss], [c1])
            TT("dve", v4(c2), ci_b, mr_b, ALU.mult, RD + [cc], [c2])
            TT("dve", c1[:, :], c1[:, :], c2[:, :], ALU.add, [c1, c2], [c1])
            for gp in range(2):
                TS("dve", d["L3"][:, 1, qh * 4:(qh + 1) * 4, :, gp * 16:(gp + 1) * 16], v4(c1), cs("mB%d" % gp), ALU.mult, [c1, "cst"], ["s5m"],
                   s2=-1.0, op1=ALU.mult)
            free(c1); free(c2)
        ACT(d["r8"][:, :], Bc(2), AF.Exp, [b_], [("r8", l)], scale=8.0)
        TT("dve", d["rtab"][:, :].rearrange("p (q c) -> p q c", c=64), bc(d["r8"][:, :].unsqueeze(2), [128, 8, 64]),
           cs("segm").rearrange("p (q c) -> p q c", c=64), ALU.mult, [("r8", l), "cst"], [("rtab", l)])
        TS("dve", Bc(4), Bc(3), 8.0, ALU.mult, [b_], [b_], s2=64.0, op1=ALU.add)
        CP("dve", sb_int[:, 0:8], Bc(4), [b_], ["sb_int"])
        CP("dve", Bc(5), sb_int[:, 0:8], ["sb_int"], [b_])
        TT("dve", Bc(4), Bc(4), Bc(5), ALU.subtract, [b_], [b_])
        TS("dve", Bc(5), Bc(4), 0.5, ALU.is_gt, [b_], [b_])
        TT("dve", Bc(4), Bc(4), Bc(5), ALU.subtract, [b_], [b_])
        TS("dve", Bc(5), Bc(4), -0.5, ALU.is_lt, [b_], [b_])
        TT("dve", Bc(4), Bc(4), Bc(5), ALU.add, [b_], [b_])
        TT("dve", u.v3(64), bc(Bc(4).unsqueeze(2), [128, 8, 64]), bc(cs("cidx").unsqueeze(1), [128, 8, 64]), ALU.mult, [b_, "cst"], [u])
        trig(u[:, :], 512, d["cosR"][:, :], d["sinR"][:, :], u, ("cosR", l), ("sinR", l))
        free(u); free(me); free(cc); free(ss); free(b_)

    def prologue_taps(l):
        d = L[l]
        if "KL" in tap_out:
            tf = falloc(4)
            CP("dve", tf[:, 0:2048], s5m[:, 8192:10240], ["s5m"], [tf])
            TAP("KL", l, tf[:, 0:2048], [tf]); free(tf)
        if "L1" in tap_out:
            tf = falloc(8)
            CP("dve", tf[:, 0:4096], s5m[:, 0:4096], ["s5m"], [tf])
            TAP("L1", l, tf[:, 0:4096], [tf]); free(tf)
        if "L3" in tap_out:
            tf = falloc(8)
            CP("dve", tf[:, 0:4096], s5m[:, 4096:8192], ["s5m"], [tf])
            TAP("L3", l, tf[:, 0:4096], [tf]); free(tf)
        if "rot" in tap_out:
            TAP("rot", (l, 0), d["cosR"][:, :], [("cosR", l)])
            TAP("rot", (l, 1), d["sinR"][:, :], [("sinR", l)])
            TAP("rot", (l, 2), d["rtab"][:, :], [("rtab", l)])

    for l in layers:
        prologue_layer(l)
        prologue_taps(l)
        P.dma("sp", s5m_d[l], s5m[:, :], reads=["s5m"], writes=[("s5m_d", l)], chan="s5m_st")
    free(prm_sb)

    tile_list = [(s, ti, l) for s in range(n_seq) for ti in range(n_tiles) for l in layers]
    chunk_seq = [(l, c) for (s, ti, l) in tile_list for c in range(NCHUNK)]
    ring_state = dict(issued=0, used=0)

    def issue_next():
        n = ring_state["issued"]
        if n >= len(chunk_seq):
            return
        l, c = chunk_seq[n]
        slot = n % NRING
        ci = l * NCHUNK + c
        P.dma("sp", ring[slot][:, :, :].rearrange("p k n -> p (k n)"), wsc[ci], reads=[("wsc", ci)], writes=[("ring", slot)], chan=("ring", slot))
        ring_state["issued"] += 1

    def get_chunk(l, c):
        n = ring_state["used"]
        assert chunk_seq[n] == (l, c), (chunk_seq[n], l, c)
        while ring_state["issued"] <= n:
            issue_next()
        return n % NRING

    def release_chunk():
        ring_state["used"] += 1
        while ring_state["issued"] < min(len(chunk_seq), ring_state["used"] + NRING):
            issue_next()

    for _ in range(NRING):
        issue_next()

    def rmsnorm_to_h(l, gcol):
        sp = L[l]["sp"]
        for k in range(8):
            sq = balloc()
            ACT(sq[:, :], xs[:, k, :], AF.Square, [("xs", k)], [sq])
            MM([mm(banks[7][:, :], ones_bf[:, :], sq[:, :], start=(k == 0), stop=(k == 7))], [sq, "ones_bf"], [BK[7]])
            free(sq)
        rs = falloc()
        ACT(rs[:, :], banks[7][:, :], AF.Sqrt, [BK[7]], [rs], scale=1.0 / DM, bias=EPS)
        RECIP(rs[:, :], rs[:, :], [rs], [rs])
        for k in range(8):
            STT(hbuf[:, k, :], xs[:, k, :], sp[:, gcol + k:gcol + k + 1], rs[:, :], ALU.mult, ALU.mult, [("xs", k), ("sp", l), rs], [("h", k)])
        free(rs)

    HK = [("h", k) for k in range(8)]

    def proj_fm(slot, m, bank):
        MM([mm(banks[bank][:, :], ring[slot][:, k, m * 128:(m + 1) * 128], hbuf[:, k, :], start=(k == 0), stop=(k == 7)) for k in range(8)],
           [("ring", slot)] + HK, [BK[bank]])

    def layer_tile(s, ti, l):
        d = L[l]
        sp = d["sp"]
        SPK = ("sp", l)
        first = (ti == 0)
        t0 = ti * NT
        if l == layers[0]:
            for k in range(8):
                P.dma("act", xs[:, k, :], xT[s, k * 128:(k + 1) * 128, t0:t0 + NT], writes=[("xs", k)], chan=("xs", k))
        if first:
            P.op("pool", lambda e: e.memset(d["zbuf"][:, :, 0:2], 0.0), writes=[("zbuf", l)])
            P.op("pool", lambda e: e.memset(d["Er"][:, :, 0:1], 0.0), writes=[("Er", l)])
            P.op("pool", lambda e: e.memset(d["Ei"][:, :, 0:1], 0.0), writes=[("Ei", l)])
            for j in range(2):
                P.op("pool", lambda e, j=j: e.memset(d["S"][j][:, :], 0.0), writes=[("S", l, j)])
        P.dma("act", s5m[:, :], s5m_d[l], reads=[("s5m_d", l)], writes=["s5m"], chan="s5m_ld")
        rmsnorm_to_h(l, SPC["gmix"])
        CHK("norm1")
        Y = [falloc() for _ in range(8)]

        slot = get_chunk(l, 0)
        for m in range(4):
            proj_fm(slot, m, m)
        release_chunk()
        hsb = [falloc() for _ in range(2)]
        bsb = [falloc() for _ in range(2)]
        for j in range(2):
            CP("act", hsb[j][:, :], banks[j][:, :], [BK[j]], [hsb[j]])
            CP("act", bsb[j][:, :], banks[2 + j][:, :], [BK[2 + j]], [bsb[j]])
        slot = get_chunk(l, 1)
        for m in range(4):
            proj_fm(slot, m, 4 + m)
        release_chunk()
        ubf = balloc(2)
        for j in range(2):
            CP("act", ubf[:, j * NT:(j + 1) * NT].rearrange("p (t c) -> p t c", c=NC5), banks[6 + j][:, :].rearrange("p (c t) -> p t c", t=TS5),
               [BK[6 + j]], [ubf])
        for j in range(2):
            zb = d["zbuf"]
            TT("dve", zb[:, j, 2:NT + 2], banks[4 + j][:, :], hsb[j][:, :], ALU.mult, [BK[4 + j], hsb[j]], [("zbuf", l)])
            acc = falloc()
            cw = lambda i: sp[:, SPC["convw"] + j * 3 + i:SPC["convw"] + j * 3 + i + 1]
            TS("pool", acc[:, :], zb[:, j, 2:NT + 2], cw(2), ALU.mult, [("zbuf", l), SPK], [acc])
            STT(acc[:, :], zb[:, j, 1:NT + 1], cw(1), acc[:, :], ALU.mult, ALU.add, [("zbuf", l), SPK, acc], [acc])
            STT(acc[:, :], zb[:, j, 0:NT], cw(0), acc[:, :], ALU.mult, ALU.add, [("zbuf", l), SPK, acc], [acc])
            TT("pool", Y[j][:, :], acc[:, :], bsb[j][:, :], ALU.mult, [acc, bsb[j]], [Y[j]])
            free(acc)
        P.op("pool", lambda e: e.tensor_copy(out=d["zbuf"][:, :, 0:2], in_=d["zbuf"][:, :, NT:NT + 2]), [("zbuf", l)], [("zbuf", l)])
        for b_ in hsb + bsb:
            free(b_)

        CHK("conv")
        ut = [ubf[:, j * NT:(j + 1) * NT] for j in range(2)]
        for ri in range(2):
            for qq in range(4):
                mms = []
                for j in range(2):
                    q = j * 4 + qq
                    for s_ in range(TS5):
                        mms.append(mm(banks[ri][:, q * NC5:(q + 1) * NC5], d["L1"][qq * 32:(qq + 1) * 32, j, ri, TS5 - 1 - s_, :],
                                      ut[j][qq * 32:(qq + 1) * 32, s_ * NC5:(s_ + 1) * NC5], start=(s_ == 0), stop=(s_ == TS5 - 1), tp=(qq * 32, 0)))
                MM(mms, [ubf, "s5m"], [BK[ri]], force=True)
        CHK("s5p1")
        Wr = falloc(); Wi = falloc(); t1 = falloc(); t2 = falloc()
        cosR, sinR, rtab = d["cosR"], d["sinR"], d["rtab"]
        CK, SK, RK = ("cosR", l), ("sinR", l), ("rtab", l)
        TT("dve", t1[:, :], banks[0][:, :], cosR[:, :], ALU.mult, [BK[0], CK], [t1])
        TT("dve", t2[:, :], banks[1][:, :], sinR[:, :], ALU.mult, [BK[1], SK], [t2])
        TT("pool", Wr[:, :], t1[:, :], t2[:, :], ALU.add, [t1, t2], [Wr])
        TT("dve", t1[:, :], banks[1][:, :], cosR[:, :], ALU.mult, [BK[1], CK], [t1])
        TT("dve", t2[:, :], banks[0][:, :], sinR[:, :], ALU.mult, [BK[0], SK], [t2])
        TT("pool", Wi[:, :], t1[:, :], t2[:, :], ALU.subtract, [t1, t2], [Wi])
        for (Wx, Ex, ek) in ((Wr, d["Er"], ("Er", l)), (Wi, d["Ei"], ("Ei", l))):
            TT("dve", t1[:, 0:8], d["r8"][:, :], Ex[:, :, 0], ALU.mult, [("r8", l), ek], [t1])
            TT("dve", Wx.v3(NC5)[:, :, 0], Wx.v3(NC5)[:, :, 0], t1[:, 0:8], ALU.add, [Wx, t1], [Wx])
        Fr = falloc(); Fi = falloc()
        for (Wx, Fx) in ((Wr, Fr), (Wi, Fi)):
            P.op("dve", lambda e, Wx=Wx, Fx=Fx: e.tensor_tensor_scan(out=Fx[:, :], data0=rtab[:, :], data1=Wx[:, :], initial=0.0,
                                                                    op0=ALU.mult, op1=ALU.add), [RK, Wx], [Fx])
        TT("dve", t1[:, :], Fr[:, :], cosR[:, :], ALU.mult, [Fr, CK], [t1])
        TT("dve", t2[:, :], Fi[:, :], sinR[:, :], ALU.mult, [Fi, SK], [t2])
        TT("pool", d["Er"][:, :, 1:NC5 + 1], t1.v3(NC5), t2.v3(NC5), ALU.subtract, [t1, t2], [("Er", l)])
        TT("dve", t1[:, :], Fi[:, :], cosR[:, :], ALU.mult, [Fi, CK], [t1])
        TT("dve", t2[:, :], Fr[:, :], sinR[:, :], ALU.mult, [Fr, SK], [t2])
        TT("pool", d["Ei"][:, :, 1:NC5 + 1], t1.v3(NC5), t2.v3(NC5), ALU.add, [t1, t2], [("Ei", l)])
        for b_ in (Wr, Wi, t1, t2, Fr, Fi):
            free(b_)
        Ebf = balloc(2)
        CP("act", Ebf[:, 0:NT].rearrange("p (q c) -> p q c", c=NC5), d["Er"][:, :, 0:NC5], [("Er", l)], [Ebf])
        CP("act", Ebf[:, NT:2 * NT].rearrange("p (q c) -> p q c", c=NC5), d["Ei"][:, :, 0:NC5], [("Ei", l)], [Ebf])
        P.op("pool", lambda e: e.tensor_copy(out=d["Er"][:, :, 0:1], in_=d["Er"][:, :, NC5:NC5 + 1]), [("Er", l)], [("Er", l)])
        P.op("pool", lambda e: e.tensor_copy(out=d["Ei"][:, :, 0:1], in_=d["Ei"][:, :, NC5:NC5 + 1]), [("Ei", l)], [("Ei", l)])
        CHK("s5p2")
        E4 = [Ebf[:, ri * NT:(ri + 1) * NT].rearrange("p (q c) -> p q c", c=NC5) for ri in range(2)]
        for j in range(2):
            yb = banks[2 + j]
            mms = []
            for tau in range(TS5):
                mms.append(mm(yb[:, tau * NC5:NT], d["KL"][:, j, tau, :], ut[j][:, 0:(TS5 - tau) * NC5], start=(tau == 0), stop=False))
            for qq in range(4):
                q = j * 4 + qq
                for t_ in range(TS5):
                    for ri in range(2):
                        mms.append(mm(yb[qq * 32:(qq + 1) * 32, t_ * NC5:(t_ + 1) * NC5], d["L3"][:, ri, q, t_, :], E4[ri][:, q, :], start=False,
                                      stop=(qq == 3 and t_ == TS5 - 1 and ri == 1), tp=(0, qq * 32)))
            MM(mms, [ubf, Ebf, "s5m", "s5m"], [BK[2 + j]])
        gl = balloc(2)
        for j in range(2):
            ysb = falloc(); sq = falloc()
            ynat = banks[2 + j][:, :].rearrange("p (t c) -> p c t", c=NC5)
            CP("act", ysb.v3(TS5), ynat, [BK[2 + j]], [ysb])
            ACT(sq.v3(TS5), ynat, AF.Square, [BK[2 + j]], [sq])
            TS("dve", sq[:, :], sq[:, :], 0.044715, ALU.mult, [sq], [sq], s2=1.0, op1=ALU.add)
            TT("dve", sq[:, :], sq[:, :], ysb[:, :], ALU.mult, [sq, ysb], [sq])
            ACT(sq[:, :], sq[:, :], AF.Sigmoid, [sq], [sq], scale=1.5957691216057308)
            TT("pool", gl[:, j * NT:(j + 1) * NT], ysb[:, :], sq[:, :], ALU.mult, [ysb, sq], [gl])
            free(ysb); free(sq)
        free(ubf); free(Ebf)
        for n in range(4):
            MM([mm(banks[4 + n][:, :], d["glu"][:, k, n * 128:(n + 1) * 128], gl[:, k * NT:(k + 1) * NT], start=(k == 0), stop=(k == 1)) for k in range(2)],
               [gl, ("glu", l)], [BK[4 + n]])
        free(gl)
        for j in range(2):
            sg = falloc()
            ACT(sg[:, :], banks[6 + j][:, :], AF.Sigmoid, [BK[6 + j]], [sg])
            TT("dve", Y[2 + j][:, :], banks[4 + j][:, :], sg[:, :], ALU.mult, [BK[4 + j], sg], [Y[2 + j]])
            free(sg)

        CHK("s5")
        slot = get_chunk(l, 2)
        for m in range(3):
            proj_fm(slot, m, m)
        MM([mm(banks[3][:, blk * 128:(blk + 1) * 128], hbuf[:, k, blk * 128:(blk + 1) * 128], ring[slot][:, k, 384:512], start=(k == 0), stop=(k == 7))
            for blk in range(4) for k in range(8)], [("ring", slot)] + HK, [BK[3]])
        release_chunk()
        kbuf, vbuf = d["kbuf"], d["vbuf"]
        KB, VB = ("kbuf", l), ("vbuf", l)
        qn = balloc(2)
        for (bk, gcol, outv, okey, gkey) in ((0, SPC["qgs"], qn[:, 0:NT], qn, SPK), (1, SPC["qgs"], qn[:, NT:2 * NT], qn, SPK),
                                             (2, SPC["kg"], kbuf[:, 128:128 + NT], KB, SPK)):
            sq = balloc(); rs = falloc()
            ACT(sq[:, :], banks[bk][:, :], AF.Square, [BK[bk]], [sq])
            MM([mm(banks[4][:, :], bones_bf[:, :], sq[:, :])], [sq, "bones_bf"], [BK[4]])
            ACT(rs[:, :], banks[4][:, :], AF.Sqrt, [BK[4]], [rs], scale=1.0 / 64, bias=EPS)
            RECIP(rs[:, :], rs[:, :], [rs], [rs])
            STT(outv, banks[bk][:, :], sp[:, gcol:gcol + 1], rs[:, :], ALU.mult, ALU.mult, [BK[bk], SPK, rs], [okey])
            free(sq); free(rs)
        CP("act", vbuf[:, 1:5, :], banks[3][:, :].rearrange("p (b f) -> p b f", f=128), [BK[3]], [VB])
        jb_list = list(range(1 if first else 0, 5))
        for jbi, jb in enumerate(jb_list):
            sbk = (4, 5) if jbi % 2 == 0 else (6, 7)
            qlo, qhi = max(0, 2 * jb - 2), min(8, 2 * jb + 2)
            off = (qlo - (2 * jb - 2)) * 64
            ncol = (qhi - qlo) * 64
            for kh in range(2):
                MM([mm(banks[sbk[kh]][:, r * 256 + off:r * 256 + off + ncol], kbuf[kh * 64:(kh + 1) * 64, jb * 128:(jb + 1) * 128],
                       qn[kh * 64:(kh + 1) * 64, r * NT + qlo * 64:r * NT + qhi * 64]) for r in range(2)], [KB, qn], [BK[sbk[kh]]])
            pT = balloc(2)
            if True:
                P.op("pool", lambda e, pT=pT: e.memset(pT[:, :], 0.0), writes=[pT])
            for kh in range(2):
                for half, (c0, c1) in enumerate(((0, 192), (64, 256))):
                    a, b2 = max(c0, off), min(c1, off + ncol)
                    if b2 <= a:
                        continue
                    src = banks[sbk[kh]][half * 64:(half + 1) * 64, :].rearrange("p (r c) -> p r c", c=256)[:, :, a:b2]
                    dst = pT[half * 64:(half + 1) * 64, kh * 512:(kh + 1) * 512].rearrange("p (r c) -> p r c", c=256)[:, :, a:b2]
                    ACT(dst, src, AF.Exp, [BK[sbk[kh]]], [pT])
            for r in range(2):
                mms_n, mms_d = [], []
                for kh in range(2):
                    pv = pT[:, kh * 512 + r * 256:kh * 512 + (r + 1) * 256]
                    for part in range(2):
                        pair = jb - 1 + part
                        if pair < 0 or pair > 3:
                            continue
                        first_contrib = (part == 1) or (first and jb == 1)
                        last_contrib = (part == 0) or (jb == 4)
                        if part == 1 and jb == 4:
                            continue
                        cols = slice(pair * 128, (pair + 1) * 128)
                        mms_n.append(mm(banks[r][kh * 64:(kh + 1) * 64, cols], vbuf[:, jb, kh * 64:(kh + 1) * 64], pv[:, part * 128:(part + 1) * 128],
                                        start=first_contrib, stop=last_contrib))
                        mms_d.append(mm(banks[2 + r][kh * 64:(kh + 1) * 64, cols], ones_bf[:, 0:64], pv[:, part * 128:(part + 1) * 128],
                                        start=first_contrib, stop=last_contrib))
                MM(mms_n, [pT, VB], [BK[r]])
                MM(mms_d, [pT, "ones_bf"], [BK[2 + r]])
            free(pT)
        for r in range(2):
            rec = falloc()
            TS("dve", rec[:, :], banks[2 + r][:, :], sp[:, SPC["esink"] + r:SPC["esink"] + r + 1], ALU.add, [BK[2 + r], SPK], [rec])
            RECIP(rec[:, :], rec[:, :], [rec], [rec])
            TT("dve", Y[4 + r][:, :], banks[r][:, :], rec[:, :], ALU.mult, [BK[r], rec], [Y[4 + r]])
            free(rec)
        free(qn)
        P.op("pool", lambda e: e.tensor_copy(out=kbuf[:, 0:128], in_=kbuf[:, NT:NT + 128]), [KB], [KB])
        P.op("pool", lambda e: e.tensor_copy(out=vbuf[:, 0, :], in_=vbuf[:, 4, :]), [VB], [VB])

        CHK("attn")
        slot = get_chunk(l, 3)
        for m in range(4):
            proj_fm(slot, m, 4 + m)
        release_chunk()
        qt = balloc(2); qh = balloc(4); kt = balloc(2); kend = balloc(2)
        e3s = []
        for j in range(2):
            sig = falloc(); lg = falloc(); kk = falloc(); B = falloc(); e1 = falloc(); e2 = falloc(); e3 = falloc()
            ACT(sig[:, :], banks[6 + j][:, :], AF.Sigmoid, [BK[6 + j]], [sig])
            TS("dve", sig[:, :], sig[:, :], sp[:, SPC["oml"] + j:SPC["oml"] + j + 1], ALU.mult, [sig, SPK], [sig],
               s2=sp[:, SPC["lb"] + j:SPC["lb"] + j + 1], op1=ALU.add)
            ACT(lg[:, :], sig[:, :], AF.Ln, [sig], [lg])
            TS("pool", kk[:, :], sig[:, :], -1.0, ALU.mult, [sig], [kk], s2=1.0, op1=ALU.add)
            P.op("dve", lambda e, B=B, lg=lg: e.tensor_tensor_scan(out=B[:, :], data0=cs("segm"), data1=lg[:, :], initial=0.0,
                                                                  op0=ALU.mult, op1=ALU.add), ["cst", lg], [B])
            ACT(e3[:, :], B[:, :], AF.Exp, [B], [e3])
            TT("dve", lg.v3(64), B.v3(64), bc(B.v3(64)[:, :, 31:32], [128, 8, 64]), ALU.subtract, [B], [lg])
            ACT(e1[:, :], lg[:, :], AF.Exp, [lg], [e1])
            ACT(e2[:, :], lg[:, :], AF.Exp, [lg], [e2], scale=-1.0)
            TT("dve", qt[:, j * NT:(j + 1) * NT], banks[4 + j][:, :], e1[:, :], ALU.mult, [BK[4 + j], e1], [qt])
            for hh in range(2):
                STT(qh[:, (j * 2 + hh) * NT:(j * 2 + hh + 1) * NT], banks[4 + j][:, :], cs("mB%d" % hh), e3[:, :], ALU.mult, ALU.mult,
                    [BK[4 + j], e3, "cst"], [qh])
            TT("pool", kt[:, j * NT:(j + 1) * NT], kk[:, :], e2[:, :], ALU.mult, [kk, e2], [kt])
            TT("pool", kend[:, j * NT:(j + 1) * NT].rearrange("p (b t) -> p b t", t=64), kt[:, j * NT:(j + 1) * NT].rearrange("p (b t) -> p b t", t=64),
               bc(e1.v3(64)[:, :, 63:64], [128, 8, 64]), ALU.mult, [kt, e1], [kend])
            e3s.append(e3)
            for b_ in (sig, lg, kk, B, e1, e2):
                free(b_)
        b4bf = banks[4][:, :].bitcast(BF16)
        def tr_fn(e):
            ins = None
            for bp in range(4):
                for j in range(2):
                    ins = e.transpose(out=b4bf[:, (bp * 2 + j) * 128:(bp * 2 + j + 1) * 128], in_=kend[:, j * NT + bp * 128:j * NT + (bp + 1) * 128],
                                      identity=ident_bf[:, :])
            return ins
        P.op("pe", tr_fn, [kend, "ident_bf"], [BK[4]])
        kendT = balloc(2)
        CP("act", kendT[:, :], b4bf, [BK[4]], [kendT])
        free(kend)
        slot = get_chunk(l, 4)
        for half in range(2):
            MM([mm(banks[5 + half][:, bq * 256:(bq + 1) * 256], hbuf[:, k, (half * 2 + bq) * 128:(half * 2 + bq + 1) * 128], ring[slot][:, k, 0:256],
                   start=(k == 0), stop=(k == 7)) for bq in range(2) for k in range(8)], [("ring", slot)] + HK, [BK[5 + half]])
        for j in range(2):
            proj_fm(slot, 2 + j, j)
        release_chunk()
        vT = balloc(2)
        for half in range(2):
            CP("act", vT[:, half * NT:(half + 1) * NT], banks[5 + half][:, :], [BK[5 + half]], [vT])
        sgs = []
        for j in range(2):
            sg = falloc()
            ACT(sg[:, :], banks[j][:, :], AF.Silu, [BK[j]], [sg])
            sgs.append(sg)
        for hp in range(2):
            mms = []
            for j in range(2):
                for b in range(8):
                    bp, half = b // 2, b % 2
                    mms.append(mm(banks[2 + hp][half * 64:(half + 1) * 64, j * 256 + bp * 64:j * 256 + (bp + 1) * 64],
                                  kt[hp * 64:(hp + 1) * 64, j * NT + b * 64:j * NT + (b + 1) * 64],
                                  qt[hp * 64:(hp + 1) * 64, j * NT + b * 64:j * NT + (b + 1) * 64]))
            MM(mms, [kt, qt], [BK[2 + hp]])
        for hp in range(2):
            for half in range(2):
                rows = slice(half * 64, (half + 1) * 64)
                dst = Amz[rows, :].rearrange("p (j hp bp hf t) -> p j hp bp hf t", j=2, hp=2, bp=4, hf=2, t=64)[:, :, hp, :, half, :]
                src = banks[2 + hp][rows, :].rearrange("p (j bp t) -> p j bp t", j=2, bp=4)
                o_c, _w = _CST_OFF["cmask"]
                msk = bc(cst[rows, o_c:o_c + 64].unsqueeze(1).unsqueeze(1), [64, 2, 4, 64])
                TT("dve", dst, src, msk, ALU.mult, [BK[2 + hp], "cst"], ["Amz"])
        free(kt); free(qt)
        for half in range(2):
            mms = []
            for j in range(2):
                for hh in range(2):
                    h = 2 * j + hh
                    for bp in range(4):
                        mms.append(mm(banks[6 + half][hh * 64:(hh + 1) * 64, j * 256 + bp * 64:j * 256 + (bp + 1) * 64],
                                      kendT[half * 64:(half + 1) * 64, bp * 256 + h * 64:bp * 256 + (h + 1) * 64],
                                      vT[half * 64:(half + 1) * 64, bp * 256 + h * 64:bp * 256 + (h + 1) * 64]))
            MM(mms, [kendT, vT], [BK[6 + half]])
        free(kendT)
        Sall = balloc(3)
        for j in range(2):
            Sj = d["S"][j]
            SKj = ("S", l, j)
            CP("act", Sall[:, j * 768:j * 768 + 64], Sj[:, :], [SKj], [Sall])
            for b in range(8):
                bp, half = b // 2, b % 2
                STT(Sj[:, :], Sj[:, :], e3s[j][:, b * 64 + 63:b * 64 + 64], banks[6 + half][:, j * 256 + bp * 64:j * 256 + (bp + 1) * 64], ALU.mult, ALU.add,
                    [SKj, e3s[j], BK[6 + half]], [SKj])
                if b < 7:
                    CP("act", Sall[:, j * 768 + (b + 1) * 64:j * 768 + (b + 2) * 64], Sj[:, :], [SKj], [Sall])
        for e3 in e3s:
            free(e3)
        for j in range(2):
            mms = []
            for hh in range(2):
                h = 2 * j + hh
                for b in range(8):
                    bp = b // 2
                    o_ = banks[4 + j][hh * 64:(hh + 1) * 64, b * 64:(b + 1) * 64]
                    mms.append(mm(o_, Sall[:, j * 768 + b * 64:j * 768 + (b + 1) * 64],
                                  qh[:, (j * 2 + hh) * NT + b * 64:(j * 2 + hh) * NT + (b + 1) * 64], start=True, stop=False))
                    mms.append(mm(o_, vT[:, bp * 256 + h * 64:bp * 256 + (h + 1) * 64],
                                  Amz[:, (h * 8 + b) * 64:(h * 8 + b + 1) * 64], start=False, stop=True))
            MM(mms, [Sall, qh, vT, "Amz"], [BK[4 + j]])
        free(Sall); free(qh); free(vT)
        for j in range(2):
            sq = balloc(); rs = falloc()
            ACT(sq[:, :], banks[4 + j][:, :], AF.Square, [BK[4 + j]], [sq])
            MM([mm(banks[2][:, :], bones_bf[:, :], sq[:, :])], [sq, "bones_bf"], [BK[2]])
            ACT(rs[:, :], banks[2][:, :], AF.Sqrt, [BK[2]], [rs], scale=1.0 / 64, bias=EPS)
            RECIP(rs[:, :], rs[:, :], [rs], [rs])
            STT(rs[:, :], banks[4 + j][:, :], sp[:, SPC["og"]:SPC["og"] + 1], rs[:, :], ALU.mult, ALU.mult, [BK[4 + j], SPK, rs], [rs])
            TT("pool", Y[6 + j][:, :], rs[:, :], sgs[j][:, :], ALU.mult, [rs, sgs[j]], [Y[6 + j]])
            free(sq); free(rs); free(sgs[j])

        if "Y" in tap_out and (s, ti) == taps.get("_ysel_tile", (0, 0)):
            for i in range(8):
                TAP("Y", (l, i), Y[i][:, :], [Y[i]])

        CHK("hgrn")
        for gI in range(4):
            for i in range(2):
                sq = balloc()
                ACT(sq[:, :], Y[2 * gI + i][:, :], AF.Square, [Y[2 * gI + i]], [sq])
                MM([mm(banks[7][:, :], ones_bf[:, :], sq[:, :], start=(i == 0), stop=(i == 1))], [sq, "ones_bf"], [BK[7]])
                free(sq)
            rs = falloc()
            ACT(rs[:, :], banks[7][:, :], AF.Sqrt, [BK[7]], [rs], scale=1.0 / 256, bias=EPS)
            RECIP(rs[:, :], rs[:, :], [rs], [rs])
            for i in range(2):
                k = 2 * gI + i
                STT(hbuf[:, k, :], Y[k][:, :], sp[:, SPC["ggn"] + k:SPC["ggn"] + k + 1], rs[:, :], ALU.mult, ALU.mult, [Y[k], SPK, rs], [("h", k)])
            free(rs)
        for y_ in Y:
            free(y_)
        for c in range(2):
            slot = get_chunk(l, 5 + c)
            for m in range(4):
                proj_fm(slot, m, m)
            release_chunk()
            for m in range(4):
                k = c * 4 + m
                TT("dve", xs[:, k, :], xs[:, k, :], banks[m][:, :], ALU.add, [("xs", k), BK[m]], [("xs", k)])
        if "xmid" in tap_out and (s, ti) == taps.get("_ysel_tile", (0, 0)):
            for k in range(8):
                TAP("xmid", (l, k), xs[:, k, :], [("xs", k)])

        CHK("gn")
        rmsnorm_to_h(l, SPC["gffn"])
        hid = balloc(32)
        HIDK = hid.keys_
        for c in range(8):
            slot = get_chunk(l, 7 + c)
            for m in range(4):
                bk = (c * 4 + m) % 4
                proj_fm(slot, m, bk)
                r_ = falloc()
                ACT(r_[:, :], banks[bk][:, :], AF.Relu, [BK[bk]], [r_])
                idx = c * 4 + m
                TT("pool", hid[:, idx * NT:(idx + 1) * NT], r_[:, :], r_[:, :], ALU.mult, [r_], [HIDK[idx]])
                free(r_)
            release_chunk()
        for cg in range(2):
            for kg in range(4):
                slot = get_chunk(l, 15 + cg * 4 + kg)
                for m in range(4):
                    MM([mm(banks[4 + m][:, :], ring[slot][:, k, m * 128:(m + 1) * 128], hid[:, (kg * 8 + k) * NT:(kg * 8 + k + 1) * NT],
                           start=(kg == 0 and k == 0), stop=(kg == 3 and k == 7)) for k in range(8)],
                       [("ring", slot)] + HIDK[kg * 8:(kg + 1) * 8], [BK[4 + m]])
                release_chunk()
            for m in range(4):
                k = cg * 4 + m
                TT("dve", xs[:, k, :], xs[:, k, :], banks[4 + m][:, :], ALU.add, [("xs", k), BK[4 + m]], [("xs", k)])
        free(hid)
        if "xout" in tap_out and (s, ti) == taps.get("_ysel_tile", (0, 0)):
            for k in range(8):
                TAP("xout", (l, k), xs[:, k, :], [("xs", k)])
        if l == layers[-1]:
            outs = []
            for k in range(8):
                outs.append(P.dma("sp", oT[s, k * 128:(k + 1) * 128, t0:t0 + NT], xs[:, k, :], reads=[("xs", k)], writes=[("oT", s, ti, k)], chan=("out", k)))
            return outs
        return []

    all_out = []
    try:
        CHK("prologue")
        for (s, ti, l) in tile_list:
            all_out += layer_tile(s, ti, l)
    except StopBuild:
        pass
    P.emit(final_deps=all_out + tap_dmas)
    return nc, P


_CACHE = {}


def prepare_inputs(inputs):
    wch, glu = _build_weights(inputs)
    prm = np.stack([_build_params(inputs, l) for l in range(DEPTH)], axis=0)
    cst = _build_consts()
    x = np.asarray(inputs["x"])
    in_maps = []
    for c in range(NCORES):
        xc = x[c * SEQ_PER_CORE:(c + 1) * SEQ_PER_CORE]
        xT = np.ascontiguousarray(xc.transpose(0, 2, 1))
        in_maps.append({"xT": xT, "wch": wch, "glu": glu, "prm": prm, "cst": cst})
    return in_maps


def kernel(**inputs):
    in_maps = prepare_inputs(inputs)
    if "nc" not in _CACHE:
        _CACHE["nc"] = build_program()[0]
    res = run_bass_kernel_spmd(_CACHE["nc"], in_maps, core_ids=list(range(NCORES)))
    outs = []
    for c in range(NCORES):
        oT = res.results[c]["oT"]
        outs.append(np.ascontiguousarray(oT.transpose(0, 2, 1)))
    return np.concatenate(outs, axis=0).astype(np.float32)
```

```python
import contextlib
import numpy as np
import concourse.bass as bass
import concourse.mybir as mybir
from concourse.bass_utils import run_bass_kernel_spmd

F32 = mybir.dt.float32
BF16 = mybir.dt.bfloat16
I32 = mybir.dt.int32
ALU = mybir.AluOpType
AF = mybir.ActivationFunctionType
AX = mybir.AxisListType

NCORES = 8
SEQ_PER_CORE = 4
SEQ = 2048
NT = 512
TILES_PER_SEQ = SEQ // NT
DM = 1024
DEPTH = 2
NCHUNK = 23
EPS = 1e-6
TS5 = 8
NC5 = NT // TS5
TWO_PI = 6.283185307179586

ENGS = ("pe", "act", "dve", "pool", "sp")
SEM_CAP = 1000


class Op:
    __slots__ = ("eng", "fn", "deps", "is_dma", "chan", "needs_inc", "sem", "val")

    def __init__(self, eng, fn, is_dma=False, chan=None):
        self.eng = eng
        self.fn = fn
        self.deps = []
        self.is_dma = is_dma
        self.chan = chan
        self.needs_inc = is_dma
        self.sem = None
        self.val = None


def _keys(items):
    out = []
    for it in items:
        if hasattr(it, "keys_"):
            out.extend(it.keys_)
        else:
            out.append(it)
    return out


class Prog:
    def __init__(self, nc, same_engine_sync=True):
        self.nc = nc
        self.ops = {e: [] for e in ENGS}
        self.last_writer = {}
        self.readers = {}
        self.same_engine_sync = same_engine_sync

    def op(self, eng, fn, reads=(), writes=(), is_dma=False, chan=None, force=False):
        reads = _keys(reads)
        writes = _keys(writes)
        o = Op(eng, fn, is_dma, chan)
        deps = {}
        for k in reads:
            w = self.last_writer.get(k)
            if w is not None:
                deps[id(w)] = w
        for k in writes:
            w = self.last_writer.get(k)
            if w is not None:
                deps[id(w)] = w
            for r in self.readers.get(k, ()):
                deps[id(r)] = r
        for d in deps.values():
            if (not d.is_dma) and d.eng == eng and ((eng == "pe" and not force) or not self.same_engine_sync):
                continue
            d.needs_inc = True
            o.deps.append(d)
        for k in reads:
            self.readers.setdefault(k, []).append(o)
        for k in writes:
            self.last_writer[k] = o
            self.readers[k] = []
        self.ops[eng].append(o)
        return o

    def dma(self, eng, out, in_, reads=(), writes=(), chan=None, **kw):
        return self.op(eng, lambda e: e.dma_start(out=out, in_=in_, **kw), reads, list(writes) + [("chan", chan)], is_dma=True, chan=chan)

    def emit(self, final_deps=()):
        nc = self.nc
        stack = contextlib.ExitStack()
        eng_sems = {e: [] for e in ENGS}
        eng_cnt = {e: 0 for e in ENGS}
        chan_sem, chan_cnt = {}, {}
        nsem = 0
        for e in ENGS:
            for o in self.ops[e]:
                if o.is_dma:
                    if o.chan not in chan_sem or chan_cnt[o.chan] + 16 > SEM_CAP:
                        chan_sem[o.chan] = stack.enter_context(nc.semaphore(f"c{nsem}"))
                        nsem += 1
                        chan_cnt[o.chan] = 0
                    chan_cnt[o.chan] += 16
                    o.sem, o.val = chan_sem[o.chan], chan_cnt[o.chan]
                elif o.needs_inc:
                    if eng_cnt[e] % SEM_CAP == 0:
                        eng_sems[e].append(stack.enter_context(nc.semaphore(f"e{nsem}")))
                        nsem += 1
                    eng_cnt[e] += 1
                    o.sem, o.val = eng_sems[e][-1], (eng_cnt[e] - 1) % SEM_CAP + 1
        self.nsem = nsem
        final_deps = list(final_deps)

        def run_engine(ename, eng):
            waited = {}

            def wait_for(d):
                key = id(d.sem)
                if waited.get(key, 0) >= d.val:
                    return
                eng.wait_ge(d.sem, d.val)
                waited[key] = d.val

            for o in self.ops[ename]:
                for d in o.deps:
                    wait_for(d)
                ins = o.fn(eng)
                if o.needs_inc:
                    ins.then_inc(o.sem, 16 if o.is_dma else 1)
            if ename == "sp":
                for d in final_deps:
                    wait_for(d)

        with nc.Block() as block:
            @block.tensor
            def _(e):
                run_engine("pe", e)

            @block.scalar
            def _(e):
                run_engine("act", e)

            @block.vector
            def _(e):
                run_engine("dve", e)

            @block.gpsimd
            def _(e):
                run_engine("pool", e)

            @block.sync
            def _(e):
                run_engine("sp", e)
        stack.close()


def _col8(v):
    return np.ascontiguousarray(v.reshape(8, 128).T)


def _col2(v):
    return np.ascontiguousarray(v.reshape(2, 128).T)


def _qperm():
    idx = np.zeros(256, np.int64)
    for r in range(2):
        for kh in range(2):
            for d in range(64):
                idx[r * 128 + kh * 64 + d] = (kh * 2 + r) * 64 + d
    return idx


_PRM_FIELDS = [
    ("gmix", 8), ("gffn", 8), ("ggn", 8), ("convw", 6), ("s5d", 2), ("qg", 1), ("kg", 1), ("sink", 2),
    ("hlb0", 2), ("hlb1", 2), ("og", 1),
    ("A_lre", 128), ("A_lim", 128), ("A_ldt", 2), ("A_bre", 128), ("A_bim", 128),
    ("A_cre", 2048), ("A_cim", 2048),
    ("B_lre", 8), ("B_lim", 8), ("B_ldt", 8), ("B_cre", 128), ("B_cim", 128),
]
_PRM_OFF = {}
_o = 0
for _n, _w in _PRM_FIELDS:
    _PRM_OFF[_n] = (_o, _w)
    _o += _w
NPRM = _o

_CST_FIELDS = [
    ("ident", 128), ("bones", 128),
    ("mgp0", 1), ("mgp1", 1), ("mB0", 1), ("mB1", 1),
    ("mg8", 8), ("tauA", 8), ("tauB", 8), ("cidx", 64),
    ("segm", 512), ("cmask", 64),
]
_CST_OFF = {}
_o = 0
for _n, _w in _CST_FIELDS:
    _CST_OFF[_n] = (_o, _w)
    _o += _w
NCST = _o


def _build_consts():
    c = np.zeros((128, NCST), np.float32)

    def put(name, arr):
        o, w = _CST_OFF[name]
        c[:, o:o + w] = np.asarray(arr, np.float32).reshape(128, w)

    p = np.arange(128)
    put("ident", np.eye(128))
    bo = np.zeros((128, 128))
    bo[:64, :64] = 1
    bo[64:, 64:] = 1
    put("bones", bo)
    put("mgp0", ((p // 16) % 2 == 0)[:, None])
    put("mgp1", ((p // 16) % 2 == 1)[:, None])
    put("mB0", (p < 64)[:, None])
    put("mB1", (p >= 64)[:, None])
    put("mg8", (p[:, None] // 16) == np.arange(8)[None, :])
    put("tauA", np.tile(np.arange(8.0)[None], (128, 1)))
    put("tauB", np.tile(np.arange(1.0, 9.0)[None], (128, 1)))
    put("cidx", np.tile(np.arange(1.0, 65.0)[None], (128, 1)))
    seg = np.ones((8, 64))
    seg[:, 0] = 0
    put("segm", np.tile(seg.reshape(1, 512), (128, 1)))
    s = (p % 64)[:, None]
    t = np.arange(64)[None, :]
    put("cmask", (s <= t))
    return c


def _build_params(inp, l):
    prm = np.zeros((128, NPRM), np.float32)

    def put(name, arr):
        o, w = _PRM_OFF[name]
        prm[:, o:o + w] = np.asarray(arr, np.float32).reshape(128, w)

    qp = _qperm()
    put("gmix", _col8(inp["norm_mix"][l]))
    put("gffn", _col8(inp["norm_ffn"][l]))
    gn = np.array(inp["group_norm"][l])
    gn[512:768] = np.array(inp["group_norm"][l])[512:768][qp]
    put("ggn", _col8(gn))
    cw = np.asarray(inp["conv_w"][l])
    put("convw", np.stack([_col2(cw[i]) for i in range(3)], axis=2).reshape(128, 6))
    put("s5d", _col2(np.asarray(inp["s5_d"][l])))
    put("qg", np.tile(np.asarray(inp["attn_q_norm"][l]), 2)[:, None])
    put("kg", np.tile(np.asarray(inp["attn_k_norm"][l]), 2)[:, None])
    sk = np.asarray(inp["attn_sinks"][l])
    put("sink", np.stack([np.repeat(sk[[0 * 2 + r, 1 * 2 + r]], 64) for r in range(2)], axis=1))
    put("hlb0", _col2(np.asarray(inp["hg_lower_bounds"][0])))
    put("hlb1", _col2(np.asarray(inp["hg_lower_bounds"][1])))
    put("og", np.tile(np.asarray(inp["hg_out_norm"][l]), 2)[:, None])
    lre, lim, ldt = (np.asarray(inp[k][l]) for k in ("s5_lam_re", "s5_lam_im", "s5_log_dt"))
    bre, bim = np.asarray(inp["s5_b_re"][l]), np.asarray(inp["s5_b_im"][l])
    cre, cim = np.asarray(inp["s5_c_re"][l]), np.asarray(inp["s5_c_im"][l])
    put("A_lre", np.repeat(lre.reshape(2, 8, 1, 64), 16, axis=2).transpose(1, 2, 0, 3).reshape(128, 128))
    put("A_lim", np.repeat(lim.reshape(2, 8, 1, 64), 16, axis=2).transpose(1, 2, 0, 3).reshape(128, 128))
    put("A_ldt", np.repeat(ldt.reshape(2, 8, 1), 16, axis=2).transpose(1, 2, 0).reshape(128, 2))
    put("A_bre", bre.reshape(2, 8, 64, 16).transpose(1, 3, 0, 2).reshape(128, 128))
    put("A_bim", bim.reshape(2, 8, 64, 16).transpose(1, 3, 0, 2).reshape(128, 128))
    put("A_cre", np.repeat(cre.reshape(2, 8, 1, 16, 64), 16, axis=2).transpose(1, 2, 0, 3, 4).reshape(128, 2048))
    put("A_cim", np.repeat(cim.reshape(2, 8, 1, 16, 64), 16, axis=2).transpose(1, 2, 0, 3, 4).reshape(128, 2048))
    put("B_lre", lre.reshape(8, 2, 64).transpose(1, 2, 0).reshape(128, 8))
    put("B_lim", lim.reshape(8, 2, 64).transpose(1, 2, 0).reshape(128, 8))
    put("B_ldt", np.repeat(ldt.reshape(8, 2, 1), 64, axis=2).transpose(1, 2, 0).reshape(128, 8))
    put("B_cre", cre.reshape(8, 2, 16, 64).transpose(1, 3, 0, 2).reshape(128, 128))
    put("B_cim", cim.reshape(8, 2, 16, 64).transpose(1, 3, 0, 2).reshape(128, 128))
    return prm


def _chunks_kn(W):
    K, N = W.shape
    out = []
    for cg in range(N // 512):
        for kg in range(K // 1024):
            blk = W[kg * 1024:(kg + 1) * 1024, cg * 512:(cg + 1) * 512]
            out.append(np.ascontiguousarray(blk.reshape(8, 128, 512).transpose(1, 0, 2)).reshape(128, 4096))
    return out


def _build_weights(inp):
    qp = _qperm()
    chunks = []
    for l in range(DEPTH):
        w_in = np.asarray(inp["w_in"][l])
        win = np.array(w_in)
        win[:, 1024:1280] = w_in[:, 1024:1280][:, qp]
        w_out = np.asarray(inp["w_out"][l])
        wout = np.array(w_out)
        wout[512:768, :] = w_out[512:768, :][qp, :]
        chunks += _chunks_kn(win)
        chunks += _chunks_kn(wout)
        chunks += _chunks_kn(np.asarray(inp["w_ff1"][l]))
        chunks += _chunks_kn(np.asarray(inp["w_ff2"][l]))
    wch = np.stack(chunks, axis=0).astype(np.float32)
    glu = np.stack([np.asarray(inp["s5_w_glu"][l]).reshape(2, 128, 512).transpose(1, 0, 2) for l in range(DEPTH)], axis=0)
    return wch, np.ascontiguousarray(glu, np.float32)


class Buf:
    def __init__(self, ap2d, keys):
        self.a = ap2d
        self.keys_ = keys

    def __getitem__(self, k):
        return self.a[k]

    def v3(self, b):
        return self.a.rearrange("p (a b) -> p a b", b=b)


def bc(ap, shape):
    return ap.broadcast_to(list(shape))


def build_program(n_seq=SEQ_PER_CORE, n_tiles=TILES_PER_SEQ, layers=(0, 1), taps=None, same_engine_sync=True,
                  stop_after=None, skip_prologue=False):
    taps = taps or {}

    class StopBuild(Exception):
        pass

    def CHK(name):
        if stop_after == name:
            raise StopBuild()
    nc = bass.Bass("TRN2", target_bir_lowering=False)
    P = Prog(nc, same_engine_sync=same_engine_sync)
    NL = DEPTH

    xT = nc.dram_tensor("xT", [SEQ_PER_CORE, DM, SEQ], F32, kind="ExternalInput").ap()
    wch = nc.dram_tensor("wch", [NL * NCHUNK, 128, 4096], F32, kind="ExternalInput").ap()
    glu_d = nc.dram_tensor("glu", [NL, 128, 2, 512], F32, kind="ExternalInput").ap()
    prm_d = nc.dram_tensor("prm", [NL, 128, NPRM], F32, kind="ExternalInput").ap()
    cst_d = nc.dram_tensor("cst", [128, NCST], F32, kind="ExternalInput").ap()
    oT = nc.dram_tensor("oT", [SEQ_PER_CORE, DM, SEQ], F32, kind="ExternalOutput").ap()
    wsc = nc.dram_tensor("wsc", [NL * NCHUNK, 128, 4096], BF16).ap()
    tap_out = {}
    for name, shape in taps.items():
        if name.startswith("_"):
            continue
        tap_out[name] = nc.dram_tensor("tap_" + name, list(shape), F32, kind="ExternalOutput").ap()
    s5m_d = nc.dram_tensor("s5m_d", [NL, 128, 10240], BF16).ap()
    tap_dmas = []

    def sb(name, shape, dt=F32):
        return nc.alloc_sbuf_tensor("s_" + name, list(shape), dt)

    NRING = 4
    ring = [sb(f"ring{i}", [128, 8, 512], BF16) for i in range(NRING)]
    xs = sb("xs", [128, 8, NT], F32)
    hbuf = sb("hbuf", [128, 8, NT], BF16)
    cst = sb("cst", [128, NCST], F32)
    NF, NB = 23, 32
    farena = sb("farena", [128, NF * NT], F32)
    barena = sb("barena", [128, NB * NT], BF16)
    fmap = [False] * NF
    bmap = [False] * NB

    def _alloc(arena, amap, tag, n):
        for i in range(len(amap) - n + 1):
            if not any(amap[i:i + n]):
                for j in range(i, i + n):
                    amap[j] = True
                b = Buf(arena[:, i * NT:(i + n) * NT], [(tag, j) for j in range(i, i + n)])
                b.rng = (i, n)
                return b
        raise RuntimeError(f"arena {tag} exhausted")

    def falloc(n=1):
        return _alloc(farena, fmap, "fp", n)

    def balloc(n=1):
        return _alloc(barena, bmap, "bp", n)

    def free(b):
        i, n = b.rng
        amap = fmap if b.keys_[0][0] == "fp" else bmap
        for j in range(i, i + n):
            assert amap[j]
            amap[j] = False

    banks = [nc.alloc_psum_tensor(f"bank{i}", [128, 512], F32) for i in range(8)]
    BK = [("bank", i) for i in range(8)]

    def cs(name):
        o, w = _CST_OFF[name]
        return cst[:, o:o + w]

    ident_bf = sb("ident_bf", [128, 128], BF16)
    bones_bf = sb("bones_bf", [128, 128], BF16)
    ones_bf = sb("ones_bf", [128, 128], BF16)
    sb_int = sb("sb_int", [128, 512], I32)
    Amz = sb("Amz", [128, 2048], BF16)

    s5m = sb("s5m", [128, 10240], BF16)
    L1v = s5m[:, 0:4096].rearrange("p (j r t c) -> p j r t c", j=2, r=2, t=8)
    L3v = s5m[:, 4096:8192].rearrange("p (r q t c) -> p r q t c", r=2, q=8, t=8)
    KLv = s5m[:, 8192:10240].rearrange("p (j t c) -> p j t c", j=2, t=8)
    L = []
    for l in range(NL):
        d = dict(
            sp=sb(f"sp{l}", [128, 48], F32),
            glu=sb(f"glu{l}", [128, 2, 512], BF16),
            L1=L1v,
            L3=L3v,
            KL=KLv,
            cosR=sb(f"cosR{l}", [128, 512], F32),
            sinR=sb(f"sinR{l}", [128, 512], F32),
            rtab=sb(f"rtab{l}", [128, 512], F32),
            r8=sb(f"r8_{l}", [128, 8], F32),
            zbuf=sb(f"zbuf{l}", [128, 2, NT + 2], F32),
            Er=sb(f"Er{l}", [128, 8, NC5 + 1], F32),
            Ei=sb(f"Ei{l}", [128, 8, NC5 + 1], F32),
            kbuf=sb(f"kbuf{l}", [128, 128 + NT], BF16),
            vbuf=sb(f"vbuf{l}", [128, 5, 128], BF16),
            S=[sb(f"S{l}_{j}", [128, 64], F32) for j in range(2)],
        )
        L.append(d)
    SPC = dict(gmix=0, gffn=8, ggn=16, convw=24, s5d=30, qgs=32, kg=33, esink=34, lb=36, oml=38, og=40)

    def ACT(out, in_, func, reads, writes, scale=1.0, bias=None):
        if bias is None:
            return P.op("act", lambda e: e.activation(out=out, in_=in_, func=func, scale=scale), reads, writes)
        return P.op("act", lambda e: e.activation(out=out, in_=in_, func=func, scale=scale, bias=bias), reads, writes)

    def TT(eng, out, a, b, op, reads, writes):
        return P.op(eng, lambda e: e.tensor_tensor(out=out, in0=a, in1=b, op=op), reads, writes)

    def TS(eng, out, a, s1, op0, reads, writes, s2=None, op1=None):
        if op1 is None:
            return P.op(eng, lambda e: e.tensor_scalar(out=out, in0=a, scalar1=s1, scalar2=None, op0=op0), reads, writes)
        return P.op(eng, lambda e: e.tensor_scalar(out=out, in0=a, scalar1=s1, scalar2=s2, op0=op0, op1=op1), reads, writes)

    def STT(out, in0, scalar, in1, op0, op1, reads, writes):
        return P.op("dve", lambda e: e.scalar_tensor_tensor(out=out, in0=in0, scalar=scalar, in1=in1, op0=op0, op1=op1), reads, writes)

    def CP(eng, out, in_, reads, writes):
        if eng == "act":
            return P.op("act", lambda e: e.activation(out=out, in_=in_, func=AF.Copy), reads, writes)
        return P.op(eng, lambda e: e.tensor_copy(out=out, in_=in_), reads, writes)

    def RECIP(out, in_, reads, writes):
        return P.op("dve", lambda e: e.reciprocal(out=out, in_=in_), reads, writes)

    def MM(mms, reads, writes, force=False):
        def fn(e):
            ins = None
            for m in mms:
                if m.get("tp") is not None:
                    ins = e.matmul(m["out"], lhsT=m["lhsT"], rhs=m["rhs"], start=m["start"], stop=m["stop"], skip_group_check=True,
                                   tile_position=m["tp"])
                else:
                    ins = e.matmul(m["out"], lhsT=m["lhsT"], rhs=m["rhs"], start=m["start"], stop=m["stop"], skip_group_check=True)
            return ins
        return P.op("pe", fn, reads, writes, force=force)

    def mm(out, lhsT, rhs, start=True, stop=True, tp=None):
        return dict(out=out, lhsT=lhsT, rhs=rhs, start=start, stop=stop, tp=tp)

    def TAP(name, idx, src_ap, reads):
        if name in tap_out:
            o = P.dma("sp", tap_out[name][idx], src_ap, reads=reads, writes=[("tap", name, idx)], chan=("tap", name))
            tap_dmas.append(o)

    P.dma("sp", cst[:], cst_d, writes=["cst"], chan="cst")
    CP("dve", ident_bf[:], cs("ident"), ["cst"], ["ident_bf"])
    CP("dve", bones_bf[:], cs("bones"), ["cst"], ["bones_bf"])
    P.op("pool", lambda e: e.memset(ones_bf[:], 1.0), writes=["ones_bf"])
    P.op("pool", lambda e: e.memset(Amz[:, :], 0.0), writes=["Amz"])

    for l in layers:
        for c in range(NCHUNK):
            ci = l * NCHUNK + c
            P.dma("pool", wsc[ci].rearrange("p (a b) -> p a b", b=2048), wch[ci].rearrange("p (a b) -> p a b", b=2048),
                  writes=[("wsc", ci)], chan=("wcast", ci % 4))

    prm_sb = falloc(2)
    _ac_lo = _PRM_OFF["A_cre"][0]
    _ac_hi = _PRM_OFF["A_cim"][0] + 2048
    NSMALL = NPRM - 4096
    assert NSMALL <= 2 * NT

    def prologue_layer(l):
        d = L[l]
        sp = d["sp"]
        SPK = ("sp", l)
        P.dma("sp", prm_sb[:, 0:_ac_lo], prm_d[l, :, 0:_ac_lo], writes=[prm_sb], chan="prm")
        P.dma("sp", prm_sb[:, _ac_lo:NSMALL], prm_d[l, :, _ac_hi:NPRM], writes=[prm_sb], chan="prm2")

        def pf(name, a=0, b=None):
            o, w = _PRM_OFF[name]
            if o >= _ac_hi:
                o -= 4096
            b = w if b is None else b
            return prm_sb[:, o + a:o + b]

        RD = [prm_sb, "cst"]
        for nm in ("gmix", "gffn", "ggn", "convw", "s5d", "kg", "og"):
            w = _PRM_OFF[nm][1]
            CP("dve", sp[:, SPC[nm]:SPC[nm] + w], pf(nm), RD, [SPK])
        TS("dve", sp[:, SPC["qgs"]:SPC["qgs"] + 1], pf("qg"), 0.125, ALU.mult, RD, [SPK])
        ACT(sp[:, SPC["esink"]:SPC["esink"] + 2], pf("sink"), AF.Exp, RD, [SPK])
        if l == 0:
            P.op("pool", lambda e: e.memset(sp[:, SPC["lb"]:SPC["lb"] + 2], 0.0), writes=[SPK])
        else:
            tmp = falloc()
            TT("dve", tmp[:, 0:2], pf("hlb1"), pf("hlb0"), ALU.subtract, RD, [tmp])
            ACT(sp[:, SPC["lb"]:SPC["lb"] + 2], tmp[:, 0:2], AF.Sigmoid, [tmp], [SPK])
            free(tmp)
        TS("dve", sp[:, SPC["oml"]:SPC["oml"] + 2], sp[:, SPC["lb"]:SPC["lb"] + 2], -1.0, ALU.mult, [SPK], [SPK], s2=1.0, op1=ALU.add)
        gl32 = falloc(2)
        P.dma("sp", gl32[:, 0:1024].rearrange("p (k n) -> p k n", n=512), glu_d[l], writes=[gl32], chan="glu")
        CP("dve", d["glu"][:, 0, :], gl32[:, 0:512], [gl32], [("glu", l)])
        CP("dve", d["glu"][:, 1, :], gl32[:, 512:1024], [gl32], [("glu", l)])
        free(gl32)

        def trig(u, n, cos_out, sin_out, ub, cb, sbk, shape3=None):
            t1 = falloc(); t2 = falloc()
            for (shift, outv, ob) in ((0.0, sin_out, sbk), (0.25, cos_out, cb)):
                TS("dve", t1[:, 0:n], u, shift + 64.0, ALU.add, [ub], [t1])
                CP("dve", sb_int[:, 0:n], t1[:, 0:n], [t1], ["sb_int"])
                CP("dve", t2[:, 0:n], sb_int[:, 0:n], ["sb_int"], [t2])
                TT("dve", t1[:, 0:n], t1[:, 0:n], t2[:, 0:n], ALU.subtract, [t1, t2], [t1])
                TS("dve", t2[:, 0:n], t1[:, 0:n], 0.5, ALU.is_gt, [t1], [t2])
                TT("dve", t1[:, 0:n], t1[:, 0:n], t2[:, 0:n], ALU.subtract, [t1, t2], [t1])
                TS("dve", t2[:, 0:n], t1[:, 0:n], -0.5, ALU.is_lt, [t1], [t2])
                TT("dve", t1[:, 0:n], t1[:, 0:n], t2[:, 0:n], ALU.add, [t1, t2], [t1])
                ACT(outv, t1[:, 0:n], AF.Sin, [t1], [ob], scale=TWO_PI)
            free(t1); free(t2)
            return None

        a_lr = falloc(); a_dt = falloc(); a_e1 = falloc(); a_th = falloc()
        TS("dve", a_lr[:, 0:128], pf("A_lre"), -1e-4, ALU.min, RD, [a_lr])
        ACT(a_dt[:, 0:2], pf("A_ldt"), AF.Exp, RD, [a_dt])
        for j in range(2):
            sl = slice(j * 64, (j + 1) * 64)
            TS("dve", a_e1[:, sl], a_lr[:, sl], a_dt[:, j:j + 1], ALU.mult, [a_lr, a_dt], [a_e1])
            TS("dve", a_th[:, sl], pf("A_lim")[:, sl], a_dt[:, j:j + 1], ALU.mult, RD + [a_dt], [a_th], s2=1.0 / TWO_PI, op1=ALU.mult)
        for j in range(2):
            sl = slice(j * 64, (j + 1) * 64)
            u = falloc(); me = falloc(); cc = falloc(); ss = falloc(); mr = falloc(); mi = falloc()
            u3, me3 = u.v3(64), me.v3(64)
            tau_b = bc(cs("tauA").unsqueeze(2), [128, 8, 64])
            TT("dve", u3, bc(a_th[:, sl].unsqueeze(1), [128, 8, 64]), tau_b, ALU.mult, [a_th, "cst"], [u])
            TT("dve", me3, bc(a_e1[:, sl].unsqueeze(1), [128, 8, 64]), tau_b, ALU.mult, [a_e1, "cst"], [me])
            ACT(me[:, :], me[:, :], AF.Exp, [me], [me])
            trig(u[:, :], 512, cc[:, :], ss[:, :], u, cc, ss)
            TT("dve", mr[:, :], me[:, :], cc[:, :], ALU.mult, [me, cc], [mr])
            TT("dve", mi[:, :], me[:, :], ss[:, :], ALU.mult, [me, ss], [mi])
            free(u); free(me); free(cc); free(ss)
            w = falloc()
            W = lambda i: w[:, i * 64:(i + 1) * 64]
            lr_, li_ = a_lr[:, sl], pf("A_lim")[:, sl]
            TT("dve", W(0), lr_, lr_, ALU.mult, [a_lr], [w])
            TT("dve", W(2), li_, li_, ALU.mult, RD, [w])
            TT("dve", W(0), W(0), W(2), ALU.add, [w], [w])
            RECIP(W(0), W(0), [w], [w])
            TS("dve", W(1), mr[:, 64:128], -1.0, ALU.add, [mr], [w])
            TT("dve", W(2), W(1), lr_, ALU.mult, [w, a_lr], [w])
            TT("dve", W(7), mi[:, 64:128], li_, ALU.mult, [mi] + RD, [w])
            TT("dve", W(2), W(2), W(7), ALU.add, [w], [w])
            TT("dve", W(3), W(2), W(0), ALU.mult, [w], [w])
            TT("dve", W(2), mi[:, 64:128], lr_, ALU.mult, [mi, a_lr], [w])
            TT("dve", W(7), W(1), li_, ALU.mult, [w] + RD, [w])
            TT("dve", W(2), W(2), W(7), ALU.subtract, [w], [w])
            TT("dve", W(4), W(2), W(0), ALU.mult, [w], [w])
            bre_, bim_ = pf("A_bre")[:, sl], pf("A_bim")[:, sl]
            TT("dve", W(5), W(3), bre_, ALU.mult, [w] + RD, [w])
            TT("dve", W(7), W(4), bim_, ALU.mult, [w] + RD, [w])
            TT("dve", W(5), W(5), W(7), ALU.subtract, [w], [w])
            TT("dve", W(6), W(3), bim_, ALU.mult, [w] + RD, [w])
            TT("dve", W(7), W(4), bre_, ALU.mult, [w] + RD, [w])
            TT("dve", W(6), W(6), W(7), ALU.add, [w], [w])
            p1r = falloc(); p1i = falloc(); t = falloc()
            bbr_b = bc(W(5).unsqueeze(1), [128, 8, 64]); bbi_b = bc(W(6).unsqueeze(1), [128, 8, 64])
            TT("dve", p1r.v3(64), mr.v3(64), bbr_b, ALU.mult, [mr, w], [p1r])
            TT("dve", t.v3(64), mi.v3(64), bbi_b, ALU.mult, [mi, w], [t])
            TT("dve", p1r[:, :], p1r[:, :], t[:, :], ALU.subtract, [p1r, t], [p1r])
            TT("dve", p1i.v3(64), mr.v3(64), bbi_b, ALU.mult, [mr, w], [p1i])
            TT("dve", t.v3(64), mi.v3(64), bbr_b, ALU.mult, [mi, w], [t])
            TT("dve", p1i[:, :], p1i[:, :], t[:, :], ALU.add, [p1i, t], [p1i])
            free(w); free(mr); free(mi)
            for ri, src in ((0, p1r), (1, p1i)):
                for gp in range(2):
                    TS("dve", d["L1"][:, j, ri, :, gp * 64:(gp + 1) * 64], src.v3(64), cs("mgp%d" % gp), ALU.mult, [src, "cst"], ["s5m"])
            kc = falloc()
            t2 = falloc()
            acr = falloc(2); aci = falloc(2)
            ocr, oci = _PRM_OFF["A_cre"][0], _PRM_OFF["A_cim"][0]
            P.dma("sp", acr[:, 0:1024], prm_d[l, :, ocr + j * 1024:ocr + (j + 1) * 1024], writes=[acr], chan="acr")
            P.dma("sp", aci[:, 0:1024], prm_d[l, :, oci + j * 1024:oci + (j + 1) * 1024], writes=[aci], chan="aci")
            for ho in range(16):
                cr_b = bc(acr[:, ho * 64:(ho + 1) * 64].unsqueeze(1), [128, 8, 64])
                ci_b = bc(aci[:, ho * 64:(ho + 1) * 64].unsqueeze(1), [128, 8, 64])
                TT("dve", t.v3(64), p1r.v3(64), cr_b, ALU.mult, [p1r, acr], [t])
                TT("dve", t2.v3(64), p1i.v3(64), ci_b, ALU.mult, [p1i, aci], [t2])
                TT("dve", t[:, :], t[:, :], t2[:, :], ALU.subtract, [t, t2], [t])
                P.op("dve", lambda e, ho=ho: e.tensor_reduce(out=kc[:, 0:128].rearrange("p (a b) -> p a b", b=16)[:, :, ho], in_=t.v3(64),
                                                             axis=AX.X, op=ALU.add), [t], [kc])
            free(t2); free(p1r); free(p1i); free(acr); free(aci)
            klf = falloc(2)
            for g in range(8):
                o_, _w = _CST_OFF["mg8"]
                TS("dve", klf[:, 0:1024].rearrange("p (a b) -> p a b", b=128)[:, :, g * 16:(g + 1) * 16],
                   kc[:, 0:128].rearrange("p (a b) -> p a b", b=16), cst[:, o_ + g:o_ + g + 1], ALU.mult, [kc, "cst"], [klf])
            STT(klf[:, 0:128], cs("ident"), sp[:, SPC["s5d"] + j:SPC["s5d"] + j + 1], klf[:, 0:128], ALU.mult, ALU.add, ["cst", SPK, klf], [klf])
            CP("dve", d["KL"][:, j, :, :], klf[:, 0:1024].rearrange("p (a b) -> p a b", b=128), [klf], ["s5m"])
            free(kc); free(klf); free(t)
        free(a_lr); free(a_dt); free(a_e1); free(a_th)

        b_ = falloc()
        Bc = lambda i: b_[:, i * 8:(i + 1) * 8]
        TS("dve", Bc(0), pf("B_lre"), -1e-4, ALU.min, RD, [b_])
        ACT(Bc(1), pf("B_ldt"), AF.Exp, RD, [b_])
        TT("dve", Bc(2), Bc(0), Bc(1), ALU.mult, [b_], [b_])
        TT("dve", Bc(3), pf("B_lim"), Bc(1), ALU.mult, RD + [b_], [b_])
        TS("dve", Bc(3), Bc(3), 1.0 / TWO_PI, ALU.mult, [b_], [b_])
        u = falloc(); me = falloc(); cc = falloc(); ss = falloc()
        tauB_b = bc(cs("tauB").unsqueeze(1), [128, 8, 8])
        TT("dve", u[:, 0:64].rearrange("p (a b) -> p a b", b=8), bc(Bc(3).unsqueeze(2), [128, 8, 8]), tauB_b, ALU.mult, [b_, "cst"], [u])
        TT("dve", me[:, 0:64].rearrange("p (a b) -> p a b", b=8), bc(Bc(2).unsqueeze(2), [128, 8, 8]), tauB_b, ALU.mult, [b_, "cst"], [me])
        ACT(me[:, 0:64], me[:, 0:64], AF.Exp, [me], [me])
        trig(u[:, 0:64], 64, cc[:, 0:64], ss[:, 0:64], u, cc, ss)
        TT("dve", cc[:, 0:64], cc[:, 0:64], me[:, 0:64], ALU.mult, [cc, me], [cc])
        TT("dve", ss[:, 0:64], ss[:, 0:64], me[:, 0:64], ALU.mult, [ss, me], [ss])
        for qh in range(2):
            cr_b = bc(pf("B_cre")[:, qh * 64:(qh + 1) * 64].rearrange("p (q h) -> p q h", h=16).unsqueeze(2), [128, 4, 8, 16])
            ci_b = bc(pf("B_cim")[:, qh * 64:(qh + 1) * 64].rearrange("p (q h) -> p q h", h=16).unsqueeze(2), [128, 4, 8, 16])
            mr_b = bc(cc[:, qh * 32:(qh + 1) * 32].rearrange("p (q t) -> p q t", t=8).unsqueeze(3), [128, 4, 8, 16])
            mi_b = bc(ss[:, qh * 32:(qh + 1) * 32].rearrange("p (q t) -> p q t", t=8).unsqueeze(3), [128, 4, 8, 16])
            c1 = falloc(); c2 = falloc()
            v4 = lambda b: b[:, :].rearrange("p (q t h) -> p q t h", t=8, h=16)
            TT("dve", v4(c1), cr_b, mr_b, ALU.mult, RD + [cc], [c1])
            TT("dve", v4(c2), ci_b, mi_b, ALU.mult, RD + [ss], [c2])
            TT("dve", c1[:, :], c1[:, :], c2[:, :], ALU.subtract, [c1, c2], [c1])
            for gp in range(2):
                TS("dve", d["L3"][:, 0, qh * 4:(qh + 1) * 4, :, gp * 16:(gp + 1) * 16], v4(c1), cs("mB%d" % gp), ALU.mult, [c1, "cst"], ["s5m"])
            TT("dve", v4(c1), cr_b, mi_b, ALU.mult, RD + [ss], [c1])
            TT("dve", v4(c2), ci_b, mr_b, ALU.mult, RD + [cc], [c2])
            TT("dve", c1[:, :], c1[:, :], c2[:, :], ALU.add, [c1, c2], [c1])
            for gp in range(2):
                TS("dve", d["L3"][:, 1, qh * 4:(qh + 1) * 4, :, gp * 16:(gp + 1) * 16], v4(c1), cs("mB%d" % gp), ALU.mult, [c1, "cst"], ["s5m"],
                   s2=-1.0, op1=ALU.mult)
            free(c1); free(c2)
        ACT(d["r8"][:, :], Bc(2), AF.Exp, [b_], [("r8", l)], scale=8.0)
        TT("dve", d["rtab"][:, :].rearrange("p (q c) -> p q c", c=64), bc(d["r8"][:, :].unsqueeze(2), [128, 8, 64]),
           cs("segm").rearrange("p (q c) -> p q c", c=64), ALU.mult, [("r8", l), "cst"], [("rtab", l)])
        TS("dve", Bc(4), Bc(3), 8.0, ALU.mult, [b_], [b_], s2=64.0, op1=ALU.add)
        CP("dve", sb_int[:, 0:8], Bc(4), [b_], ["sb_int"])
        CP("dve", Bc(5), sb_int[:, 0:8], ["sb_int"], [b_])
        TT("dve", Bc(4), Bc(4), Bc(5), ALU.subtract, [b_], [b_])
        TS("dve", Bc(5), Bc(4), 0.5, ALU.is_gt, [b_], [b_])
        TT("dve", Bc(4), Bc(4), Bc(5), ALU.subtract, [b_], [b_])
        TS("dve", Bc(5), Bc(4), -0.5, ALU.is_lt, [b_], [b_])
        TT("dve", Bc(4), Bc(4), Bc(5), ALU.add, [b_], [b_])
        TT("dve", u.v3(64), bc(Bc(4).unsqueeze(2), [128, 8, 64]), bc(cs("cidx").unsqueeze(1), [128, 8, 64]), ALU.mult, [b_, "cst"], [u])
        trig(u[:, :], 512, d["cosR"][:, :], d["sinR"][:, :], u, ("cosR", l), ("sinR", l))
        free(u); free(me); free(cc); free(ss); free(b_)

    def prologue_taps(l):
        d = L[l]
        if "KL" in tap_out:
            tf = falloc(4)
            CP("dve", tf[:, 0:2048], s5m[:, 8192:10240], ["s5m"], [tf])
            TAP("KL", l, tf[:, 0:2048], [tf]); free(tf)
        if "L1" in tap_out:
            tf = falloc(8)
            CP("dve", tf[:, 0:4096], s5m[:, 0:4096], ["s5m"], [tf])
            TAP("L1", l, tf[:, 0:4096], [tf]); free(tf)
        if "L3" in tap_out:
            tf = falloc(8)
            CP("dve", tf[:, 0:4096], s5m[:, 4096:8192], ["s5m"], [tf])
            TAP("L3", l, tf[:, 0:4096], [tf]); free(tf)
        if "rot" in tap_out:
            TAP("rot", (l, 0), d["cosR"][:, :], [("cosR", l)])
            TAP("rot", (l, 1), d["sinR"][:, :], [("sinR", l)])
            TAP("rot", (l, 2), d["rtab"][:, :], [("rtab", l)])

    for l in layers:
        prologue_layer(l)
        prologue_taps(l)
        P.dma("sp", s5m_d[l], s5m[:, :], reads=["s5m"], writes=[("s5m_d", l)], chan="s5m_st")
    free(prm_sb)

    tile_list = [(s, ti, l) for s in range(n_seq) for ti in range(n_tiles) for l in layers]
    chunk_seq = [(l, c) for (s, ti, l) in tile_list for c in range(NCHUNK)]
    ring_state = dict(issued=0, used=0)

    def issue_next():
        n = ring_state["issued"]
        if n >= len(chunk_seq):
            return
        l, c = chunk_seq[n]
        slot = n % NRING
        ci = l * NCHUNK + c
        P.dma("sp", ring[slot][:, :, :].rearrange("p k n -> p (k n)"), wsc[ci], reads=[("wsc", ci)], writes=[("ring", slot)], chan=("ring", slot))
        ring_state["issued"] += 1

    def get_chunk(l, c):
        n = ring_state["used"]
        assert chunk_seq[n] == (l, c), (chunk_seq[n], l, c)
        while ring_state["issued"] <= n:
            issue_next()
        return n % NRING

    def release_chunk():
        ring_state["used"] += 1
        while ring_state["issued"] < min(len(chunk_seq), ring_state["used"] + NRING):
            issue_next()

    for _ in range(NRING):
        issue_next()

    def rmsnorm_to_h(l, gcol):
        sp = L[l]["sp"]
        for k in range(8):
            sq = balloc()
            ACT(sq[:, :], xs[:, k, :], AF.Square, [("xs", k)], [sq])
            MM([mm(banks[7][:, :], ones_bf[:, :], sq[:, :], start=(k == 0), stop=(k == 7))], [sq, "ones_bf"], [BK[7]])
            free(sq)
        rs = falloc()
        ACT(rs[:, :], banks[7][:, :], AF.Sqrt, [BK[7]], [rs], scale=1.0 / DM, bias=EPS)
        RECIP(rs[:, :], rs[:, :], [rs], [rs])
        for k in range(8):
            STT(hbuf[:, k, :], xs[:, k, :], sp[:, gcol + k:gcol + k + 1], rs[:, :], ALU.mult, ALU.mult, [("xs", k), ("sp", l), rs], [("h", k)])
        free(rs)

    HK = [("h", k) for k in range(8)]

    def proj_fm(slot, m, bank):
        MM([mm(banks[bank][:, :], ring[slot][:, k, m * 128:(m + 1) * 128], hbuf[:, k, :], start=(k == 0), stop=(k == 7)) for k in range(8)],
           [("ring", slot)] + HK, [BK[bank]])

    def layer_tile(s, ti, l):
        d = L[l]
        sp = d["sp"]
        SPK = ("sp", l)
        first = (ti == 0)
        t0 = ti * NT
        if l == layers[0]:
            for k in range(8):
                P.dma("act", xs[:, k, :], xT[s, k * 128:(k + 1) * 128, t0:t0 + NT], writes=[("xs", k)], chan=("xs", k))
        if first:
            P.op("pool", lambda e: e.memset(d["zbuf"][:, :, 0:2], 0.0), writes=[("zbuf", l)])
            P.op("pool", lambda e: e.memset(d["Er"][:, :, 0:1], 0.0), writes=[("Er", l)])
            P.op("pool", lambda e: e.memset(d["Ei"][:, :, 0:1], 0.0), writes=[("Ei", l)])
            for j in range(2):
                P.op("pool", lambda e, j=j: e.memset(d["S"][j][:, :], 0.0), writes=[("S", l, j)])
        P.dma("act", s5m[:, :], s5m_d[l], reads=[("s5m_d", l)], writes=["s5m"], chan="s5m_ld")
        rmsnorm_to_h(l, SPC["gmix"])
        CHK("norm1")
        Y = [falloc() for _ in range(8)]
        YN = [balloc() for _ in range(8)]

        def group_norm(gI, ssb):
            for i in range(2):
                sq = balloc()
                ACT(sq[:, :], Y[2 * gI + i][:, :], AF.Square, [Y[2 * gI + i]], [sq])
                MM([mm(banks[ssb][:, :], ones_bf[:, :], sq[:, :], start=(i == 0), stop=(i == 1))], [sq, "ones_bf"], [BK[ssb]])
                free(sq)
            rs = falloc()
            ACT(rs[:, :], banks[ssb][:, :], AF.Sqrt, [BK[ssb]], [rs], scale=1.0 / 256, bias=EPS)
            RECIP(rs[:, :], rs[:, :], [rs], [rs])
            for i in range(2):
                k = 2 * gI + i
                STT(YN[k][:, :], Y[k][:, :], sp[:, SPC["ggn"] + k:SPC["ggn"] + k + 1], rs[:, :], ALU.mult, ALU.mult, [Y[k], SPK, rs], [YN[k]])
            free(rs)
            free(Y[2 * gI]); free(Y[2 * gI + 1])

        def chainX():
            slot = get_chunk(l, 0)
            hsb = [falloc() for _ in range(2)]
            bsb = [falloc() for _ in range(2)]
            for j in range(2):
                proj_fm(slot, j, j)
                CP("act", hsb[j][:, :], banks[j][:, :], [BK[j]], [hsb[j]])
            for j in range(2):
                proj_fm(slot, 2 + j, j)
                CP("act", bsb[j][:, :], banks[j][:, :], [BK[j]], [bsb[j]])
            release_chunk()
            slot = get_chunk(l, 1)
            zb = d["zbuf"]
            for j in range(2):
                proj_fm(slot, j, j)
                TT("dve", zb[:, j, 2:NT + 2], banks[j][:, :], hsb[j][:, :], ALU.mult, [BK[j], hsb[j]], [("zbuf", l)])
            ubf = balloc(2)
            for j in range(2):
                proj_fm(slot, 2 + j, j)
                CP("act", ubf[:, j * NT:(j + 1) * NT].rearrange("p (t c) -> p t c", c=NC5), banks[j][:, :].rearrange("p (c t) -> p t c", t=TS5),
                   [BK[j]], [ubf])
            release_chunk()
            yield
            for j in range(2):
                acc = falloc()
                cw = lambda i: sp[:, SPC["convw"] + j * 3 + i:SPC["convw"] + j * 3 + i + 1]
                TS("pool", acc[:, :], zb[:, j, 2:NT + 2], cw(2), ALU.mult, [("zbuf", l), SPK], [acc])
                STT(acc[:, :], zb[:, j, 1:NT + 1], cw(1), acc[:, :], ALU.mult, ALU.add, [("zbuf", l), SPK, acc], [acc])
                STT(acc[:, :], zb[:, j, 0:NT], cw(0), acc[:, :], ALU.mult, ALU.add, [("zbuf", l), SPK, acc], [acc])
                TT("pool", Y[j][:, :], acc[:, :], bsb[j][:, :], ALU.mult, [acc, bsb[j]], [Y[j]])
                free(acc)
            P.op("pool", lambda e: e.tensor_copy(out=d["zbuf"][:, :, 0:2], in_=d["zbuf"][:, :, NT:NT + 2]), [("zbuf", l)], [("zbuf", l)])
            for b_ in hsb + bsb:
                free(b_)
            yield
            group_norm(0, 1)
            yield
            ut = [ubf[:, j * NT:(j + 1) * NT] for j in range(2)]
            for ri in range(2):
                for qq in range(4):
                    mms = []
                    for j in range(2):
                        q = j * 4 + qq
                        for s_ in range(TS5):
                            mms.append(mm(banks[ri][:, q * NC5:(q + 1) * NC5], d["L1"][qq * 32:(qq + 1) * 32, j, ri, TS5 - 1 - s_, :],
                                          ut[j][qq * 32:(qq + 1) * 32, s_ * NC5:(s_ + 1) * NC5], start=(s_ == 0), stop=(s_ == TS5 - 1), tp=(qq * 32, 0)))
                    MM(mms, [ubf, "s5m"], [BK[ri]], force=True)
            yield
            Wr = falloc(); Wi = falloc(); t1 = falloc(); t2 = falloc()
            cosR, sinR, rtab = d["cosR"], d["sinR"], d["rtab"]
            CK, SK, RK = ("cosR", l), ("sinR", l), ("rtab", l)
            TT("dve", t1[:, :], banks[0][:, :], cosR[:, :], ALU.mult, [BK[0], CK], [t1])
            TT("dve", t2[:, :], banks[1][:, :], sinR[:, :], ALU.mult, [BK[1], SK], [t2])
            TT("pool", Wr[:, :], t1[:, :], t2[:, :], ALU.add, [t1, t2], [Wr])
            TT("dve", t1[:, :], banks[1][:, :], cosR[:, :], ALU.mult, [BK[1], CK], [t1])
            TT("dve", t2[:, :], banks[0][:, :], sinR[:, :], ALU.mult, [BK[0], SK], [t2])
            TT("pool", Wi[:, :], t1[:, :], t2[:, :], ALU.subtract, [t1, t2], [Wi])
            yield
            for (Wx, Ex, ek) in ((Wr, d["Er"], ("Er", l)), (Wi, d["Ei"], ("Ei", l))):
                TT("dve", t1[:, 0:8], d["r8"][:, :], Ex[:, :, 0], ALU.mult, [("r8", l), ek], [t1])
                TT("dve", Wx.v3(NC5)[:, :, 0], Wx.v3(NC5)[:, :, 0], t1[:, 0:8], ALU.add, [Wx, t1], [Wx])
            Fr = falloc(); Fi = falloc()
            for (Wx, Fx) in ((Wr, Fr), (Wi, Fi)):
                P.op("dve", lambda e, Wx=Wx, Fx=Fx: e.tensor_tensor_scan(out=Fx[:, :], data0=rtab[:, :], data1=Wx[:, :], initial=0.0,
                                                                        op0=ALU.mult, op1=ALU.add), [RK, Wx], [Fx])
            yield
            TT("dve", t1[:, :], Fr[:, :], cosR[:, :], ALU.mult, [Fr, CK], [t1])
            TT("dve", t2[:, :], Fi[:, :], sinR[:, :], ALU.mult, [Fi, SK], [t2])
            TT("pool", d["Er"][:, :, 1:NC5 + 1], t1.v3(NC5), t2.v3(NC5), ALU.subtract, [t1, t2], [("Er", l)])
            TT("dve", t1[:, :], Fi[:, :], cosR[:, :], ALU.mult, [Fi, CK], [t1])
            TT("dve", t2[:, :], Fr[:, :], sinR[:, :], ALU.mult, [Fr, SK], [t2])
            TT("pool", d["Ei"][:, :, 1:NC5 + 1], t1.v3(NC5), t2.v3(NC5), ALU.add, [t1, t2], [("Ei", l)])
            for b_ in (Wr, Wi, t1, t2, Fr, Fi):
                free(b_)
            Ebf = balloc(2)
            CP("act", Ebf[:, 0:NT].rearrange("p (q c) -> p q c", c=NC5), d["Er"][:, :, 0:NC5], [("Er", l)], [Ebf])
            CP("act", Ebf[:, NT:2 * NT].rearrange("p (q c) -> p q c", c=NC5), d["Ei"][:, :, 0:NC5], [("Ei", l)], [Ebf])
            P.op("pool", lambda e: e.tensor_copy(out=d["Er"][:, :, 0:1], in_=d["Er"][:, :, NC5:NC5 + 1]), [("Er", l)], [("Er", l)])
            P.op("pool", lambda e: e.tensor_copy(out=d["Ei"][:, :, 0:1], in_=d["Ei"][:, :, NC5:NC5 + 1]), [("Ei", l)], [("Ei", l)])
            yield
            E4 = [Ebf[:, ri * NT:(ri + 1) * NT].rearrange("p (q c) -> p q c", c=NC5) for ri in range(2)]
            for j in range(2):
                yb = banks[j]
                mms = []
                for tau in range(TS5):
                    mms.append(mm(yb[:, tau * NC5:NT], d["KL"][:, j, tau, :], ut[j][:, 0:(TS5 - tau) * NC5], start=(tau == 0), stop=False))
                for qq in range(4):
                    q = j * 4 + qq
                    for t_ in range(TS5):
                        for ri in range(2):
                            mms.append(mm(yb[qq * 32:(qq + 1) * 32, t_ * NC5:(t_ + 1) * NC5], d["L3"][:, ri, q, t_, :], E4[ri][:, q, :], start=False,
                                          stop=(qq == 3 and t_ == TS5 - 1 and ri == 1), tp=(0, qq * 32)))
                MM(mms, [ubf, Ebf, "s5m"], [BK[j]])
            yield
            gl = balloc(2)
            for j in range(2):
                ysb = falloc(); sq = falloc()
                ynat = banks[j][:, :].rearrange("p (t c) -> p c t", c=NC5)
                CP("act", ysb.v3(TS5), ynat, [BK[j]], [ysb])
                ACT(sq.v3(TS5), ynat, AF.Square, [BK[j]], [sq])
                TS("dve", sq[:, :], sq[:, :], 0.044715, ALU.mult, [sq], [sq], s2=1.0, op1=ALU.add)
                TT("dve", sq[:, :], sq[:, :], ysb[:, :], ALU.mult, [sq, ysb], [sq])
                ACT(sq[:, :], sq[:, :], AF.Sigmoid, [sq], [sq], scale=1.5957691216057308)
                TT("pool", gl[:, j * NT:(j + 1) * NT], ysb[:, :], sq[:, :], ALU.mult, [ysb, sq], [gl])
                free(ysb); free(sq)
                yield
            free(ubf); free(Ebf)
            for j in range(2):
                for (n, bk) in ((j, 0), (2 + j, 1)):
                    MM([mm(banks[bk][:, :], d["glu"][:, k, n * 128:(n + 1) * 128], gl[:, k * NT:(k + 1) * NT], start=(k == 0), stop=(k == 1)) for k in range(2)],
                       [gl, ("glu", l)], [BK[bk]])
                sg = falloc()
                ACT(sg[:, :], banks[1][:, :], AF.Sigmoid, [BK[1]], [sg])
                TT("dve", Y[2 + j][:, :], banks[0][:, :], sg[:, :], ALU.mult, [BK[0], sg], [Y[2 + j]])
                free(sg)
                yield
            free(gl)
            group_norm(1, 1)

        def chainY():
            slot = get_chunk(l, 2)
            for m in range(3):
                proj_fm(slot, m, 2 + m)
            MM([mm(banks[5][:, blk * 128:(blk + 1) * 128], hbuf[:, k, blk * 128:(blk + 1) * 128], ring[slot][:, k, 384:512], start=(k == 0), stop=(k == 7))
                for blk in range(4) for k in range(8)], [("ring", slot)] + HK, [BK[5]])
            release_chunk()
            kbuf, vbuf = d["kbuf"], d["vbuf"]
            KB, VB = ("kbuf", l), ("vbuf", l)
            qn = balloc(2)
            CP("act", vbuf[:, 1:5, :], banks[5][:, :].rearrange("p (b f) -> p b f", f=128), [BK[5]], [VB])
            yield
            for (bk, gcol, outv, okey) in ((2, SPC["qgs"], qn[:, 0:NT], qn), (3, SPC["qgs"], qn[:, NT:2 * NT], qn),
                                           (4, SPC["kg"], kbuf[:, 128:128 + NT], KB)):
                sq = balloc(); rs = falloc()
                ACT(sq[:, :], banks[bk][:, :], AF.Square, [BK[bk]], [sq])
                MM([mm(banks[6][:, :], bones_bf[:, :], sq[:, :])], [sq, "bones_bf"], [BK[6]])
                ACT(rs[:, :], banks[6][:, :], AF.Sqrt, [BK[6]], [rs], scale=1.0 / 64, bias=EPS)
                RECIP(rs[:, :], rs[:, :], [rs], [rs])
                STT(outv, banks[bk][:, :], sp[:, gcol:gcol + 1], rs[:, :], ALU.mult, ALU.mult, [BK[bk], SPK, rs], [okey])
                free(sq); free(rs)
                yield
            jb_list = list(range(1 if first else 0, 5))
            for jbi, jb in enumerate(jb_list):
                qlo, qhi = max(0, 2 * jb - 2), min(8, 2 * jb + 2)
                off = (qlo - (2 * jb - 2)) * 64
                ncol = (qhi - qlo) * 64
                for kh in range(2):
                    MM([mm(banks[2 + kh][:, r * 256 + off:r * 256 + off + ncol], kbuf[kh * 64:(kh + 1) * 64, jb * 128:(jb + 1) * 128],
                           qn[kh * 64:(kh + 1) * 64, r * NT + qlo * 64:r * NT + qhi * 64]) for r in range(2)], [KB, qn], [BK[2 + kh]])
                pT = balloc(2)
                P.op("pool", lambda e, pT=pT: e.memset(pT[:, :], 0.0), writes=[pT])
                for kh in range(2):
                    for half, (c0, c1) in enumerate(((0, 192), (64, 256))):
                        a, b2 = max(c0, off), min(c1, off + ncol)
                        if b2 <= a:
                            continue
                        src = banks[2 + kh][half * 64:(half + 1) * 64, :].rearrange("p (r c) -> p r c", c=256)[:, :, a:b2]
                        dst = pT[half * 64:(half + 1) * 64, kh * 512:(kh + 1) * 512].rearrange("p (r c) -> p r c", c=256)[:, :, a:b2]
                        ACT(dst, src, AF.Exp, [BK[2 + kh]], [pT])
                for r in range(2):
                    mms_n, mms_d = [], []
                    for kh in range(2):
                        pv = pT[:, kh * 512 + r * 256:kh * 512 + (r + 1) * 256]
                        for part in range(2):
                            pair = jb - 1 + part
                            if pair < 0 or pair > 3:
                                continue
                            first_contrib = (part == 1) or (first and jb == 1)
                            last_contrib = (part == 0) or (jb == 4)
                            if part == 1 and jb == 4:
                                continue
                            cols = slice(pair * 128, (pair + 1) * 128)
                            mms_n.append(mm(banks[4 + r][kh * 64:(kh + 1) * 64, cols], vbuf[:, jb, kh * 64:(kh + 1) * 64], pv[:, part * 128:(part + 1) * 128],
                                            start=first_contrib, stop=last_contrib))
                            mms_d.append(mm(banks[6 + r][kh * 64:(kh + 1) * 64, cols], ones_bf[:, 0:64], pv[:, part * 128:(part + 1) * 128],
                                            start=first_contrib, stop=last_contrib))
                    MM(mms_n, [pT, VB], [BK[4 + r]])
                    MM(mms_d, [pT, "ones_bf"], [BK[6 + r]])
                free(pT)
                yield
            for r in range(2):
                rec = falloc()
                TS("dve", rec[:, :], banks[6 + r][:, :], sp[:, SPC["esink"] + r:SPC["esink"] + r + 1], ALU.add, [BK[6 + r], SPK], [rec])
                RECIP(rec[:, :], rec[:, :], [rec], [rec])
                TT("dve", Y[4 + r][:, :], banks[4 + r][:, :], rec[:, :], ALU.mult, [BK[4 + r], rec], [Y[4 + r]])
                free(rec)
            free(qn)
            P.op("pool", lambda e: e.tensor_copy(out=kbuf[:, 0:128], in_=kbuf[:, NT:NT + 128]), [KB], [KB])
            P.op("pool", lambda e: e.tensor_copy(out=vbuf[:, 0, :], in_=vbuf[:, 4, :]), [VB], [VB])
            yield
            group_norm(2, 2)
            yield
            slot = get_chunk(l, 3)
            for m in range(4):
                proj_fm(slot, m, 2 + m)
            release_chunk()
            qt = balloc(2); qh = balloc(4); kt = balloc(2); kend = balloc(2)
            e3s = []
            for j in range(2):
                sig = falloc(); lg = falloc(); kk = falloc(); B = falloc(); e1 = falloc(); e2 = falloc(); e3 = falloc()
                ACT(sig[:, :], banks[4 + j][:, :], AF.Sigmoid, [BK[4 + j]], [sig])
                TS("dve", sig[:, :], sig[:, :], sp[:, SPC["oml"] + j:SPC["oml"] + j + 1], ALU.mult, [sig, SPK], [sig],
                   s2=sp[:, SPC["lb"] + j:SPC["lb"] + j + 1], op1=ALU.add)
                ACT(lg[:, :], sig[:, :], AF.Ln, [sig], [lg])
                TS("pool", kk[:, :], sig[:, :], -1.0, ALU.mult, [sig], [kk], s2=1.0, op1=ALU.add)
                P.op("dve", lambda e, B=B, lg=lg: e.tensor_tensor_scan(out=B[:, :], data0=cs("segm"), data1=lg[:, :], initial=0.0,
                                                                      op0=ALU.mult, op1=ALU.add), ["cst", lg], [B])
                yield
                ACT(e3[:, :], B[:, :], AF.Exp, [B], [e3])
                TT("dve", lg.v3(64), B.v3(64), bc(B.v3(64)[:, :, 31:32], [128, 8, 64]), ALU.subtract, [B], [lg])
                ACT(e1[:, :], lg[:, :], AF.Exp, [lg], [e1])
                ACT(e2[:, :], lg[:, :], AF.Exp, [lg], [e2], scale=-1.0)
                TT("dve", qt[:, j * NT:(j + 1) * NT], banks[2 + j][:, :], e1[:, :], ALU.mult, [BK[2 + j], e1], [qt])
                for hh in range(2):
                    STT(qh[:, (j * 2 + hh) * NT:(j * 2 + hh + 1) * NT], banks[2 + j][:, :], cs("mB%d" % hh), e3[:, :], ALU.mult, ALU.mult,
                        [BK[2 + j], e3, "cst"], [qh])
                TT("pool", kt[:, j * NT:(j + 1) * NT], kk[:, :], e2[:, :], ALU.mult, [kk, e2], [kt])
                TT("pool", kend[:, j * NT:(j + 1) * NT].rearrange("p (b t) -> p b t", t=64), kt[:, j * NT:(j + 1) * NT].rearrange("p (b t) -> p b t", t=64),
                   bc(e1.v3(64)[:, :, 63:64], [128, 8, 64]), ALU.mult, [kt, e1], [kend])
                e3s.append(e3)
                for b_ in (sig, lg, kk, B, e1, e2):
                    free(b_)
                yield
            b6bf = banks[6][:, :].bitcast(BF16)
            def tr_fn(e):
                ins = None
                for bp in range(4):
                    for j in range(2):
                        ins = e.transpose(out=b6bf[:, (bp * 2 + j) * 128:(bp * 2 + j + 1) * 128], in_=kend[:, j * NT + bp * 128:j * NT + (bp + 1) * 128],
                                          identity=ident_bf[:, :])
                return ins
            P.op("pe", tr_fn, [kend, "ident_bf"], [BK[6]])
            kendT = balloc(2)
            CP("act", kendT[:, :], b6bf, [BK[6]], [kendT])
            free(kend)
            yield
            slot = get_chunk(l, 4)
            hib = (7, 2)
            for half in range(2):
                MM([mm(banks[hib[half]][:, bq * 256:(bq + 1) * 256], hbuf[:, k, (half * 2 + bq) * 128:(half * 2 + bq + 1) * 128], ring[slot][:, k, 0:256],
                       start=(k == 0), stop=(k == 7)) for bq in range(2) for k in range(8)], [("ring", slot)] + HK, [BK[hib[half]]])
            for j in range(2):
                proj_fm(slot, 2 + j, 3 + j)
            release_chunk()
            vT = balloc(2)
            for half in range(2):
                CP("act", vT[:, half * NT:(half + 1) * NT], banks[hib[half]][:, :], [BK[hib[half]]], [vT])
            sgs = []
            for j in range(2):
                sg = falloc()
                ACT(sg[:, :], banks[3 + j][:, :], AF.Silu, [BK[3 + j]], [sg])
                sgs.append(sg)
            yield
            for hp in range(2):
                mms = []
                for j in range(2):
                    for b in range(8):
                        bp, half = b // 2, b % 2
                        mms.append(mm(banks[5 + hp][half * 64:(half + 1) * 64, j * 256 + bp * 64:j * 256 + (bp + 1) * 64],
                                      kt[hp * 64:(hp + 1) * 64, j * NT + b * 64:j * NT + (b + 1) * 64],
                                      qt[hp * 64:(hp + 1) * 64, j * NT + b * 64:j * NT + (b + 1) * 64]))
                MM(mms, [kt, qt], [BK[5 + hp]])
            for hp in range(2):
                for half in range(2):
                    rows = slice(half * 64, (half + 1) * 64)
                    dst = Amz[rows, :].rearrange("p (j hp bp hf t) -> p j hp bp hf t", j=2, hp=2, bp=4, hf=2, t=64)[:, :, hp, :, half, :]
                    src = banks[5 + hp][rows, :].rearrange("p (j bp t) -> p j bp t", j=2, bp=4)
                    o_c, _w = _CST_OFF["cmask"]
                    msk = bc(cst[rows, o_c:o_c + 64].unsqueeze(1).unsqueeze(1), [64, 2, 4, 64])
                    TT("dve", dst, src, msk, ALU.mult, [BK[5 + hp], "cst"], ["Amz"])
            free(kt); free(qt)
            yield
            ub = (7, 2)
            for half in range(2):
                mms = []
                for j in range(2):
                    for hh in range(2):
                        h = 2 * j + hh
                        for bp in range(4):
                            mms.append(mm(banks[ub[half]][hh * 64:(hh + 1) * 64, j * 256 + bp * 64:j * 256 + (bp + 1) * 64],
                                          kendT[half * 64:(half + 1) * 64, bp * 256 + h * 64:bp * 256 + (h + 1) * 64],
                                          vT[half * 64:(half + 1) * 64, bp * 256 + h * 64:bp * 256 + (h + 1) * 64]))
                MM(mms, [kendT, vT], [BK[ub[half]]])
            free(kendT)
            yield
            Sall = balloc(3)
            for j in range(2):
                Sj = d["S"][j]
                SKj = ("S", l, j)
                CP("act", Sall[:, j * 768:j * 768 + 64], Sj[:, :], [SKj], [Sall])
                for b in range(8):
                    bp, half = b // 2, b % 2
                    STT(Sj[:, :], Sj[:, :], e3s[j][:, b * 64 + 63:b * 64 + 64], banks[ub[half]][:, j * 256 + bp * 64:j * 256 + (bp + 1) * 64], ALU.mult, ALU.add,
                        [SKj, e3s[j], BK[ub[half]]], [SKj])
                    if b < 7:
                        CP("act", Sall[:, j * 768 + (b + 1) * 64:j * 768 + (b + 2) * 64], Sj[:, :], [SKj], [Sall])
                    if b % 2 == 1:
                        yield
            for e3 in e3s:
                free(e3)
            for j in range(2):
                mms = []
                for hh in range(2):
                    h = 2 * j + hh
                    for b in range(8):
                        bp = b // 2
                        o_ = banks[5 + j][hh * 64:(hh + 1) * 64, b * 64:(b + 1) * 64]
                        mms.append(mm(o_, Sall[:, j * 768 + b * 64:j * 768 + (b + 1) * 64],
                                      qh[:, (j * 2 + hh) * NT + b * 64:(j * 2 + hh) * NT + (b + 1) * 64], start=True, stop=False))
                        mms.append(mm(o_, vT[:, bp * 256 + h * 64:bp * 256 + (h + 1) * 64],
                                      Amz[:, (h * 8 + b) * 64:(h * 8 + b + 1) * 64], start=False, stop=True))
                MM(mms, [Sall, qh, vT, "Amz"], [BK[5 + j]])
            free(Sall); free(qh); free(vT)
            yield
            for j in range(2):
                sq = balloc(); rs = falloc()
                ACT(sq[:, :], banks[5 + j][:, :], AF.Square, [BK[5 + j]], [sq])
                MM([mm(banks[3][:, :], bones_bf[:, :], sq[:, :])], [sq, "bones_bf"], [BK[3]])
                ACT(rs[:, :], banks[3][:, :], AF.Sqrt, [BK[3]], [rs], scale=1.0 / 64, bias=EPS)
                RECIP(rs[:, :], rs[:, :], [rs], [rs])
                STT(rs[:, :], banks[5 + j][:, :], sp[:, SPC["og"]:SPC["og"] + 1], rs[:, :], ALU.mult, ALU.mult, [BK[5 + j], SPK, rs], [rs])
                TT("pool", Y[6 + j][:, :], rs[:, :], sgs[j][:, :], ALU.mult, [rs, sgs[j]], [Y[6 + j]])
                free(sq); free(rs); free(sgs[j])
                yield
            group_norm(3, 3)

        gens = [chainX(), chainY()]
        while gens:
            for g in list(gens):
                try:
                    next(g)
                except StopIteration:
                    gens.remove(g)
        CHK("hgrn")
        for c in range(2):
            slot = get_chunk(l, 5 + c)
            for m in range(4):
                MM([mm(banks[m][:, :], ring[slot][:, k, m * 128:(m + 1) * 128], YN[k][:, :], start=(k == 0), stop=(k == 7)) for k in range(8)],
                   [("ring", slot)] + YN, [BK[m]])
            release_chunk()
            for m in range(4):
                k = c * 4 + m
                TT("dve", xs[:, k, :], xs[:, k, :], banks[m][:, :], ALU.add, [("xs", k), BK[m]], [("xs", k)])
        for y_ in YN:
            free(y_)
        if "xmid" in tap_out and (s, ti) == taps.get("_ysel_tile", (0, 0)):
            for k in range(8):
                TAP("xmid", (l, k), xs[:, k, :], [("xs", k)])

        CHK("gn")
        rmsnorm_to_h(l, SPC["gffn"])
        hid = balloc(32)
        HIDK = hid.keys_
        for c in range(8):
            slot = get_chunk(l, 7 + c)
            for m in range(4):
                bk = (c * 4 + m) % 4
                proj_fm(slot, m, bk)
                r_ = falloc()
                ACT(r_[:, :], banks[bk][:, :], AF.Relu, [BK[bk]], [r_])
                idx = c * 4 + m
                TT("pool", hid[:, idx * NT:(idx + 1) * NT], r_[:, :], r_[:, :], ALU.mult, [r_], [HIDK[idx]])
                free(r_)
            release_chunk()
        for cg in range(2):
            for kg in range(4):
                slot = get_chunk(l, 15 + cg * 4 + kg)
                for m in range(4):
                    MM([mm(banks[4 + m][:, :], ring[slot][:, k, m * 128:(m + 1) * 128], hid[:, (kg * 8 + k) * NT:(kg * 8 + k + 1) * NT],
                           start=(kg == 0 and k == 0), stop=(kg == 3 and k == 7)) for k in range(8)],
                       [("ring", slot)] + HIDK[kg * 8:(kg + 1) * 8], [BK[4 + m]])
                release_chunk()
            for m in range(4):
                k = cg * 4 + m
                TT("dve", xs[:, k, :], xs[:, k, :], banks[4 + m][:, :], ALU.add, [("xs", k), BK[4 + m]], [("xs", k)])
        free(hid)
        if "xout" in tap_out and (s, ti) == taps.get("_ysel_tile", (0, 0)):
            for k in range(8):
                TAP("xout", (l, k), xs[:, k, :], [("xs", k)])
        if l == layers[-1]:
            outs = []
            for k in range(8):
                outs.append(P.dma("sp", oT[s, k * 128:(k + 1) * 128, t0:t0 + NT], xs[:, k, :], reads=[("xs", k)], writes=[("oT", s, ti, k)], chan=("out", k)))
            return outs
        return []

    all_out = []
    try:
        CHK("prologue")
        for (s, ti, l) in tile_list:
            all_out += layer_tile(s, ti, l)
    except StopBuild:
        pass
    P.emit(final_deps=all_out + tap_dmas)
    return nc, P


_CACHE = {}


def prepare_inputs(inputs):
    wch, glu = _build_weights(inputs)
    prm = np.stack([_build_params(inputs, l) for l in range(DEPTH)], axis=0)
    cst = _build_consts()
    x = np.asarray(inputs["x"])
    in_maps = []
    for c in range(NCORES):
        xc = x[c * SEQ_PER_CORE:(c + 1) * SEQ_PER_CORE]
        xT = np.ascontiguousarray(xc.transpose(0, 2, 1))
        in_maps.append({"xT": xT, "wch": wch, "glu": glu, "prm": prm, "cst": cst})
    return in_maps


def kernel(**inputs):
    in_maps = prepare_inputs(inputs)
    if "nc" not in _CACHE:
        _CACHE["nc"] = build_program()[0]
    res = run_bass_kernel_spmd(_CACHE["nc"], in_maps, core_ids=list(range(NCORES)))
    outs = []
    for c in range(NCORES):
        oT = res.results[c]["oT"]
        outs.append(np.ascontiguousarray(oT.transpose(0, 2, 1)))
    return np.concatenate(outs, axis=0).astype(np.float32)
```

```python
import contextlib
import numpy as np
import concourse.bass as bass
import concourse.mybir as mybir
from concourse.bass_utils import run_bass_kernel_spmd

F32 = mybir.dt.float32
BF16 = mybir.dt.bfloat16
I32 = mybir.dt.int32
ALU = mybir.AluOpType
AF = mybir.ActivationFunctionType
AX = mybir.AxisListType

NCORES = 8
SEQ_PER_CORE = 4
SEQ = 2048
NT = 512
TILES_PER_SEQ = SEQ // NT
DM = 1024
DEPTH = 2
NCHUNK = 23
EPS = 1e-6
TS5 = 8
NC5 = NT // TS5
TWO_PI = 6.283185307179586

ENGS = ("pe", "act", "dve", "pool", "sp")
SEM_CAP = 1000


class Op:
    __slots__ = ("eng", "fn", "deps", "is_dma", "chan", "needs_inc", "sem", "val", "all_deps", "cost", "seq", "dma_us")

    def __init__(self, eng, fn, is_dma=False, chan=None):
        self.eng = eng
        self.fn = fn
        self.deps = []
        self.is_dma = is_dma
        self.chan = chan
        self.needs_inc = is_dma
        self.sem = None
        self.val = None
        self.all_deps = []
        self.cost = 0.3
        self.dma_us = 0.0
        self.seq = 0


def _keys(items):
    out = []
    for it in items:
        if hasattr(it, "keys_"):
            out.extend(it.keys_)
        else:
            out.append(it)
    return out


class Prog:
    def __init__(self, nc, same_engine_sync=True):
        self.nc = nc
        self.ops = {e: [] for e in ENGS}
        self.last_writer = {}
        self.readers = {}
        self.same_engine_sync = same_engine_sync
        self.nops = 0

    def schedule(self):
        import heapq
        allops = [o for e in ENGS for o in self.ops[e]]
        allops.sort(key=lambda o: o.seq)
        ndep = {}
        succ = {}
        for o in allops:
            ndep[id(o)] = len(o.all_deps)
            for d in o.all_deps:
                succ.setdefault(id(d), []).append(o)
        ready_t = {id(o): 0.0 for o in allops}
        finish = {}
        eng_free = {e: 0.0 for e in ENGS}
        heap = []
        for o in allops:
            if ndep[id(o)] == 0:
                heapq.heappush(heap, (0.0, o.seq, o))
        new_order = {e: [] for e in ENGS}
        nsched = 0
        while heap:
            key, _, o = heapq.heappop(heap)
            start = max(eng_free[o.eng], ready_t[id(o)])
            if start > key + 1e-9:
                heapq.heappush(heap, (start, o.seq, o))
                continue
            if o.is_dma:
                eng_free[o.eng] = start + 0.06
                fin = start + 2.0 + o.dma_us
            else:
                eng_free[o.eng] = start + o.cost
                fin = start + o.cost
            finish[id(o)] = fin
            new_order[o.eng].append(o)
            nsched += 1
            for c in succ.get(id(o), ()):
                lat = 0.06 if (c.eng == o.eng and not o.is_dma) else 0.25
                ready_t[id(c)] = max(ready_t[id(c)], fin + lat)
                ndep[id(c)] -= 1
                if ndep[id(c)] == 0:
                    heapq.heappush(heap, (ready_t[id(c)], c.seq, c))
        assert nsched == len(allops), (nsched, len(allops))
        self.ops = new_order
        self.est_us = max(finish.values()) if finish else 0.0

    def op(self, eng, fn, reads=(), writes=(), is_dma=False, chan=None, force=False, cost=0.3, dma_us=0.0):
        reads = _keys(reads)
        writes = _keys(writes)
        bank_reads = [k for k in reads if isinstance(k, tuple) and k and k[0] == "bank"]
        if bank_reads:
            reads = [k for k in reads if k not in bank_reads]
            writes = list(writes) + bank_reads
        o = Op(eng, fn, is_dma, chan)
        o.cost = cost
        o.dma_us = dma_us
        o.seq = self.nops
        self.nops += 1
        deps = {}
        for k in reads:
            w = self.last_writer.get(k)
            if w is not None:
                deps[id(w)] = w
        for k in writes:
            w = self.last_writer.get(k)
            if w is not None:
                deps[id(w)] = w
            for r in self.readers.get(k, ()):
                deps[id(r)] = r
        o.all_deps = list(deps.values())
        for d in deps.values():
            if (not d.is_dma) and d.eng == eng and ((eng == "pe" and not force) or not self.same_engine_sync):
                continue
            d.needs_inc = True
            o.deps.append(d)
        for k in reads:
            self.readers.setdefault(k, []).append(o)
        for k in writes:
            self.last_writer[k] = o
            self.readers[k] = []
        self.ops[eng].append(o)
        return o

    def dma(self, eng, out, in_, reads=(), writes=(), chan=None, **kw):
        nbytes = 1
        for d_ in out.shape:
            nbytes *= d_
        nbytes *= mybir.dt.size(out.dtype)
        return self.op(eng, lambda e: e.dma_start(out=out, in_=in_, **kw), reads, list(writes) + [("chan", chan)], is_dma=True, chan=chan,
                       dma_us=nbytes / 150e3)

    def emit(self, final_deps=()):
        nc = self.nc
        stack = contextlib.ExitStack()
        eng_sems = {e: [] for e in ENGS}
        eng_cnt = {e: 0 for e in ENGS}
        chan_sem, chan_cnt = {}, {}
        nsem = 0
        for e in ENGS:
            for o in self.ops[e]:
                if o.is_dma:
                    if o.chan not in chan_sem or chan_cnt[o.chan] + 16 > SEM_CAP:
                        chan_sem[o.chan] = stack.enter_context(nc.semaphore(f"c{nsem}"))
                        nsem += 1
                        chan_cnt[o.chan] = 0
                    chan_cnt[o.chan] += 16
                    o.sem, o.val = chan_sem[o.chan], chan_cnt[o.chan]
                elif o.needs_inc:
                    if eng_cnt[e] % SEM_CAP == 0:
                        eng_sems[e].append(stack.enter_context(nc.semaphore(f"e{nsem}")))
                        nsem += 1
                    eng_cnt[e] += 1
                    o.sem, o.val = eng_sems[e][-1], (eng_cnt[e] - 1) % SEM_CAP + 1
        self.nsem = nsem
        final_deps = list(final_deps)

        def run_engine(ename, eng):
            waited = {}

            def wait_for(d):
                key = id(d.sem)
                if waited.get(key, 0) >= d.val:
                    return
                eng.wait_ge(d.sem, d.val)
                waited[key] = d.val

            for o in self.ops[ename]:
                for d in o.deps:
                    wait_for(d)
                ins = o.fn(eng)
                if o.needs_inc:
                    ins.then_inc(o.sem, 16 if o.is_dma else 1)
            if ename == "sp":
                for d in final_deps:
                    wait_for(d)

        with nc.Block() as block:
            @block.tensor
            def _(e):
                run_engine("pe", e)

            @block.scalar
            def _(e):
                run_engine("act", e)

            @block.vector
            def _(e):
                run_engine("dve", e)

            @block.gpsimd
            def _(e):
                run_engine("pool", e)

            @block.sync
            def _(e):
                run_engine("sp", e)
        stack.close()


def _col8(v):
    return np.ascontiguousarray(v.reshape(8, 128).T)


def _col2(v):
    return np.ascontiguousarray(v.reshape(2, 128).T)


def _qperm():
    idx = np.zeros(256, np.int64)
    for r in range(2):
        for kh in range(2):
            for d in range(64):
                idx[r * 128 + kh * 64 + d] = (kh * 2 + r) * 64 + d
    return idx


_PRM_FIELDS = [
    ("gmix", 8), ("gffn", 8), ("ggn", 8), ("convw", 6), ("s5d", 2), ("qg", 1), ("kg", 1), ("sink", 2),
    ("hlb0", 2), ("hlb1", 2), ("og", 1),
    ("A_lre", 128), ("A_lim", 128), ("A_ldt", 2), ("A_bre", 128), ("A_bim", 128),
    ("A_cre", 2048), ("A_cim", 2048),
    ("B_lre", 8), ("B_lim", 8), ("B_ldt", 8), ("B_cre", 128), ("B_cim", 128),
]
_PRM_OFF = {}
_o = 0
for _n, _w in _PRM_FIELDS:
    _PRM_OFF[_n] = (_o, _w)
    _o += _w
NPRM = _o

_CST_FIELDS = [
    ("ident", 128), ("bones", 128),
    ("mgp0", 1), ("mgp1", 1), ("mB0", 1), ("mB1", 1),
    ("mg8", 8), ("tauA", 8), ("tauB", 8), ("cidx", 64),
    ("segm", 512), ("cmask", 64),
]
_CST_OFF = {}
_o = 0
for _n, _w in _CST_FIELDS:
    _CST_OFF[_n] = (_o, _w)
    _o += _w
NCST = _o


def _build_consts():
    c = np.zeros((128, NCST), np.float32)

    def put(name, arr):
        o, w = _CST_OFF[name]
        c[:, o:o + w] = np.asarray(arr, np.float32).reshape(128, w)

    p = np.arange(128)
    put("ident", np.eye(128))
    bo = np.zeros((128, 128))
    bo[:64, :64] = 1
    bo[64:, 64:] = 1
    put("bones", bo)
    put("mgp0", ((p // 16) % 2 == 0)[:, None])
    put("mgp1", ((p // 16) % 2 == 1)[:, None])
    put("mB0", (p < 64)[:, None])
    put("mB1", (p >= 64)[:, None])
    put("mg8", (p[:, None] // 16) == np.arange(8)[None, :])
    put("tauA", np.tile(np.arange(8.0)[None], (128, 1)))
    put("tauB", np.tile(np.arange(1.0, 9.0)[None], (128, 1)))
    put("cidx", np.tile(np.arange(1.0, 65.0)[None], (128, 1)))
    seg = np.ones((8, 64))
    seg[:, 0] = 0
    put("segm", np.tile(seg.reshape(1, 512), (128, 1)))
    s = (p % 64)[:, None]
    t = np.arange(64)[None, :]
    put("cmask", (s <= t))
    return c


def _build_params(inp, l):
    prm = np.zeros((128, NPRM), np.float32)

    def put(name, arr):
        o, w = _PRM_OFF[name]
        prm[:, o:o + w] = np.asarray(arr, np.float32).reshape(128, w)

    qp = _qperm()
    put("gmix", _col8(inp["norm_mix"][l]))
    put("gffn", _col8(inp["norm_ffn"][l]))
    gn = np.array(inp["group_norm"][l])
    gn[512:768] = np.array(inp["group_norm"][l])[512:768][qp]
    put("ggn", _col8(gn))
    cw = np.asarray(inp["conv_w"][l])
    put("convw", np.stack([_col2(cw[i]) for i in range(3)], axis=2).reshape(128, 6))
    put("s5d", _col2(np.asarray(inp["s5_d"][l])))
    put("qg", np.tile(np.asarray(inp["attn_q_norm"][l]), 2)[:, None])
    put("kg", np.tile(np.asarray(inp["attn_k_norm"][l]), 2)[:, None])
    sk = np.asarray(inp["attn_sinks"][l])
    put("sink", np.stack([np.repeat(sk[[0 * 2 + r, 1 * 2 + r]], 64) for r in range(2)], axis=1))
    put("hlb0", _col2(np.asarray(inp["hg_lower_bounds"][0])))
    put("hlb1", _col2(np.asarray(inp["hg_lower_bounds"][1])))
    put("og", np.tile(np.asarray(inp["hg_out_norm"][l]), 2)[:, None])
    lre, lim, ldt = (np.asarray(inp[k][l]) for k in ("s5_lam_re", "s5_lam_im", "s5_log_dt"))
    bre, bim = np.asarray(inp["s5_b_re"][l]), np.asarray(inp["s5_b_im"][l])
    cre, cim = np.asarray(inp["s5_c_re"][l]), np.asarray(inp["s5_c_im"][l])
    put("A_lre", np.repeat(lre.reshape(2, 8, 1, 64), 16, axis=2).transpose(1, 2, 0, 3).reshape(128, 128))
    put("A_lim", np.repeat(lim.reshape(2, 8, 1, 64), 16, axis=2).transpose(1, 2, 0, 3).reshape(128, 128))
    put("A_ldt", np.repeat(ldt.reshape(2, 8, 1), 16, axis=2).transpose(1, 2, 0).reshape(128, 2))
    put("A_bre", bre.reshape(2, 8, 64, 16).transpose(1, 3, 0, 2).reshape(128, 128))
    put("A_bim", bim.reshape(2, 8, 64, 16).transpose(1, 3, 0, 2).reshape(128, 128))
    put("A_cre", np.repeat(cre.reshape(2, 8, 1, 16, 64), 16, axis=2).transpose(1, 2, 0, 3, 4).reshape(128, 2048))
    put("A_cim", np.repeat(cim.reshape(2, 8, 1, 16, 64), 16, axis=2).transpose(1, 2, 0, 3, 4).reshape(128, 2048))
    put("B_lre", lre.reshape(8, 2, 64).transpose(1, 2, 0).reshape(128, 8))
    put("B_lim", lim.reshape(8, 2, 64).transpose(1, 2, 0).reshape(128, 8))
    put("B_ldt", np.repeat(ldt.reshape(8, 2, 1), 64, axis=2).transpose(1, 2, 0).reshape(128, 8))
    put("B_cre", cre.reshape(8, 2, 16, 64).transpose(1, 3, 0, 2).reshape(128, 128))
    put("B_cim", cim.reshape(8, 2, 16, 64).transpose(1, 3, 0, 2).reshape(128, 128))
    return prm


def _chunks_kn(W):
    K, N = W.shape
    out = []
    for cg in range(N // 512):
        for kg in range(K // 1024):
            blk = W[kg * 1024:(kg + 1) * 1024, cg * 512:(cg + 1) * 512]
            out.append(np.ascontiguousarray(blk.reshape(8, 128, 512).transpose(1, 0, 2)).reshape(128, 4096))
    return out


def _build_weights(inp):
    qp = _qperm()
    chunks = []
    for l in range(DEPTH):
        w_in = np.asarray(inp["w_in"][l])
        win = np.array(w_in)
        win[:, 1024:1280] = w_in[:, 1024:1280][:, qp]
        w_out = np.asarray(inp["w_out"][l])
        wout = np.array(w_out)
        wout[512:768, :] = w_out[512:768, :][qp, :]
        chunks += _chunks_kn(win)
        chunks += _chunks_kn(wout)
        chunks += _chunks_kn(np.asarray(inp["w_ff1"][l]))
        chunks += _chunks_kn(np.asarray(inp["w_ff2"][l]))
    wch = np.stack(chunks, axis=0).astype(np.float32)
    glu = np.stack([np.asarray(inp["s5_w_glu"][l]).reshape(2, 128, 512).transpose(1, 0, 2) for l in range(DEPTH)], axis=0)
    return wch, np.ascontiguousarray(glu, np.float32)


class Buf:
    def __init__(self, ap2d, keys):
        self.a = ap2d
        self.keys_ = keys

    def __getitem__(self, k):
        return self.a[k]

    def v3(self, b):
        return self.a.rearrange("p (a b) -> p a b", b=b)


def bc(ap, shape):
    return ap.broadcast_to(list(shape))


def build_program(n_seq=SEQ_PER_CORE, n_tiles=TILES_PER_SEQ, layers=(0, 1), taps=None, same_engine_sync=True,
                  stop_after=None, skip_prologue=False, list_schedule=True):
    taps = taps or {}

    class StopBuild(Exception):
        pass

    def CHK(name):
        if stop_after == name:
            raise StopBuild()
    nc = bass.Bass("TRN2", target_bir_lowering=False)
    P = Prog(nc, same_engine_sync=same_engine_sync)
    NL = DEPTH

    xT = nc.dram_tensor("xT", [SEQ_PER_CORE, DM, SEQ], F32, kind="ExternalInput").ap()
    wch = nc.dram_tensor("wch", [NL * NCHUNK, 128, 4096], F32, kind="ExternalInput").ap()
    glu_d = nc.dram_tensor("glu", [NL, 128, 2, 512], F32, kind="ExternalInput").ap()
    prm_d = nc.dram_tensor("prm", [NL, 128, NPRM], F32, kind="ExternalInput").ap()
    cst_d = nc.dram_tensor("cst", [128, NCST], F32, kind="ExternalInput").ap()
    oT = nc.dram_tensor("oT", [SEQ_PER_CORE, DM, SEQ], F32, kind="ExternalOutput").ap()
    wsc = nc.dram_tensor("wsc", [NL * NCHUNK, 128, 4096], BF16).ap()
    tap_out = {}
    for name, shape in taps.items():
        if name.startswith("_"):
            continue
        tap_out[name] = nc.dram_tensor("tap_" + name, list(shape), F32, kind="ExternalOutput").ap()
    s5m_d = nc.dram_tensor("s5m_d", [NL, 128, 10240], BF16).ap()
    tap_dmas = []

    def sb(name, shape, dt=F32):
        return nc.alloc_sbuf_tensor("s_" + name, list(shape), dt)

    NRING = 4
    ring = [sb(f"ring{i}", [128, 8, 512], BF16) for i in range(NRING)]
    xs = sb("xs", [128, 8, NT], F32)
    hbuf = sb("hbuf", [128, 8, NT], BF16)
    cst = sb("cst", [128, NCST], F32)
    NF, NB = 23, 32
    farena = sb("farena", [128, NF * NT], F32)
    barena = sb("barena", [128, NB * NT], BF16)
    fmap = [False] * NF
    bmap = [False] * NB

    def _alloc(arena, amap, tag, n):
        for i in range(len(amap) - n + 1):
            if not any(amap[i:i + n]):
                for j in range(i, i + n):
                    amap[j] = True
                b = Buf(arena[:, i * NT:(i + n) * NT], [(tag, j) for j in range(i, i + n)])
                b.rng = (i, n)
                return b
        raise RuntimeError(f"arena {tag} exhausted")

    def falloc(n=1):
        return _alloc(farena, fmap, "fp", n)

    def balloc(n=1):
        return _alloc(barena, bmap, "bp", n)

    def free(b):
        i, n = b.rng
        amap = fmap if b.keys_[0][0] == "fp" else bmap
        for j in range(i, i + n):
            assert amap[j]
            amap[j] = False

    banks = [nc.alloc_psum_tensor(f"bank{i}", [128, 512], F32) for i in range(8)]
    BK = [("bank", i) for i in range(8)]

    def cs(name):
        o, w = _CST_OFF[name]
        return cst[:, o:o + w]

    ident_bf = sb("ident_bf", [128, 128], BF16)
    bones_bf = sb("bones_bf", [128, 128], BF16)
    ones_bf = sb("ones_bf", [128, 128], BF16)
    sb_int = sb("sb_int", [128, 512], I32)
    Amz = sb("Amz", [128, 2048], BF16)

    s5m = sb("s5m", [128, 10240], BF16)
    L1v = s5m[:, 0:4096].rearrange("p (j r t c) -> p j r t c", j=2, r=2, t=8)
    L3v = s5m[:, 4096:8192].rearrange("p (r q t c) -> p r q t c", r=2, q=8, t=8)
    KLv = s5m[:, 8192:10240].rearrange("p (j t c) -> p j t c", j=2, t=8)
    L = []
    for l in range(NL):
        d = dict(
            sp=sb(f"sp{l}", [128, 48], F32),
            glu=sb(f"glu{l}", [128, 2, 512], BF16),
            L1=L1v,
            L3=L3v,
            KL=KLv,
            cosR=sb(f"cosR{l}", [128, 512], F32),
            sinR=sb(f"sinR{l}", [128, 512], F32),
            rtab=sb(f"rtab{l}", [128, 512], F32),
            r8=sb(f"r8_{l}", [128, 8], F32),
            zbuf=sb(f"zbuf{l}", [128, 2, NT + 2], F32),
            Er=sb(f"Er{l}", [128, 8, NC5 + 1], F32),
            Ei=sb(f"Ei{l}", [128, 8, NC5 + 1], F32),
            kbuf=sb(f"kbuf{l}", [128, 128 + NT], BF16),
            vbuf=sb(f"vbuf{l}", [128, 5, 128], BF16),
            S=[sb(f"S{l}_{j}", [128, 64], F32) for j in range(2)],
        )
        L.append(d)
    SPC = dict(gmix=0, gffn=8, ggn=16, convw=24, s5d=30, qgs=32, kg=33, esink=34, lb=36, oml=38, og=40)

    def _nfree(ap):
        n = 1
        for d_ in ap.shape[1:]:
            n *= d_
        return n

    def _cost(eng, ap):
        n = _nfree(ap)
        if eng == "act":
            return 0.15 + n * 0.00083
        if eng == "pool":
            return 0.2 + n * 0.0021
        return 0.12 + n * 0.00104

    def ACT(out, in_, func, reads, writes, scale=1.0, bias=None):
        if bias is None:
            return P.op("act", lambda e: e.activation(out=out, in_=in_, func=func, scale=scale), reads, writes, cost=_cost("act", out))
        return P.op("act", lambda e: e.activation(out=out, in_=in_, func=func, scale=scale, bias=bias), reads, writes, cost=_cost("act", out))

    def TT(eng, out, a, b, op, reads, writes):
        return P.op(eng, lambda e: e.tensor_tensor(out=out, in0=a, in1=b, op=op), reads, writes, cost=_cost(eng, out))

    def TS(eng, out, a, s1, op0, reads, writes, s2=None, op1=None):
        if op1 is None:
            return P.op(eng, lambda e: e.tensor_scalar(out=out, in0=a, scalar1=s1, scalar2=None, op0=op0), reads, writes, cost=_cost(eng, out))
        return P.op(eng, lambda e: e.tensor_scalar(out=out, in0=a, scalar1=s1, scalar2=s2, op0=op0, op1=op1), reads, writes, cost=_cost(eng, out))

    def STT(out, in0, scalar, in1, op0, op1, reads, writes):
        return P.op("dve", lambda e: e.scalar_tensor_tensor(out=out, in0=in0, scalar=scalar, in1=in1, op0=op0, op1=op1), reads, writes,
                    cost=_cost("dve", out))

    def CP(eng, out, in_, reads, writes):
        if eng == "act":
            return P.op("act", lambda e: e.activation(out=out, in_=in_, func=AF.Copy), reads, writes, cost=_cost("act", out))
        return P.op(eng, lambda e: e.tensor_copy(out=out, in_=in_), reads, writes, cost=_cost(eng, out))

    def RECIP(out, in_, reads, writes):
        return P.op("dve", lambda e: e.reciprocal(out=out, in_=in_), reads, writes, cost=_cost("dve", out))

    def MM(mms, reads, writes, force=False):
        def fn(e):
            ins = None
            for m in mms:
                if m.get("tp") is not None:
                    ins = e.matmul(m["out"], lhsT=m["lhsT"], rhs=m["rhs"], start=m["start"], stop=m["stop"], skip_group_check=True,
                                   tile_position=m["tp"])
                else:
                    ins = e.matmul(m["out"], lhsT=m["lhsT"], rhs=m["rhs"], start=m["start"], stop=m["stop"], skip_group_check=True)
            return ins
        c_ = 0.0
        for m in mms:
            c_ += max(_nfree(m["rhs"]), 64) / 2400.0 + 0.045
        return P.op("pe", fn, reads, writes, force=force, cost=c_)

    def mm(out, lhsT, rhs, start=True, stop=True, tp=None):
        return dict(out=out, lhsT=lhsT, rhs=rhs, start=start, stop=stop, tp=tp)

    def TAP(name, idx, src_ap, reads):
        if name in tap_out:
            o = P.dma("sp", tap_out[name][idx], src_ap, reads=reads, writes=[("tap", name, idx)], chan=("tap", name))
            tap_dmas.append(o)

    P.dma("sp", cst[:], cst_d, writes=["cst"], chan="cst")
    CP("dve", ident_bf[:], cs("ident"), ["cst"], ["ident_bf"])
    CP("dve", bones_bf[:], cs("bones"), ["cst"], ["bones_bf"])
    P.op("pool", lambda e: e.memset(ones_bf[:], 1.0), writes=["ones_bf"])
    P.op("pool", lambda e: e.memset(Amz[:, :], 0.0), writes=["Amz"])

    for l in layers:
        for c in range(NCHUNK):
            ci = l * NCHUNK + c
            P.dma("pool", wsc[ci].rearrange("p (a b) -> p a b", b=2048), wch[ci].rearrange("p (a b) -> p a b", b=2048),
                  writes=[("wsc", ci)], chan=("wcast", ci % 4))

    prm_sb = falloc(2)
    _ac_lo = _PRM_OFF["A_cre"][0]
    _ac_hi = _PRM_OFF["A_cim"][0] + 2048
    NSMALL = NPRM - 4096
    assert NSMALL <= 2 * NT

    def prologue_layer(l):
        d = L[l]
        sp = d["sp"]
        SPK = ("sp", l)
        P.dma("sp", prm_sb[:, 0:_ac_lo], prm_d[l, :, 0:_ac_lo], writes=[prm_sb], chan="prm")
        P.dma("sp", prm_sb[:, _ac_lo:NSMALL], prm_d[l, :, _ac_hi:NPRM], writes=[prm_sb], chan="prm2")

        def pf(name, a=0, b=None):
            o, w = _PRM_OFF[name]
            if o >= _ac_hi:
                o -= 4096
            b = w if b is None else b
            return prm_sb[:, o + a:o + b]

        RD = [prm_sb, "cst"]
        for nm in ("gmix", "gffn", "ggn", "convw", "s5d", "kg", "og"):
            w = _PRM_OFF[nm][1]
            CP("dve", sp[:, SPC[nm]:SPC[nm] + w], pf(nm), RD, [SPK])
        TS("dve", sp[:, SPC["qgs"]:SPC["qgs"] + 1], pf("qg"), 0.125, ALU.mult, RD, [SPK])
        ACT(sp[:, SPC["esink"]:SPC["esink"] + 2], pf("sink"), AF.Exp, RD, [SPK])
        if l == 0:
            P.op("pool", lambda e: e.memset(sp[:, SPC["lb"]:SPC["lb"] + 2], 0.0), writes=[SPK])
        else:
            tmp = falloc()
            TT("dve", tmp[:, 0:2], pf("hlb1"), pf("hlb0"), ALU.subtract, RD, [tmp])
            ACT(sp[:, SPC["lb"]:SPC["lb"] + 2], tmp[:, 0:2], AF.Sigmoid, [tmp], [SPK])
            free(tmp)
        TS("dve", sp[:, SPC["oml"]:SPC["oml"] + 2], sp[:, SPC["lb"]:SPC["lb"] + 2], -1.0, ALU.mult, [SPK], [SPK], s2=1.0, op1=ALU.add)
        gl32 = falloc(2)
        P.dma("sp", gl32[:, 0:1024].rearrange("p (k n) -> p k n", n=512), glu_d[l], writes=[gl32], chan="glu")
        CP("dve", d["glu"][:, 0, :], gl32[:, 0:512], [gl32], [("glu", l)])
        CP("dve", d["glu"][:, 1, :], gl32[:, 512:1024], [gl32], [("glu", l)])
        free(gl32)

        def trig(u, n, cos_out, sin_out, ub, cb, sbk, shape3=None):
            t1 = falloc(); t2 = falloc()
            for (shift, outv, ob) in ((0.0, sin_out, sbk), (0.25, cos_out, cb)):
                TS("dve", t1[:, 0:n], u, shift + 64.0, ALU.add, [ub], [t1])
                CP("dve", sb_int[:, 0:n], t1[:, 0:n], [t1], ["sb_int"])
                CP("dve", t2[:, 0:n], sb_int[:, 0:n], ["sb_int"], [t2])
                TT("dve", t1[:, 0:n], t1[:, 0:n], t2[:, 0:n], ALU.subtract, [t1, t2], [t1])
                TS("dve", t2[:, 0:n], t1[:, 0:n], 0.5, ALU.is_gt, [t1], [t2])
                TT("dve", t1[:, 0:n], t1[:, 0:n], t2[:, 0:n], ALU.subtract, [t1, t2], [t1])
                TS("dve", t2[:, 0:n], t1[:, 0:n], -0.5, ALU.is_lt, [t1], [t2])
                TT("dve", t1[:, 0:n], t1[:, 0:n], t2[:, 0:n], ALU.add, [t1, t2], [t1])
                ACT(outv, t1[:, 0:n], AF.Sin, [t1], [ob], scale=TWO_PI)
            free(t1); free(t2)
            return None

        a_lr = falloc(); a_dt = falloc(); a_e1 = falloc(); a_th = falloc()
        TS("dve", a_lr[:, 0:128], pf("A_lre"), -1e-4, ALU.min, RD, [a_lr])
        ACT(a_dt[:, 0:2], pf("A_ldt"), AF.Exp, RD, [a_dt])
        for j in range(2):
            sl = slice(j * 64, (j + 1) * 64)
            TS("dve", a_e1[:, sl], a_lr[:, sl], a_dt[:, j:j + 1], ALU.mult, [a_lr, a_dt], [a_e1])
            TS("dve", a_th[:, sl], pf("A_lim")[:, sl], a_dt[:, j:j + 1], ALU.mult, RD + [a_dt], [a_th], s2=1.0 / TWO_PI, op1=ALU.mult)
        for j in range(2):
            sl = slice(j * 64, (j + 1) * 64)
            u = falloc(); me = falloc(); cc = falloc(); ss = falloc(); mr = falloc(); mi = falloc()
            u3, me3 = u.v3(64), me.v3(64)
            tau_b = bc(cs("tauA").unsqueeze(2), [128, 8, 64])
            TT("dve", u3, bc(a_th[:, sl].unsqueeze(1), [128, 8, 64]), tau_b, ALU.mult, [a_th, "cst"], [u])
            TT("dve", me3, bc(a_e1[:, sl].unsqueeze(1), [128, 8, 64]), tau_b, ALU.mult, [a_e1, "cst"], [me])
            ACT(me[:, :], me[:, :], AF.Exp, [me], [me])
            trig(u[:, :], 512, cc[:, :], ss[:, :], u, cc, ss)
            TT("dve", mr[:, :], me[:, :], cc[:, :], ALU.mult, [me, cc], [mr])
            TT("dve", mi[:, :], me[:, :], ss[:, :], ALU.mult, [me, ss], [mi])
            free(u); free(me); free(cc); free(ss)
            w = falloc()
            W = lambda i: w[:, i * 64:(i + 1) * 64]
            lr_, li_ = a_lr[:, sl], pf("A_lim")[:, sl]
            TT("dve", W(0), lr_, lr_, ALU.mult, [a_lr], [w])
            TT("dve", W(2), li_, li_, ALU.mult, RD, [w])
            TT("dve", W(0), W(0), W(2), ALU.add, [w], [w])
            RECIP(W(0), W(0), [w], [w])
            TS("dve", W(1), mr[:, 64:128], -1.0, ALU.add, [mr], [w])
            TT("dve", W(2), W(1), lr_, ALU.mult, [w, a_lr], [w])
            TT("dve", W(7), mi[:, 64:128], li_, ALU.mult, [mi] + RD, [w])
            TT("dve", W(2), W(2), W(7), ALU.add, [w], [w])
            TT("dve", W(3), W(2), W(0), ALU.mult, [w], [w])
            TT("dve", W(2), mi[:, 64:128], lr_, ALU.mult, [mi, a_lr], [w])
            TT("dve", W(7), W(1), li_, ALU.mult, [w] + RD, [w])
            TT("dve", W(2), W(2), W(7), ALU.subtract, [w], [w])
            TT("dve", W(4), W(2), W(0), ALU.mult, [w], [w])
            bre_, bim_ = pf("A_bre")[:, sl], pf("A_bim")[:, sl]
            TT("dve", W(5), W(3), bre_, ALU.mult, [w] + RD, [w])
            TT("dve", W(7), W(4), bim_, ALU.mult, [w] + RD, [w])
            TT("dve", W(5), W(5), W(7), ALU.subtract, [w], [w])
            TT("dve", W(6), W(3), bim_, ALU.mult, [w] + RD, [w])
            TT("dve", W(7), W(4), bre_, ALU.mult, [w] + RD, [w])
            TT("dve", W(6), W(6), W(7), ALU.add, [w], [w])
            p1r = falloc(); p1i = falloc(); t = falloc()
            bbr_b = bc(W(5).unsqueeze(1), [128, 8, 64]); bbi_b = bc(W(6).unsqueeze(1), [128, 8, 64])
            TT("dve", p1r.v3(64), mr.v3(64), bbr_b, ALU.mult, [mr, w], [p1r])
            TT("dve", t.v3(64), mi.v3(64), bbi_b, ALU.mult, [mi, w], [t])
            TT("dve", p1r[:, :], p1r[:, :], t[:, :], ALU.subtract, [p1r, t], [p1r])
            TT("dve", p1i.v3(64), mr.v3(64), bbi_b, ALU.mult, [mr, w], [p1i])
            TT("dve", t.v3(64), mi.v3(64), bbr_b, ALU.mult, [mi, w], [t])
            TT("dve", p1i[:, :], p1i[:, :], t[:, :], ALU.add, [p1i, t], [p1i])
            free(w); free(mr); free(mi)
            for ri, src in ((0, p1r), (1, p1i)):
                for gp in range(2):
                    TS("dve", d["L1"][:, j, ri, :, gp * 64:(gp + 1) * 64], src.v3(64), cs("mgp%d" % gp), ALU.mult, [src, "cst"], ["s5m"])
            kc = falloc()
            t2 = falloc()
            acr = falloc(2); aci = falloc(2)
            ocr, oci = _PRM_OFF["A_cre"][0], _PRM_OFF["A_cim"][0]
            P.dma("sp", acr[:, 0:1024], prm_d[l, :, ocr + j * 1024:ocr + (j + 1) * 1024], writes=[acr], chan="acr")
            P.dma("sp", aci[:, 0:1024], prm_d[l, :, oci + j * 1024:oci + (j + 1) * 1024], writes=[aci], chan="aci")
            for ho in range(16):
                cr_b = bc(acr[:, ho * 64:(ho + 1) * 64].unsqueeze(1), [128, 8, 64])
                ci_b = bc(aci[:, ho * 64:(ho + 1) * 64].unsqueeze(1), [128, 8, 64])
                TT("dve", t.v3(64), p1r.v3(64), cr_b, ALU.mult, [p1r, acr], [t])
                TT("dve", t2.v3(64), p1i.v3(64), ci_b, ALU.mult, [p1i, aci], [t2])
                TT("dve", t[:, :], t[:, :], t2[:, :], ALU.subtract, [t, t2], [t])
                P.op("dve", lambda e, ho=ho: e.tensor_reduce(out=kc[:, 0:128].rearrange("p (a b) -> p a b", b=16)[:, :, ho], in_=t.v3(64),
                                                             axis=AX.X, op=ALU.add), [t], [kc])
            free(t2); free(p1r); free(p1i); free(acr); free(aci)
            klf = falloc(2)
            for g in range(8):
                o_, _w = _CST_OFF["mg8"]
                TS("dve", klf[:, 0:1024].rearrange("p (a b) -> p a b", b=128)[:, :, g * 16:(g + 1) * 16],
                   kc[:, 0:128].rearrange("p (a b) -> p a b", b=16), cst[:, o_ + g:o_ + g + 1], ALU.mult, [kc, "cst"], [klf])
            STT(klf[:, 0:128], cs("ident"), sp[:, SPC["s5d"] + j:SPC["s5d"] + j + 1], klf[:, 0:128], ALU.mult, ALU.add, ["cst", SPK, klf], [klf])
            CP("dve", d["KL"][:, j, :, :], klf[:, 0:1024].rearrange("p (a b) -> p a b", b=128), [klf], ["s5m"])
            free(kc); free(klf); free(t)
        free(a_lr); free(a_dt); free(a_e1); free(a_th)

        b_ = falloc()
        Bc = lambda i: b_[:, i * 8:(i + 1) * 8]
        TS("dve", Bc(0), pf("B_lre"), -1e-4, ALU.min, RD, [b_])
        ACT(Bc(1), pf("B_ldt"), AF.Exp, RD, [b_])
        TT("dve", Bc(2), Bc(0), Bc(1), ALU.mult, [b_], [b_])
        TT("dve", Bc(3), pf("B_lim"), Bc(1), ALU.mult, RD + [b_], [b_])
        TS("dve", Bc(3), Bc(3), 1.0 / TWO_PI, ALU.mult, [b_], [b_])
        u = falloc(); me = falloc(); cc = falloc(); ss = falloc()
        tauB_b = bc(cs("tauB").unsqueeze(1), [128, 8, 8])
        TT("dve", u[:, 0:64].rearrange("p (a b) -> p a b", b=8), bc(Bc(3).unsqueeze(2), [128, 8, 8]), tauB_b, ALU.mult, [b_, "cst"], [u])
        TT("dve", me[:, 0:64].rearrange("p (a b) -> p a b", b=8), bc(Bc(2).unsqueeze(2), [128, 8, 8]), tauB_b, ALU.mult, [b_, "cst"], [me])
        ACT(me[:, 0:64], me[:, 0:64], AF.Exp, [me], [me])
        trig(u[:, 0:64], 64, cc[:, 0:64], ss[:, 0:64], u, cc, ss)
        TT("dve", cc[:, 0:64], cc[:, 0:64], me[:, 0:64], ALU.mult, [cc, me], [cc])
        TT("dve", ss[:, 0:64], ss[:, 0:64], me[:, 0:64], ALU.mult, [ss, me], [ss])
        for qh in range(2):
            cr_b = bc(pf("B_cre")[:, qh * 64:(qh + 1) * 64].rearrange("p (q h) -> p q h", h=16).unsqueeze(2), [128, 4, 8, 16])
            ci_b = bc(pf("B_cim")[:, qh * 64:(qh + 1) * 64].rearrange("p (q h) -> p q h", h=16).unsqueeze(2), [128, 4, 8, 16])
            mr_b = bc(cc[:, qh * 32:(qh + 1) * 32].rearrange("p (q t) -> p q t", t=8).unsqueeze(3), [128, 4, 8, 16])
            mi_b = bc(ss[:, qh * 32:(qh + 1) * 32].rearrange("p (q t) -> p q t", t=8).unsqueeze(3), [128, 4, 8, 16])
            c1 = falloc(); c2 = falloc()
            v4 = lambda b: b[:, :].rearrange("p (q t h) -> p q t h", t=8, h=16)
            TT("dve", v4(c1), cr_b, mr_b, ALU.mult, RD + [cc], [c1])
            TT("dve", v4(c2), ci_b, mi_b, ALU.mult, RD + [ss], [c2])
            TT("dve", c1[:, :], c1[:, :], c2[:, :], ALU.subtract, [c1, c2], [c1])
            for gp in range(2):
                TS("dve", d["L3"][:, 0, qh * 4:(qh + 1) * 4, :, gp * 16:(gp + 1) * 16], v4(c1), cs("mB%d" % gp), ALU.mult, [c1, "cst"], ["s5m"])
            TT("dve", v4(c1), cr_b, mi_b, ALU.mult, RD + [ss], [c1])
            TT("dve", v4(c2), ci_b, mr_b, ALU.mult, RD + [cc], [c2])
            TT("dve", c1[:, :], c1[:, :], c2[:, :], ALU.add, [c1, c2], [c1])
            for gp in range(2):
                TS("dve", d["L3"][:, 1, qh * 4:(qh + 1) * 4, :, gp * 16:(gp + 1) * 16], v4(c1), cs("mB%d" % gp), ALU.mult, [c1, "cst"], ["s5m"],
                   s2=-1.0, op1=ALU.mult)
            free(c1); free(c2)
        ACT(d["r8"][:, :], Bc(2), AF.Exp, [b_], [("r8", l)], scale=8.0)
        TT("dve", d["rtab"][:, :].rearrange("p (q c) -> p q c", c=64), bc(d["r8"][:, :].unsqueeze(2), [128, 8, 64]),
           cs("segm").rearrange("p (q c) -> p q c", c=64), ALU.mult, [("r8", l), "cst"], [("rtab", l)])
        TS("dve", Bc(4), Bc(3), 8.0, ALU.mult, [b_], [b_], s2=64.0, op1=ALU.add)
        CP("dve", sb_int[:, 0:8], Bc(4), [b_], ["sb_int"])
        CP("dve", Bc(5), sb_int[:, 0:8], ["sb_int"], [b_])
        TT("dve", Bc(4), Bc(4), Bc(5), ALU.subtract, [b_], [b_])
        TS("dve", Bc(5), Bc(4), 0.5, ALU.is_gt, [b_], [b_])
        TT("dve", Bc(4), Bc(4), Bc(5), ALU.subtract, [b_], [b_])
        TS("dve", Bc(5), Bc(4), -0.5, ALU.is_lt, [b_], [b_])
        TT("dve", Bc(4), Bc(4), Bc(5), ALU.add, [b_], [b_])
        TT("dve", u.v3(64), bc(Bc(4).unsqueeze(2), [128, 8, 64]), bc(cs("cidx").unsqueeze(1), [128, 8, 64]), ALU.mult, [b_, "cst"], [u])
        trig(u[:, :], 512, d["cosR"][:, :], d["sinR"][:, :], u, ("cosR", l), ("sinR", l))
        free(u); free(me); free(cc); free(ss); free(b_)

    def prologue_taps(l):
        d = L[l]
        if "KL" in tap_out:
            tf = falloc(4)
            CP("dve", tf[:, 0:2048], s5m[:, 8192:10240], ["s5m"], [tf])
            TAP("KL", l, tf[:, 0:2048], [tf]); free(tf)
        if "L1" in tap_out:
            tf = falloc(8)
            CP("dve", tf[:, 0:4096], s5m[:, 0:4096], ["s5m"], [tf])
            TAP("L1", l, tf[:, 0:4096], [tf]); free(tf)
        if "L3" in tap_out:
            tf = falloc(8)
            CP("dve", tf[:, 0:4096], s5m[:, 4096:8192], ["s5m"], [tf])
            TAP("L3", l, tf[:, 0:4096], [tf]); free(tf)
        if "rot" in tap_out:
            TAP("rot", (l, 0), d["cosR"][:, :], [("cosR", l)])
            TAP("rot", (l, 1), d["sinR"][:, :], [("sinR", l)])
            TAP("rot", (l, 2), d["rtab"][:, :], [("rtab", l)])

    for l in layers:
        prologue_layer(l)
        prologue_taps(l)
        P.dma("sp", s5m_d[l], s5m[:, :], reads=["s5m"], writes=[("s5m_d", l)], chan="s5m_st")
    free(prm_sb)

    tile_list = [(s, ti, l) for s in range(n_seq) for ti in range(n_tiles) for l in layers]
    chunk_seq = [(l, c) for (s, ti, l) in tile_list for c in range(NCHUNK)]
    ring_state = dict(issued=0, used=0)

    def issue_next():
        n = ring_state["issued"]
        if n >= len(chunk_seq):
            return
        l, c = chunk_seq[n]
        slot = n % NRING
        ci = l * NCHUNK + c
        P.dma("sp", ring[slot][:, :, :].rearrange("p k n -> p (k n)"), wsc[ci], reads=[("wsc", ci)], writes=[("ring", slot)], chan=("ring", slot))
        ring_state["issued"] += 1

    def get_chunk(l, c):
        n = ring_state["used"]
        assert chunk_seq[n] == (l, c), (chunk_seq[n], l, c)
        while ring_state["issued"] <= n:
            issue_next()
        return n % NRING

    def release_chunk():
        ring_state["used"] += 1
        while ring_state["issued"] < min(len(chunk_seq), ring_state["used"] + NRING):
            issue_next()

    for _ in range(NRING):
        issue_next()

    def rmsnorm_to_h(l, gcol):
        sp = L[l]["sp"]
        for k in range(8):
            sq = balloc()
            ACT(sq[:, :], xs[:, k, :], AF.Square, [("xs", k)], [sq])
            MM([mm(banks[7][:, :], ones_bf[:, :], sq[:, :], start=(k == 0), stop=(k == 7))], [sq, "ones_bf"], [BK[7]])
            free(sq)
        rs = falloc()
        ACT(rs[:, :], banks[7][:, :], AF.Sqrt, [BK[7]], [rs], scale=1.0 / DM, bias=EPS)
        RECIP(rs[:, :], rs[:, :], [rs], [rs])
        for k in range(8):
            STT(hbuf[:, k, :], xs[:, k, :], sp[:, gcol + k:gcol + k + 1], rs[:, :], ALU.mult, ALU.mult, [("xs", k), ("sp", l), rs], [("h", k)])
        free(rs)

    HK = [("h", k) for k in range(8)]

    def proj_fm(slot, m, bank):
        MM([mm(banks[bank][:, :], ring[slot][:, k, m * 128:(m + 1) * 128], hbuf[:, k, :], start=(k == 0), stop=(k == 7)) for k in range(8)],
           [("ring", slot)] + HK, [BK[bank]])

    def layer_tile(s, ti, l):
        d = L[l]
        sp = d["sp"]
        SPK = ("sp", l)
        first = (ti == 0)
        t0 = ti * NT
        if l == layers[0]:
            for k in range(8):
                P.dma("act", xs[:, k, :], xT[s, k * 128:(k + 1) * 128, t0:t0 + NT], writes=[("xs", k)], chan=("xs", k))
        if first:
            P.op("pool", lambda e: e.memset(d["zbuf"][:, :, 0:2], 0.0), writes=[("zbuf", l)])
            P.op("pool", lambda e: e.memset(d["Er"][:, :, 0:1], 0.0), writes=[("Er", l)])
            P.op("pool", lambda e: e.memset(d["Ei"][:, :, 0:1], 0.0), writes=[("Ei", l)])
            for j in range(2):
                P.op("pool", lambda e, j=j: e.memset(d["S"][j][:, :], 0.0), writes=[("S", l, j)])
        P.dma("act", s5m[:, :], s5m_d[l], reads=[("s5m_d", l)], writes=["s5m"], chan="s5m_ld")
        rmsnorm_to_h(l, SPC["gmix"])
        CHK("norm1")
        Y = [falloc() for _ in range(8)]
        YN = [balloc() for _ in range(8)]

        def group_norm(gI, ssb):
            for i in range(2):
                sq = balloc()
                ACT(sq[:, :], Y[2 * gI + i][:, :], AF.Square, [Y[2 * gI + i]], [sq])
                MM([mm(banks[ssb][:, :], ones_bf[:, :], sq[:, :], start=(i == 0), stop=(i == 1))], [sq, "ones_bf"], [BK[ssb]])
                free(sq)
            rs = falloc()
            ACT(rs[:, :], banks[ssb][:, :], AF.Sqrt, [BK[ssb]], [rs], scale=1.0 / 256, bias=EPS)
            RECIP(rs[:, :], rs[:, :], [rs], [rs])
            for i in range(2):
                k = 2 * gI + i
                STT(YN[k][:, :], Y[k][:, :], sp[:, SPC["ggn"] + k:SPC["ggn"] + k + 1], rs[:, :], ALU.mult, ALU.mult, [Y[k], SPK, rs], [YN[k]])
            free(rs)
            free(Y[2 * gI]); free(Y[2 * gI + 1])

        def chainX():
            slot = get_chunk(l, 0)
            hsb = [falloc() for _ in range(2)]
            bsb = [falloc() for _ in range(2)]
            for j in range(2):
                proj_fm(slot, j, j)
                CP("act", hsb[j][:, :], banks[j][:, :], [BK[j]], [hsb[j]])
            for j in range(2):
                proj_fm(slot, 2 + j, j)
                CP("act", bsb[j][:, :], banks[j][:, :], [BK[j]], [bsb[j]])
            release_chunk()
            slot = get_chunk(l, 1)
            zb = d["zbuf"]
            for j in range(2):
                proj_fm(slot, j, j)
                TT("dve", zb[:, j, 2:NT + 2], banks[j][:, :], hsb[j][:, :], ALU.mult, [BK[j], hsb[j]], [("zbuf", l)])
            ubf = balloc(2)
            for j in range(2):
                proj_fm(slot, 2 + j, j)
                CP("act", ubf[:, j * NT:(j + 1) * NT].rearrange("p (t c) -> p t c", c=NC5), banks[j][:, :].rearrange("p (c t) -> p t c", t=TS5),
                   [BK[j]], [ubf])
            release_chunk()
            yield
            for j in range(2):
                acc = falloc()
                cw = lambda i: sp[:, SPC["convw"] + j * 3 + i:SPC["convw"] + j * 3 + i + 1]
                TS("pool", acc[:, :], zb[:, j, 2:NT + 2], cw(2), ALU.mult, [("zbuf", l), SPK], [acc])
                STT(acc[:, :], zb[:, j, 1:NT + 1], cw(1), acc[:, :], ALU.mult, ALU.add, [("zbuf", l), SPK, acc], [acc])
                STT(acc[:, :], zb[:, j, 0:NT], cw(0), acc[:, :], ALU.mult, ALU.add, [("zbuf", l), SPK, acc], [acc])
                TT("pool", Y[j][:, :], acc[:, :], bsb[j][:, :], ALU.mult, [acc, bsb[j]], [Y[j]])
                free(acc)
            P.op("pool", lambda e: e.tensor_copy(out=d["zbuf"][:, :, 0:2], in_=d["zbuf"][:, :, NT:NT + 2]), [("zbuf", l)], [("zbuf", l)])
            for b_ in hsb + bsb:
                free(b_)
            yield
            group_norm(0, 1)
            yield
            ut = [ubf[:, j * NT:(j + 1) * NT] for j in range(2)]
            for ri in range(2):
                for qq in range(4):
                    mms = []
                    for j in range(2):
                        q = j * 4 + qq
                        for s_ in range(TS5):
                            mms.append(mm(banks[ri][:, q * NC5:(q + 1) * NC5], d["L1"][qq * 32:(qq + 1) * 32, j, ri, TS5 - 1 - s_, :],
                                          ut[j][qq * 32:(qq + 1) * 32, s_ * NC5:(s_ + 1) * NC5], start=(s_ == 0), stop=(s_ == TS5 - 1), tp=(qq * 32, 0)))
                    MM(mms, [ubf, "s5m"], [BK[ri]], force=True)
            yield
            Wr = falloc(); Wi = falloc(); t1 = falloc(); t2 = falloc()
            cosR, sinR, rtab = d["cosR"], d["sinR"], d["rtab"]
            CK, SK, RK = ("cosR", l), ("sinR", l), ("rtab", l)
            TT("dve", t1[:, :], banks[0][:, :], cosR[:, :], ALU.mult, [BK[0], CK], [t1])
            TT("dve", t2[:, :], banks[1][:, :], sinR[:, :], ALU.mult, [BK[1], SK], [t2])
            TT("pool", Wr[:, :], t1[:, :], t2[:, :], ALU.add, [t1, t2], [Wr])
            TT("dve", t1[:, :], banks[1][:, :], cosR[:, :], ALU.mult, [BK[1], CK], [t1])
            TT("dve", t2[:, :], banks[0][:, :], sinR[:, :], ALU.mult, [BK[0], SK], [t2])
            TT("pool", Wi[:, :], t1[:, :], t2[:, :], ALU.subtract, [t1, t2], [Wi])
            yield
            for (Wx, Ex, ek) in ((Wr, d["Er"], ("Er", l)), (Wi, d["Ei"], ("Ei", l))):
                TT("dve", t1[:, 0:8], d["r8"][:, :], Ex[:, :, 0], ALU.mult, [("r8", l), ek], [t1])
                TT("dve", Wx.v3(NC5)[:, :, 0], Wx.v3(NC5)[:, :, 0], t1[:, 0:8], ALU.add, [Wx, t1], [Wx])
            Fr = falloc(); Fi = falloc()
            for (Wx, Fx) in ((Wr, Fr), (Wi, Fi)):
                P.op("dve", lambda e, Wx=Wx, Fx=Fx: e.tensor_tensor_scan(out=Fx[:, :], data0=rtab[:, :], data1=Wx[:, :], initial=0.0,
                                                                        op0=ALU.mult, op1=ALU.add), [RK, Wx], [Fx])
            yield
            TT("dve", t1[:, :], Fr[:, :], cosR[:, :], ALU.mult, [Fr, CK], [t1])
            TT("dve", t2[:, :], Fi[:, :], sinR[:, :], ALU.mult, [Fi, SK], [t2])
            TT("pool", d["Er"][:, :, 1:NC5 + 1], t1.v3(NC5), t2.v3(NC5), ALU.subtract, [t1, t2], [("Er", l)])
            TT("dve", t1[:, :], Fi[:, :], cosR[:, :], ALU.mult, [Fi, CK], [t1])
            TT("dve", t2[:, :], Fr[:, :], sinR[:, :], ALU.mult, [Fr, SK], [t2])
            TT("pool", d["Ei"][:, :, 1:NC5 + 1], t1.v3(NC5), t2.v3(NC5), ALU.add, [t1, t2], [("Ei", l)])
            for b_ in (Wr, Wi, t1, t2, Fr, Fi):
                free(b_)
            Ebf = balloc(2)
            CP("act", Ebf[:, 0:NT].rearrange("p (q c) -> p q c", c=NC5), d["Er"][:, :, 0:NC5], [("Er", l)], [Ebf])
            CP("act", Ebf[:, NT:2 * NT].rearrange("p (q c) -> p q c", c=NC5), d["Ei"][:, :, 0:NC5], [("Ei", l)], [Ebf])
            P.op("pool", lambda e: e.tensor_copy(out=d["Er"][:, :, 0:1], in_=d["Er"][:, :, NC5:NC5 + 1]), [("Er", l)], [("Er", l)])
            P.op("pool", lambda e: e.tensor_copy(out=d["Ei"][:, :, 0:1], in_=d["Ei"][:, :, NC5:NC5 + 1]), [("Ei", l)], [("Ei", l)])
            yield
            E4 = [Ebf[:, ri * NT:(ri + 1) * NT].rearrange("p (q c) -> p q c", c=NC5) for ri in range(2)]
            for j in range(2):
                yb = banks[j]
                mms = []
                for tau in range(TS5):
                    mms.append(mm(yb[:, tau * NC5:NT], d["KL"][:, j, tau, :], ut[j][:, 0:(TS5 - tau) * NC5], start=(tau == 0), stop=False))
                for qq in range(4):
                    q = j * 4 + qq
                    for t_ in range(TS5):
                        for ri in range(2):
                            mms.append(mm(yb[qq * 32:(qq + 1) * 32, t_ * NC5:(t_ + 1) * NC5], d["L3"][:, ri, q, t_, :], E4[ri][:, q, :], start=False,
                                          stop=(qq == 3 and t_ == TS5 - 1 and ri == 1), tp=(0, qq * 32)))
                MM(mms, [ubf, Ebf, "s5m"], [BK[j]])
            yield
            gl = balloc(2)
            for j in range(2):
                ysb = falloc(); sq = falloc()
                ynat = banks[j][:, :].rearrange("p (t c) -> p c t", c=NC5)
                CP("act", ysb.v3(TS5), ynat, [BK[j]], [ysb])
                ACT(sq.v3(TS5), ynat, AF.Square, [BK[j]], [sq])
                TS("dve", sq[:, :], sq[:, :], 0.044715, ALU.mult, [sq], [sq], s2=1.0, op1=ALU.add)
                TT("dve", sq[:, :], sq[:, :], ysb[:, :], ALU.mult, [sq, ysb], [sq])
                ACT(sq[:, :], sq[:, :], AF.Sigmoid, [sq], [sq], scale=1.5957691216057308)
                TT("pool", gl[:, j * NT:(j + 1) * NT], ysb[:, :], sq[:, :], ALU.mult, [ysb, sq], [gl])
                free(ysb); free(sq)
                yield
            free(ubf); free(Ebf)
            for j in range(2):
                for (n, bk) in ((j, 0), (2 + j, 1)):
                    MM([mm(banks[bk][:, :], d["glu"][:, k, n * 128:(n + 1) * 128], gl[:, k * NT:(k + 1) * NT], start=(k == 0), stop=(k == 1)) for k in range(2)],
                       [gl, ("glu", l)], [BK[bk]])
                sg = falloc()
                ACT(sg[:, :], banks[1][:, :], AF.Sigmoid, [BK[1]], [sg])
                TT("dve", Y[2 + j][:, :], banks[0][:, :], sg[:, :], ALU.mult, [BK[0], sg], [Y[2 + j]])
                free(sg)
                yield
            free(gl)
            group_norm(1, 1)

        def chainY():
            slot = get_chunk(l, 2)
            for m in range(3):
                proj_fm(slot, m, 2 + m)
            MM([mm(banks[5][:, blk * 128:(blk + 1) * 128], hbuf[:, k, blk * 128:(blk + 1) * 128], ring[slot][:, k, 384:512], start=(k == 0), stop=(k == 7))
                for blk in range(4) for k in range(8)], [("ring", slot)] + HK, [BK[5]])
            release_chunk()
            kbuf, vbuf = d["kbuf"], d["vbuf"]
            KB, VB = ("kbuf", l), ("vbuf", l)
            qn = balloc(2)
            CP("act", vbuf[:, 1:5, :], banks[5][:, :].rearrange("p (b f) -> p b f", f=128), [BK[5]], [VB])
            yield
            for (bk, gcol, outv, okey) in ((2, SPC["qgs"], qn[:, 0:NT], qn), (3, SPC["qgs"], qn[:, NT:2 * NT], qn),
                                           (4, SPC["kg"], kbuf[:, 128:128 + NT], KB)):
                sq = balloc(); rs = falloc()
                ACT(sq[:, :], banks[bk][:, :], AF.Square, [BK[bk]], [sq])
                MM([mm(banks[6][:, :], bones_bf[:, :], sq[:, :])], [sq, "bones_bf"], [BK[6]])
                ACT(rs[:, :], banks[6][:, :], AF.Sqrt, [BK[6]], [rs], scale=1.0 / 64, bias=EPS)
                RECIP(rs[:, :], rs[:, :], [rs], [rs])
                STT(outv, banks[bk][:, :], sp[:, gcol:gcol + 1], rs[:, :], ALU.mult, ALU.mult, [BK[bk], SPK, rs], [okey])
                free(sq); free(rs)
                yield
            jb_list = list(range(1 if first else 0, 5))
            for jbi, jb in enumerate(jb_list):
                qlo, qhi = max(0, 2 * jb - 2), min(8, 2 * jb + 2)
                off = (qlo - (2 * jb - 2)) * 64
                ncol = (qhi - qlo) * 64
                for kh in range(2):
                    MM([mm(banks[2 + kh][:, r * 256 + off:r * 256 + off + ncol], kbuf[kh * 64:(kh + 1) * 64, jb * 128:(jb + 1) * 128],
                           qn[kh * 64:(kh + 1) * 64, r * NT + qlo * 64:r * NT + qhi * 64]) for r in range(2)], [KB, qn], [BK[2 + kh]])
                pT = balloc(2)
                P.op("pool", lambda e, pT=pT: e.memset(pT[:, :], 0.0), writes=[pT])
                for kh in range(2):
                    for half, (c0, c1) in enumerate(((0, 192), (64, 256))):
                        a, b2 = max(c0, off), min(c1, off + ncol)
                        if b2 <= a:
                            continue
                        src = banks[2 + kh][half * 64:(half + 1) * 64, :].rearrange("p (r c) -> p r c", c=256)[:, :, a:b2]
                        dst = pT[half * 64:(half + 1) * 64, kh * 512:(kh + 1) * 512].rearrange("p (r c) -> p r c", c=256)[:, :, a:b2]
                        ACT(dst, src, AF.Exp, [BK[2 + kh]], [pT])
                for r in range(2):
                    mms_n, mms_d = [], []
                    for kh in range(2):
                        pv = pT[:, kh * 512 + r * 256:kh * 512 + (r + 1) * 256]
                        for part in range(2):
                            pair = jb - 1 + part
                            if pair < 0 or pair > 3:
                                continue
                            first_contrib = (part == 1) or (first and jb == 1)
                            last_contrib = (part == 0) or (jb == 4)
                            if part == 1 and jb == 4:
                                continue
                            cols = slice(pair * 128, (pair + 1) * 128)
                            mms_n.append(mm(banks[4 + r][kh * 64:(kh + 1) * 64, cols], vbuf[:, jb, kh * 64:(kh + 1) * 64], pv[:, part * 128:(part + 1) * 128],
                                            start=first_contrib, stop=last_contrib))
                            mms_d.append(mm(banks[6 + r][kh * 64:(kh + 1) * 64, cols], ones_bf[:, 0:64], pv[:, part * 128:(part + 1) * 128],
                                            start=first_contrib, stop=last_contrib))
                    MM(mms_n, [pT, VB], [BK[4 + r]])
                    MM(mms_d, [pT, "ones_bf"], [BK[6 + r]])
                free(pT)
                yield
            for r in range(2):
                rec = falloc()
                TS("dve", rec[:, :], banks[6 + r][:, :], sp[:, SPC["esink"] + r:SPC["esink"] + r + 1], ALU.add, [BK[6 + r], SPK], [rec])
                RECIP(rec[:, :], rec[:, :], [rec], [rec])
                TT("dve", Y[4 + r][:, :], banks[4 + r][:, :], rec[:, :], ALU.mult, [BK[4 + r], rec], [Y[4 + r]])
                free(rec)
            free(qn)
            P.op("pool", lambda e: e.tensor_copy(out=kbuf[:, 0:128], in_=kbuf[:, NT:NT + 128]), [KB], [KB])
            P.op("pool", lambda e: e.tensor_copy(out=vbuf[:, 0, :], in_=vbuf[:, 4, :]), [VB], [VB])
            yield
            group_norm(2, 2)
            yield
            slot = get_chunk(l, 3)
            for m in range(4):
                proj_fm(slot, m, 2 + m)
            release_chunk()
            qt = balloc(2); qh = balloc(4); kt = balloc(2); kend = balloc(2)
            e3s = []
            for j in range(2):
                sig = falloc(); lg = falloc(); kk = falloc(); B = falloc(); e1 = falloc(); e2 = falloc(); e3 = falloc()
                ACT(sig[:, :], banks[4 + j][:, :], AF.Sigmoid, [BK[4 + j]], [sig])
                TS("dve", sig[:, :], sig[:, :], sp[:, SPC["oml"] + j:SPC["oml"] + j + 1], ALU.mult, [sig, SPK], [sig],
                   s2=sp[:, SPC["lb"] + j:SPC["lb"] + j + 1], op1=ALU.add)
                ACT(lg[:, :], sig[:, :], AF.Ln, [sig], [lg])
                TS("pool", kk[:, :], sig[:, :], -1.0, ALU.mult, [sig], [kk], s2=1.0, op1=ALU.add)
                P.op("dve", lambda e, B=B, lg=lg: e.tensor_tensor_scan(out=B[:, :], data0=cs("segm"), data1=lg[:, :], initial=0.0,
                                                                      op0=ALU.mult, op1=ALU.add), ["cst", lg], [B])
                yield
                ACT(e3[:, :], B[:, :], AF.Exp, [B], [e3])
                TT("dve", lg.v3(64), B.v3(64), bc(B.v3(64)[:, :, 31:32], [128, 8, 64]), ALU.subtract, [B], [lg])
                ACT(e1[:, :], lg[:, :], AF.Exp, [lg], [e1])
                ACT(e2[:, :], lg[:, :], AF.Exp, [lg], [e2], scale=-1.0)
                TT("dve", qt[:, j * NT:(j + 1) * NT], banks[2 + j][:, :], e1[:, :], ALU.mult, [BK[2 + j], e1], [qt])
                for hh in range(2):
                    STT(qh[:, (j * 2 + hh) * NT:(j * 2 + hh + 1) * NT], banks[2 + j][:, :], cs("mB%d" % hh), e3[:, :], ALU.mult, ALU.mult,
                        [BK[2 + j], e3, "cst"], [qh])
                TT("pool", kt[:, j * NT:(j + 1) * NT], kk[:, :], e2[:, :], ALU.mult, [kk, e2], [kt])
                TT("pool", kend[:, j * NT:(j + 1) * NT].rearrange("p (b t) -> p b t", t=64), kt[:, j * NT:(j + 1) * NT].rearrange("p (b t) -> p b t", t=64),
                   bc(e1.v3(64)[:, :, 63:64], [128, 8, 64]), ALU.mult, [kt, e1], [kend])
                e3s.append(e3)
                for b_ in (sig, lg, kk, B, e1, e2):
                    free(b_)
                yield
            b6bf = banks[6][:, :].bitcast(BF16)
            def tr_fn(e):
                ins = None
                for bp in range(4):
                    for j in range(2):
                        ins = e.transpose(out=b6bf[:, (bp * 2 + j) * 128:(bp * 2 + j + 1) * 128], in_=kend[:, j * NT + bp * 128:j * NT + (bp + 1) * 128],
                                          identity=ident_bf[:, :])
                return ins
            P.op("pe", tr_fn, [kend, "ident_bf"], [BK[6]])
            kendT = balloc(2)
            CP("act", kendT[:, :], b6bf, [BK[6]], [kendT])
            free(kend)
            yield
            slot = get_chunk(l, 4)
            hib = (7, 2)
            for half in range(2):
                MM([mm(banks[hib[half]][:, bq * 256:(bq + 1) * 256], hbuf[:, k, (half * 2 + bq) * 128:(half * 2 + bq + 1) * 128], ring[slot][:, k, 0:256],
                       start=(k == 0), stop=(k == 7)) for bq in range(2) for k in range(8)], [("ring", slot)] + HK, [BK[hib[half]]])
            for j in range(2):
                proj_fm(slot, 2 + j, 3 + j)
            release_chunk()
            vT = balloc(2)
            for half in range(2):
                CP("act", vT[:, half * NT:(half + 1) * NT], banks[hib[half]][:, :], [BK[hib[half]]], [vT])
            sgs = []
            for j in range(2):
                sg = falloc()
                ACT(sg[:, :], banks[3 + j][:, :], AF.Silu, [BK[3 + j]], [sg])
                sgs.append(sg)
            yield
            for hp in range(2):
                mms = []
                for j in range(2):
                    for b in range(8):
                        bp, half = b // 2, b % 2
                        mms.append(mm(banks[5 + hp][half * 64:(half + 1) * 64, j * 256 + bp * 64:j * 256 + (bp + 1) * 64],
                                      kt[hp * 64:(hp + 1) * 64, j * NT + b * 64:j * NT + (b + 1) * 64],
                                      qt[hp * 64:(hp + 1) * 64, j * NT + b * 64:j * NT + (b + 1) * 64]))
                MM(mms, [kt, qt], [BK[5 + hp]])
            for hp in range(2):
                for half in range(2):
                    rows = slice(half * 64, (half + 1) * 64)
                    dst = Amz[rows, :].rearrange("p (j hp bp hf t) -> p j hp bp hf t", j=2, hp=2, bp=4, hf=2, t=64)[:, :, hp, :, half, :]
                    src = banks[5 + hp][rows, :].rearrange("p (j bp t) -> p j bp t", j=2, bp=4)
                    o_c, _w = _CST_OFF["cmask"]
                    msk = bc(cst[rows, o_c:o_c + 64].unsqueeze(1).unsqueeze(1), [64, 2, 4, 64])
                    TT("dve", dst, src, msk, ALU.mult, [BK[5 + hp], "cst"], ["Amz"])
            free(kt); free(qt)
            yield
            ub = (7, 2)
            for half in range(2):
                mms = []
                for j in range(2):
                    for hh in range(2):
                        h = 2 * j + hh
                        for bp in range(4):
                            mms.append(mm(banks[ub[half]][hh * 64:(hh + 1) * 64, j * 256 + bp * 64:j * 256 + (bp + 1) * 64],
                                          kendT[half * 64:(half + 1) * 64, bp * 256 + h * 64:bp * 256 + (h + 1) * 64],
                                          vT[half * 64:(half + 1) * 64, bp * 256 + h * 64:bp * 256 + (h + 1) * 64]))
                MM(mms, [kendT, vT], [BK[ub[half]]])
            free(kendT)
            yield
            Sall = balloc(3)
            for j in range(2):
                Sj = d["S"][j]
                SKj = ("S", l, j)
                CP("act", Sall[:, j * 768:j * 768 + 64], Sj[:, :], [SKj], [Sall])
                for b in range(8):
                    bp, half = b // 2, b % 2
                    STT(Sj[:, :], Sj[:, :], e3s[j][:, b * 64 + 63:b * 64 + 64], banks[ub[half]][:, j * 256 + bp * 64:j * 256 + (bp + 1) * 64], ALU.mult, ALU.add,
                        [SKj, e3s[j], BK[ub[half]]], [SKj])
                    if b < 7:
                        CP("act", Sall[:, j * 768 + (b + 1) * 64:j * 768 + (b + 2) * 64], Sj[:, :], [SKj], [Sall])
                    if b % 2 == 1:
                        yield
            for e3 in e3s:
                free(e3)
            for j in range(2):
                mms = []
                for hh in range(2):
                    h = 2 * j + hh
                    for b in range(8):
                        bp = b // 2
                        o_ = banks[5 + j][hh * 64:(hh + 1) * 64, b * 64:(b + 1) * 64]
                        mms.append(mm(o_, Sall[:, j * 768 + b * 64:j * 768 + (b + 1) * 64],
                                      qh[:, (j * 2 + hh) * NT + b * 64:(j * 2 + hh) * NT + (b + 1) * 64], start=True, stop=False))
                        mms.append(mm(o_, vT[:, bp * 256 + h * 64:bp * 256 + (h + 1) * 64],
                                      Amz[:, (h * 8 + b) * 64:(h * 8 + b + 1) * 64], start=False, stop=True))
                MM(mms, [Sall, qh, vT, "Amz"], [BK[5 + j]])
            free(Sall); free(qh); free(vT)
            yield
            for j in range(2):
                sq = balloc(); rs = falloc()
                ACT(sq[:, :], banks[5 + j][:, :], AF.Square, [BK[5 + j]], [sq])
                MM([mm(banks[3][:, :], bones_bf[:, :], sq[:, :])], [sq, "bones_bf"], [BK[3]])
                ACT(rs[:, :], banks[3][:, :], AF.Sqrt, [BK[3]], [rs], scale=1.0 / 64, bias=EPS)
                RECIP(rs[:, :], rs[:, :], [rs], [rs])
                STT(rs[:, :], banks[5 + j][:, :], sp[:, SPC["og"]:SPC["og"] + 1], rs[:, :], ALU.mult, ALU.mult, [BK[5 + j], SPK, rs], [rs])
                TT("pool", Y[6 + j][:, :], rs[:, :], sgs[j][:, :], ALU.mult, [rs, sgs[j]], [Y[6 + j]])
                free(sq); free(rs); free(sgs[j])
                yield
            group_norm(3, 3)

        gens = [chainX(), chainY()]
        while gens:
            for g in list(gens):
                try:
                    next(g)
                except StopIteration:
                    gens.remove(g)
        CHK("hgrn")
        for c in range(2):
            slot = get_chunk(l, 5 + c)
            for m in range(4):
                MM([mm(banks[m][:, :], ring[slot][:, k, m * 128:(m + 1) * 128], YN[k][:, :], start=(k == 0), stop=(k == 7)) for k in range(8)],
                   [("ring", slot)] + YN, [BK[m]])
            release_chunk()
            for m in range(4):
                k = c * 4 + m
                TT("dve", xs[:, k, :], xs[:, k, :], banks[m][:, :], ALU.add, [("xs", k), BK[m]], [("xs", k)])
        for y_ in YN:
            free(y_)
        if "xmid" in tap_out and (s, ti) == taps.get("_ysel_tile", (0, 0)):
            for k in range(8):
                TAP("xmid", (l, k), xs[:, k, :], [("xs", k)])

        CHK("gn")
        rmsnorm_to_h(l, SPC["gffn"])
        hid = balloc(32)
        HIDK = hid.keys_
        for c in range(8):
            slot = get_chunk(l, 7 + c)
            for m in range(4):
                bk = (c * 4 + m) % 4
                proj_fm(slot, m, bk)
                r_ = falloc()
                ACT(r_[:, :], banks[bk][:, :], AF.Relu, [BK[bk]], [r_])
                idx = c * 4 + m
                TT("pool", hid[:, idx * NT:(idx + 1) * NT], r_[:, :], r_[:, :], ALU.mult, [r_], [HIDK[idx]])
                free(r_)
            release_chunk()
        for cg in range(2):
            for kg in range(4):
                slot = get_chunk(l, 15 + cg * 4 + kg)
                for m in range(4):
                    MM([mm(banks[4 + m][:, :], ring[slot][:, k, m * 128:(m + 1) * 128], hid[:, (kg * 8 + k) * NT:(kg * 8 + k + 1) * NT],
                           start=(kg == 0 and k == 0), stop=(kg == 3 and k == 7)) for k in range(8)],
                       [("ring", slot)] + HIDK[kg * 8:(kg + 1) * 8], [BK[4 + m]])
                release_chunk()
            for m in range(4):
                k = cg * 4 + m
                TT("dve", xs[:, k, :], xs[:, k, :], banks[4 + m][:, :], ALU.add, [("xs", k), BK[4 + m]], [("xs", k)])
        free(hid)
        if "xout" in tap_out and (s, ti) == taps.get("_ysel_tile", (0, 0)):
            for k in range(8):
                TAP("xout", (l, k), xs[:, k, :], [("xs", k)])
        if l == layers[-1]:
            outs = []
            for k in range(8):
                outs.append(P.dma("sp", oT[s, k * 128:(k + 1) * 128, t0:t0 + NT], xs[:, k, :], reads=[("xs", k)], writes=[("oT", s, ti, k)], chan=("out", k)))
            return outs
        return []

    all_out = []
    try:
        CHK("prologue")
        for (s, ti, l) in tile_list:
            all_out += layer_tile(s, ti, l)
    except StopBuild:
        pass
    if list_schedule:
        P.schedule()
    P.emit(final_deps=all_out + tap_dmas)
    return nc, P


_CACHE = {}


def prepare_inputs(inputs):
    wch, glu = _build_weights(inputs)
    prm = np.stack([_build_params(inputs, l) for l in range(DEPTH)], axis=0)
    cst = _build_consts()
    x = np.asarray(inputs["x"])
    in_maps = []
    for c in range(NCORES):
        xc = x[c * SEQ_PER_CORE:(c + 1) * SEQ_PER_CORE]
        xT = np.ascontiguousarray(xc.transpose(0, 2, 1))
        in_maps.append({"xT": xT, "wch": wch, "glu": glu, "prm": prm, "cst": cst})
    return in_maps


def kernel(**inputs):
    in_maps = prepare_inputs(inputs)
    if "nc" not in _CACHE:
        _CACHE["nc"] = build_program()[0]
    res = run_bass_kernel_spmd(_CACHE["nc"], in_maps, core_ids=list(range(NCORES)))
    outs = []
    for c in range(NCORES):
        oT = res.results[c]["oT"]
        outs.append(np.ascontiguousarray(oT.transpose(0, 2, 1)))
    return np.concatenate(outs, axis=0).astype(np.float32)
```

```python
import contextlib
import numpy as np
import concourse.bass as bass
import concourse.mybir as mybir
from concourse.bass_utils import run_bass_kernel_spmd

F32 = mybir.dt.float32
BF16 = mybir.dt.bfloat16
I32 = mybir.dt.int32
ALU = mybir.AluOpType
AF = mybir.ActivationFunctionType
AX = mybir.AxisListType

NCORES = 8
SEQ_PER_CORE = 4
SEQ = 2048
NT = 512
TILES_PER_SEQ = SEQ // NT
DM = 1024
DEPTH = 2
NCHUNK = 23
EPS = 1e-6
TS5 = 8
NC5 = NT // TS5
TWO_PI = 6.283185307179586

ENGS = ("pe", "act", "dve", "pool", "sp")
SEM_CAP = 1000


class Op:
    __slots__ = ("eng", "fn", "deps", "is_dma", "chan", "needs_inc", "sem", "val", "all_deps", "cost", "seq", "dma_us")

    def __init__(self, eng, fn, is_dma=False, chan=None):
        self.eng = eng
        self.fn = fn
        self.deps = []
        self.is_dma = is_dma
        self.chan = chan
        self.needs_inc = is_dma
        self.sem = None
        self.val = None
        self.all_deps = []
        self.cost = 0.3
        self.dma_us = 0.0
        self.seq = 0


def _keys(items):
    out = []
    for it in items:
        if hasattr(it, "keys_"):
            out.extend(it.keys_)
        else:
            out.append(it)
    return out


class Prog:
    def __init__(self, nc, same_engine_sync=True):
        self.nc = nc
        self.ops = {e: [] for e in ENGS}
        self.last_writer = {}
        self.readers = {}
        self.same_engine_sync = same_engine_sync
        self.nops = 0

    def schedule(self):
        import heapq
        allops = [o for e in ENGS for o in self.ops[e]]
        allops.sort(key=lambda o: o.seq)
        ndep = {}
        succ = {}
        for o in allops:
            ndep[id(o)] = len(o.all_deps)
            for d in o.all_deps:
                succ.setdefault(id(d), []).append(o)
        ready_t = {id(o): 0.0 for o in allops}
        finish = {}
        eng_free = {e: 0.0 for e in ENGS}
        heap = []
        for o in allops:
            if ndep[id(o)] == 0:
                heapq.heappush(heap, (0.0, o.seq, o))
        new_order = {e: [] for e in ENGS}
        nsched = 0
        while heap:
            key, _, o = heapq.heappop(heap)
            start = max(eng_free[o.eng], ready_t[id(o)])
            if start > key + 1e-9:
                heapq.heappush(heap, (start, o.seq, o))
                continue
            if o.is_dma:
                eng_free[o.eng] = start + 0.06
                fin = start + 2.0 + o.dma_us
            else:
                eng_free[o.eng] = start + o.cost
                fin = start + o.cost
            finish[id(o)] = fin
            new_order[o.eng].append(o)
            nsched += 1
            for c in succ.get(id(o), ()):
                lat = 0.06 if (c.eng == o.eng and not o.is_dma) else 0.25
                ready_t[id(c)] = max(ready_t[id(c)], fin + lat)
                ndep[id(c)] -= 1
                if ndep[id(c)] == 0:
                    heapq.heappush(heap, (ready_t[id(c)], c.seq, c))
        assert nsched == len(allops), (nsched, len(allops))
        self.ops = new_order
        self.est_us = max(finish.values()) if finish else 0.0

    def op(self, eng, fn, reads=(), writes=(), is_dma=False, chan=None, force=False, cost=0.3, dma_us=0.0):
        reads = _keys(reads)
        writes = _keys(writes)
        bank_reads = [k for k in reads if isinstance(k, tuple) and k and k[0] == "bank"]
        if bank_reads:
            reads = [k for k in reads if k not in bank_reads]
            writes = list(writes) + bank_reads
        o = Op(eng, fn, is_dma, chan)
        o.cost = cost
        o.dma_us = dma_us
        o.seq = self.nops
        self.nops += 1
        deps = {}
        for k in reads:
            w = self.last_writer.get(k)
            if w is not None:
                deps[id(w)] = w
        for k in writes:
            w = self.last_writer.get(k)
            if w is not None:
                deps[id(w)] = w
            for r in self.readers.get(k, ()):
                deps[id(r)] = r
        o.all_deps = list(deps.values())
        for d in deps.values():
            if (not d.is_dma) and d.eng == eng and ((eng == "pe" and not force) or not self.same_engine_sync):
                continue
            d.needs_inc = True
            o.deps.append(d)
        for k in reads:
            self.readers.setdefault(k, []).append(o)
        for k in writes:
            self.last_writer[k] = o
            self.readers[k] = []
        self.ops[eng].append(o)
        return o

    def dma(self, eng, out, in_, reads=(), writes=(), chan=None, **kw):
        nbytes = 1
        for d_ in out.shape:
            nbytes *= d_
        nbytes *= mybir.dt.size(out.dtype)
        return self.op(eng, lambda e: e.dma_start(out=out, in_=in_, **kw), reads, list(writes) + [("chan", chan)], is_dma=True, chan=chan,
                       dma_us=nbytes / 150e3)

    def emit(self, final_deps=()):
        nc = self.nc
        stack = contextlib.ExitStack()
        eng_sems = {e: [] for e in ENGS}
        eng_cnt = {e: 0 for e in ENGS}
        chan_sem, chan_cnt = {}, {}
        nsem = 0
        for e in ENGS:
            for o in self.ops[e]:
                if o.is_dma:
                    if o.chan not in chan_sem or chan_cnt[o.chan] + 16 > SEM_CAP:
                        chan_sem[o.chan] = stack.enter_context(nc.semaphore(f"c{nsem}"))
                        nsem += 1
                        chan_cnt[o.chan] = 0
                    chan_cnt[o.chan] += 16
                    o.sem, o.val = chan_sem[o.chan], chan_cnt[o.chan]
                elif o.needs_inc:
                    if eng_cnt[e] % SEM_CAP == 0:
                        eng_sems[e].append(stack.enter_context(nc.semaphore(f"e{nsem}")))
                        nsem += 1
                    eng_cnt[e] += 1
                    o.sem, o.val = eng_sems[e][-1], (eng_cnt[e] - 1) % SEM_CAP + 1
        self.nsem = nsem
        final_deps = list(final_deps)

        def run_engine(ename, eng):
            waited = {}

            def wait_for(d):
                key = id(d.sem)
                if waited.get(key, 0) >= d.val:
                    return
                eng.wait_ge(d.sem, d.val)
                waited[key] = d.val

            for o in self.ops[ename]:
                for d in o.deps:
                    wait_for(d)
                ins = o.fn(eng)
                if o.needs_inc:
                    ins.then_inc(o.sem, 16 if o.is_dma else 1)
            if ename == "sp":
                for d in final_deps:
                    wait_for(d)

        with nc.Block() as block:
            @block.tensor
            def _(e):
                run_engine("pe", e)

            @block.scalar
            def _(e):
                run_engine("act", e)

            @block.vector
            def _(e):
                run_engine("dve", e)

            @block.gpsimd
            def _(e):
                run_engine("pool", e)

            @block.sync
            def _(e):
                run_engine("sp", e)
        stack.close()


def _col8(v):
    return np.ascontiguousarray(v.reshape(8, 128).T)


def _col2(v):
    return np.ascontiguousarray(v.reshape(2, 128).T)


def _qperm():
    idx = np.zeros(256, np.int64)
    for r in range(2):
        for kh in range(2):
            for d in range(64):
                idx[r * 128 + kh * 64 + d] = (kh * 2 + r) * 64 + d
    return idx


_PRM_FIELDS = [
    ("gmix", 8), ("gffn", 8), ("ggn", 8), ("convw", 6), ("s5d", 2), ("qg", 1), ("kg", 1), ("sink", 2),
    ("hlb0", 2), ("hlb1", 2), ("og", 1),
    ("A_lre", 128), ("A_lim", 128), ("A_ldt", 2), ("A_bre", 128), ("A_bim", 128),
    ("A_cre", 2048), ("A_cim", 2048),
    ("B_lre", 8), ("B_lim", 8), ("B_ldt", 8), ("B_cre", 128), ("B_cim", 128),
]
_PRM_OFF = {}
_o = 0
for _n, _w in _PRM_FIELDS:
    _PRM_OFF[_n] = (_o, _w)
    _o += _w
NPRM = _o

_CST_FIELDS = [
    ("ident", 128), ("bones", 128),
    ("mgp0", 1), ("mgp1", 1), ("mB0", 1), ("mB1", 1),
    ("mg8", 8), ("tauA", 8), ("tauB", 8), ("cidx", 64),
    ("segm", 512), ("cmask", 64),
]
_CST_OFF = {}
_o = 0
for _n, _w in _CST_FIELDS:
    _CST_OFF[_n] = (_o, _w)
    _o += _w
NCST = _o


def _build_consts():
    c = np.zeros((128, NCST), np.float32)

    def put(name, arr):
        o, w = _CST_OFF[name]
        c[:, o:o + w] = np.asarray(arr, np.float32).reshape(128, w)

    p = np.arange(128)
    put("ident", np.eye(128))
    bo = np.zeros((128, 128))
    bo[:64, :64] = 1
    bo[64:, 64:] = 1
    put("bones", bo)
    put("mgp0", ((p // 16) % 2 == 0)[:, None])
    put("mgp1", ((p // 16) % 2 == 1)[:, None])
    put("mB0", (p < 64)[:, None])
    put("mB1", (p >= 64)[:, None])
    put("mg8", (p[:, None] // 16) == np.arange(8)[None, :])
    put("tauA", np.tile(np.arange(8.0)[None], (128, 1)))
    put("tauB", np.tile(np.arange(1.0, 9.0)[None], (128, 1)))
    put("cidx", np.tile(np.arange(1.0, 65.0)[None], (128, 1)))
    seg = np.ones((8, 64))
    seg[:, 0] = 0
    put("segm", np.tile(seg.reshape(1, 512), (128, 1)))
    s = (p % 64)[:, None]
    t = np.arange(64)[None, :]
    put("cmask", (s <= t))
    return c


def _build_params(inp, l):
    prm = np.zeros((128, NPRM), np.float32)

    def put(name, arr):
        o, w = _PRM_OFF[name]
        prm[:, o:o + w] = np.asarray(arr, np.float32).reshape(128, w)

    qp = _qperm()
    put("gmix", _col8(inp["norm_mix"][l]))
    put("gffn", _col8(inp["norm_ffn"][l]))
    gn = np.array(inp["group_norm"][l])
    gn[512:768] = np.array(inp["group_norm"][l])[512:768][qp]
    put("ggn", _col8(gn))
    cw = np.asarray(inp["conv_w"][l])
    put("convw", np.stack([_col2(cw[i]) for i in range(3)], axis=2).reshape(128, 6))
    put("s5d", _col2(np.asarray(inp["s5_d"][l])))
    put("qg", np.tile(np.asarray(inp["attn_q_norm"][l]), 2)[:, None])
    put("kg", np.tile(np.asarray(inp["attn_k_norm"][l]), 2)[:, None])
    sk = np.asarray(inp["attn_sinks"][l])
    put("sink", np.stack([np.repeat(sk[[0 * 2 + r, 1 * 2 + r]], 64) for r in range(2)], axis=1))
    put("hlb0", _col2(np.asarray(inp["hg_lower_bounds"][0])))
    put("hlb1", _col2(np.asarray(inp["hg_lower_bounds"][1])))
    put("og", np.tile(np.asarray(inp["hg_out_norm"][l]), 2)[:, None])
    lre, lim, ldt = (np.asarray(inp[k][l]) for k in ("s5_lam_re", "s5_lam_im", "s5_log_dt"))
    bre, bim = np.asarray(inp["s5_b_re"][l]), np.asarray(inp["s5_b_im"][l])
    cre, cim = np.asarray(inp["s5_c_re"][l]), np.asarray(inp["s5_c_im"][l])
    put("A_lre", np.repeat(lre.reshape(2, 8, 1, 64), 16, axis=2).transpose(1, 2, 0, 3).reshape(128, 128))
    put("A_lim", np.repeat(lim.reshape(2, 8, 1, 64), 16, axis=2).transpose(1, 2, 0, 3).reshape(128, 128))
    put("A_ldt", np.repeat(ldt.reshape(2, 8, 1), 16, axis=2).transpose(1, 2, 0).reshape(128, 2))
    put("A_bre", bre.reshape(2, 8, 64, 16).transpose(1, 3, 0, 2).reshape(128, 128))
    put("A_bim", bim.reshape(2, 8, 64, 16).transpose(1, 3, 0, 2).reshape(128, 128))
    put("A_cre", np.repeat(cre.reshape(2, 8, 1, 16, 64), 16, axis=2).transpose(1, 2, 0, 3, 4).reshape(128, 2048))
    put("A_cim", np.repeat(cim.reshape(2, 8, 1, 16, 64), 16, axis=2).transpose(1, 2, 0, 3, 4).reshape(128, 2048))
    put("B_lre", lre.reshape(8, 2, 64).transpose(1, 2, 0).reshape(128, 8))
    put("B_lim", lim.reshape(8, 2, 64).transpose(1, 2, 0).reshape(128, 8))
    put("B_ldt", np.repeat(ldt.reshape(8, 2, 1), 64, axis=2).transpose(1, 2, 0).reshape(128, 8))
    put("B_cre", cre.reshape(8, 2, 16, 64).transpose(1, 3, 0, 2).reshape(128, 128))
    put("B_cim", cim.reshape(8, 2, 16, 64).transpose(1, 3, 0, 2).reshape(128, 128))
    return prm


def _chunks_kn(W):
    K, N = W.shape
    out = []
    for cg in range(N // 512):
        for kg in range(K // 1024):
            blk = W[kg * 1024:(kg + 1) * 1024, cg * 512:(cg + 1) * 512]
            out.append(np.ascontiguousarray(blk.reshape(8, 128, 512).transpose(1, 0, 2)).reshape(128, 4096))
    return out


def _build_weights(inp):
    qp = _qperm()
    chunks = []
    for l in range(DEPTH):
        w_in = np.asarray(inp["w_in"][l])
        win = np.array(w_in)
        win[:, 1024:1280] = w_in[:, 1024:1280][:, qp]
        w_out = np.asarray(inp["w_out"][l])
        wout = np.array(w_out)
        wout[512:768, :] = w_out[512:768, :][qp, :]
        chunks += _chunks_kn(win)
        chunks += _chunks_kn(wout)
        chunks += _chunks_kn(np.asarray(inp["w_ff1"][l]))
        chunks += _chunks_kn(np.asarray(inp["w_ff2"][l]))
    wch = np.stack(chunks, axis=0).astype(np.float32)
    glu = np.stack([np.asarray(inp["s5_w_glu"][l]).reshape(2, 128, 512).transpose(1, 0, 2) for l in range(DEPTH)], axis=0)
    return wch, np.ascontiguousarray(glu, np.float32)


class Buf:
    def __init__(self, ap2d, keys):
        self.a = ap2d
        self.keys_ = keys

    def __getitem__(self, k):
        return self.a[k]

    def v3(self, b):
        return self.a.rearrange("p (a b) -> p a b", b=b)


def bc(ap, shape):
    return ap.broadcast_to(list(shape))


def build_program(n_seq=SEQ_PER_CORE, n_tiles=TILES_PER_SEQ, layers=(0, 1), taps=None, same_engine_sync=True,
                  stop_after=None, skip_prologue=False, list_schedule=True):
    taps = taps or {}

    class StopBuild(Exception):
        pass

    def CHK(name):
        if stop_after == name:
            raise StopBuild()
    nc = bass.Bass("TRN2", target_bir_lowering=False)
    P = Prog(nc, same_engine_sync=same_engine_sync)
    NL = DEPTH

    xT = nc.dram_tensor("xT", [SEQ_PER_CORE, DM, SEQ], F32, kind="ExternalInput").ap()
    wch = nc.dram_tensor("wch", [NL * NCHUNK, 128, 4096], F32, kind="ExternalInput").ap()
    glu_d = nc.dram_tensor("glu", [NL, 128, 2, 512], F32, kind="ExternalInput").ap()
    prm_d = nc.dram_tensor("prm", [NL, 128, NPRM], F32, kind="ExternalInput").ap()
    cst_d = nc.dram_tensor("cst", [128, NCST], F32, kind="ExternalInput").ap()
    oT = nc.dram_tensor("oT", [SEQ_PER_CORE, DM, SEQ], F32, kind="ExternalOutput").ap()
    wsc = nc.dram_tensor("wsc", [NL * NCHUNK, 128, 4096], BF16).ap()
    tap_out = {}
    for name, shape in taps.items():
        if name.startswith("_"):
            continue
        tap_out[name] = nc.dram_tensor("tap_" + name, list(shape), F32, kind="ExternalOutput").ap()
    s5m_d = nc.dram_tensor("s5m_d", [NL, 128, 10240], BF16).ap()
    tap_dmas = []

    def sb(name, shape, dt=F32):
        return nc.alloc_sbuf_tensor("s_" + name, list(shape), dt)

    NRING = 4
    ring = [sb(f"ring{i}", [128, 8, 512], BF16) for i in range(NRING)]
    xs = sb("xs", [128, 8, NT], F32)
    hbuf = sb("hbuf", [128, 8, NT], BF16)
    cst = sb("cst", [128, NCST], F32)
    NF, NB = 23, 32
    farena = sb("farena", [128, NF * NT], F32)
    barena = sb("barena", [128, NB * NT], BF16)
    fmap = [False] * NF
    bmap = [False] * NB

    stamp = {"fp": [0] * NF, "bp": [0] * NB}
    clock = [0]

    def _alloc(arena, amap, tag, n):
        best, best_key = None, None
        for i in range(len(amap) - n + 1):
            if not any(amap[i:i + n]):
                key = max(stamp[tag][i:i + n])
                if best is None or key < best_key:
                    best, best_key = i, key
        if best is None:
            raise RuntimeError(f"arena {tag} exhausted")
        i = best
        for j in range(i, i + n):
            amap[j] = True
        b = Buf(arena[:, i * NT:(i + n) * NT], [(tag, j) for j in range(i, i + n)])
        b.rng = (i, n)
        return b

    def falloc(n=1):
        return _alloc(farena, fmap, "fp", n)

    def balloc(n=1):
        return _alloc(barena, bmap, "bp", n)

    def free(b):
        i, n = b.rng
        amap = fmap if b.keys_[0][0] == "fp" else bmap
        clock[0] += 1
        for j in range(i, i + n):
            assert amap[j]
            amap[j] = False
            stamp[b.keys_[0][0]][j] = clock[0]

    banks = [nc.alloc_psum_tensor(f"bank{i}", [128, 512], F32) for i in range(8)]
    BK = [("bank", i) for i in range(8)]

    def cs(name):
        o, w = _CST_OFF[name]
        return cst[:, o:o + w]

    ident_bf = sb("ident_bf", [128, 128], BF16)
    bones_bf = sb("bones_bf", [128, 128], BF16)
    ones_bf = sb("ones_bf", [128, 128], BF16)
    sb_int = sb("sb_int", [128, 512], I32)
    Amz = sb("Amz", [128, 2048], BF16)

    s5m = sb("s5m", [128, 10240], BF16)
    L1v = s5m[:, 0:4096].rearrange("p (j r t c) -> p j r t c", j=2, r=2, t=8)
    L3v = s5m[:, 4096:8192].rearrange("p (r q t c) -> p r q t c", r=2, q=8, t=8)
    KLv = s5m[:, 8192:10240].rearrange("p (j t c) -> p j t c", j=2, t=8)
    L = []
    for l in range(NL):
        d = dict(
            sp=sb(f"sp{l}", [128, 48], F32),
            glu=sb(f"glu{l}", [128, 2, 512], BF16),
            L1=L1v,
            L3=L3v,
            KL=KLv,
            cosR=sb(f"cosR{l}", [128, 512], F32),
            sinR=sb(f"sinR{l}", [128, 512], F32),
            rtab=sb(f"rtab{l}", [128, 512], F32),
            r8=sb(f"r8_{l}", [128, 8], F32),
            zbuf=sb(f"zbuf{l}", [128, 2, NT + 2], F32),
            Er=sb(f"Er{l}", [128, 8, NC5 + 1], F32),
            Ei=sb(f"Ei{l}", [128, 8, NC5 + 1], F32),
            kbuf=sb(f"kbuf{l}", [128, 128 + NT], BF16),
            vbuf=sb(f"vbuf{l}", [128, 5, 128], BF16),
            S=[sb(f"S{l}_{j}", [128, 64], F32) for j in range(2)],
            S2=[sb(f"S2{l}_{j}", [128, 64], F32) for j in range(2)],
        )
        L.append(d)
    SPC = dict(gmix=0, gffn=8, ggn=16, convw=24, s5d=30, qgs=32, kg=33, esink=34, lb=36, oml=38, og=40)

    def _nfree(ap):
        n = 1
        for d_ in ap.shape[1:]:
            n *= d_
        return n

    def _cost(eng, ap):
        n = _nfree(ap)
        if eng == "act":
            return 0.15 + n * 0.00083
        if eng == "pool":
            return 0.2 + n * 0.0021
        return 0.12 + n * 0.00104

    def ACT(out, in_, func, reads, writes, scale=1.0, bias=None):
        if bias is None:
            return P.op("act", lambda e: e.activation(out=out, in_=in_, func=func, scale=scale), reads, writes, cost=_cost("act", out))
        return P.op("act", lambda e: e.activation(out=out, in_=in_, func=func, scale=scale, bias=bias), reads, writes, cost=_cost("act", out))

    def TT(eng, out, a, b, op, reads, writes):
        return P.op(eng, lambda e: e.tensor_tensor(out=out, in0=a, in1=b, op=op), reads, writes, cost=_cost(eng, out))

    def TS(eng, out, a, s1, op0, reads, writes, s2=None, op1=None):
        if op1 is None:
            return P.op(eng, lambda e: e.tensor_scalar(out=out, in0=a, scalar1=s1, scalar2=None, op0=op0), reads, writes, cost=_cost(eng, out))
        return P.op(eng, lambda e: e.tensor_scalar(out=out, in0=a, scalar1=s1, scalar2=s2, op0=op0, op1=op1), reads, writes, cost=_cost(eng, out))

    def STT(out, in0, scalar, in1, op0, op1, reads, writes):
        return P.op("dve", lambda e: e.scalar_tensor_tensor(out=out, in0=in0, scalar=scalar, in1=in1, op0=op0, op1=op1), reads, writes,
                    cost=_cost("dve", out))

    def CP(eng, out, in_, reads, writes):
        if eng == "act":
            return P.op("act", lambda e: e.activation(out=out, in_=in_, func=AF.Copy), reads, writes, cost=_cost("act", out))
        return P.op(eng, lambda e: e.tensor_copy(out=out, in_=in_), reads, writes, cost=_cost(eng, out))

    def RECIP(out, in_, reads, writes):
        return P.op("dve", lambda e: e.reciprocal(out=out, in_=in_), reads, writes, cost=_cost("dve", out))

    def MM(mms, reads, writes, force=False):
        def fn(e):
            ins = None
            for m in mms:
                if m.get("tp") is not None:
                    ins = e.matmul(m["out"], lhsT=m["lhsT"], rhs=m["rhs"], start=m["start"], stop=m["stop"], skip_group_check=True,
                                   tile_position=m["tp"])
                else:
                    ins = e.matmul(m["out"], lhsT=m["lhsT"], rhs=m["rhs"], start=m["start"], stop=m["stop"], skip_group_check=True)
            return ins
        c_ = 0.0
        for m in mms:
            c_ += max(_nfree(m["rhs"]), 64) / 2400.0 + 0.045
        return P.op("pe", fn, reads, writes, force=force, cost=c_)

    def mm(out, lhsT, rhs, start=True, stop=True, tp=None):
        return dict(out=out, lhsT=lhsT, rhs=rhs, start=start, stop=stop, tp=tp)

    def TAP(name, idx, src_ap, reads):
        if name in tap_out:
            o = P.dma("sp", tap_out[name][idx], src_ap, reads=reads, writes=[("tap", name, idx)], chan=("tap", name))
            tap_dmas.append(o)

    P.dma("sp", cst[:], cst_d, writes=["cst"], chan="cst")
    CP("dve", ident_bf[:], cs("ident"), ["cst"], ["ident_bf"])
    CP("dve", bones_bf[:], cs("bones"), ["cst"], ["bones_bf"])
    P.op("pool", lambda e: e.memset(ones_bf[:], 1.0), writes=["ones_bf"])
    P.op("pool", lambda e: e.memset(Amz[:, :], 0.0), writes=["Amz"])

    for l in layers:
        for c in range(NCHUNK):
            ci = l * NCHUNK + c
            P.dma("pool", wsc[ci].rearrange("p (a b) -> p a b", b=2048), wch[ci].rearrange("p (a b) -> p a b", b=2048),
                  writes=[("wsc", ci)], chan=("wcast", ci % 4))

    prm_sb = falloc(2)
    _ac_lo = _PRM_OFF["A_cre"][0]
    _ac_hi = _PRM_OFF["A_cim"][0] + 2048
    NSMALL = NPRM - 4096
    assert NSMALL <= 2 * NT

    def prologue_layer(l):
        d = L[l]
        sp = d["sp"]
        SPK = ("sp", l)
        P.dma("sp", prm_sb[:, 0:_ac_lo], prm_d[l, :, 0:_ac_lo], writes=[prm_sb], chan="prm")
        P.dma("sp", prm_sb[:, _ac_lo:NSMALL], prm_d[l, :, _ac_hi:NPRM], writes=[prm_sb], chan="prm2")

        def pf(name, a=0, b=None):
            o, w = _PRM_OFF[name]
            if o >= _ac_hi:
                o -= 4096
            b = w if b is None else b
            return prm_sb[:, o + a:o + b]

        RD = [prm_sb, "cst"]
        for nm in ("gmix", "gffn", "ggn", "convw", "s5d", "kg", "og"):
            w = _PRM_OFF[nm][1]
            CP("dve", sp[:, SPC[nm]:SPC[nm] + w], pf(nm), RD, [SPK])
        TS("dve", sp[:, SPC["qgs"]:SPC["qgs"] + 1], pf("qg"), 0.125, ALU.mult, RD, [SPK])
        ACT(sp[:, SPC["esink"]:SPC["esink"] + 2], pf("sink"), AF.Exp, RD, [SPK])
        if l == 0:
            P.op("pool", lambda e: e.memset(sp[:, SPC["lb"]:SPC["lb"] + 2], 0.0), writes=[SPK])
        else:
            tmp = falloc()
            TT("dve", tmp[:, 0:2], pf("hlb1"), pf("hlb0"), ALU.subtract, RD, [tmp])
            ACT(sp[:, SPC["lb"]:SPC["lb"] + 2], tmp[:, 0:2], AF.Sigmoid, [tmp], [SPK])
            free(tmp)
        TS("dve", sp[:, SPC["oml"]:SPC["oml"] + 2], sp[:, SPC["lb"]:SPC["lb"] + 2], -1.0, ALU.mult, [SPK], [SPK], s2=1.0, op1=ALU.add)
        gl32 = falloc(2)
        P.dma("sp", gl32[:, 0:1024].rearrange("p (k n) -> p k n", n=512), glu_d[l], writes=[gl32], chan="glu")
        CP("dve", d["glu"][:, 0, :], gl32[:, 0:512], [gl32], [("glu", l)])
        CP("dve", d["glu"][:, 1, :], gl32[:, 512:1024], [gl32], [("glu", l)])
        free(gl32)

        def trig(u, n, cos_out, sin_out, ub, cb, sbk, shape3=None):
            t1 = falloc(); t2 = falloc()
            for (shift, outv, ob) in ((0.0, sin_out, sbk), (0.25, cos_out, cb)):
                TS("dve", t1[:, 0:n], u, shift + 64.0, ALU.add, [ub], [t1])
                CP("dve", sb_int[:, 0:n], t1[:, 0:n], [t1], ["sb_int"])
                CP("dve", t2[:, 0:n], sb_int[:, 0:n], ["sb_int"], [t2])
                TT("dve", t1[:, 0:n], t1[:, 0:n], t2[:, 0:n], ALU.subtract, [t1, t2], [t1])
                TS("dve", t2[:, 0:n], t1[:, 0:n], 0.5, ALU.is_gt, [t1], [t2])
                TT("dve", t1[:, 0:n], t1[:, 0:n], t2[:, 0:n], ALU.subtract, [t1, t2], [t1])
                TS("dve", t2[:, 0:n], t1[:, 0:n], -0.5, ALU.is_lt, [t1], [t2])
                TT("dve", t1[:, 0:n], t1[:, 0:n], t2[:, 0:n], ALU.add, [t1, t2], [t1])
                ACT(outv, t1[:, 0:n], AF.Sin, [t1], [ob], scale=TWO_PI)
            free(t1); free(t2)
            return None

        a_lr = falloc(); a_dt = falloc(); a_e1 = falloc(); a_th = falloc()
        TS("dve", a_lr[:, 0:128], pf("A_lre"), -1e-4, ALU.min, RD, [a_lr])
        ACT(a_dt[:, 0:2], pf("A_ldt"), AF.Exp, RD, [a_dt])
        for j in range(2):
            sl = slice(j * 64, (j + 1) * 64)
            TS("dve", a_e1[:, sl], a_lr[:, sl], a_dt[:, j:j + 1], ALU.mult, [a_lr, a_dt], [a_e1])
            TS("dve", a_th[:, sl], pf("A_lim")[:, sl], a_dt[:, j:j + 1], ALU.mult, RD + [a_dt], [a_th], s2=1.0 / TWO_PI, op1=ALU.mult)
        for j in range(2):
            sl = slice(j * 64, (j + 1) * 64)
            u = falloc(); me = falloc(); cc = falloc(); ss = falloc(); mr = falloc(); mi = falloc()
            u3, me3 = u.v3(64), me.v3(64)
            tau_b = bc(cs("tauA").unsqueeze(2), [128, 8, 64])
            TT("dve", u3, bc(a_th[:, sl].unsqueeze(1), [128, 8, 64]), tau_b, ALU.mult, [a_th, "cst"], [u])
            TT("dve", me3, bc(a_e1[:, sl].unsqueeze(1), [128, 8, 64]), tau_b, ALU.mult, [a_e1, "cst"], [me])
            ACT(me[:, :], me[:, :], AF.Exp, [me], [me])
            trig(u[:, :], 512, cc[:, :], ss[:, :], u, cc, ss)
            TT("dve", mr[:, :], me[:, :], cc[:, :], ALU.mult, [me, cc], [mr])
            TT("dve", mi[:, :], me[:, :], ss[:, :], ALU.mult, [me, ss], [mi])
            free(u); free(me); free(cc); free(ss)
            w = falloc()
            W = lambda i: w[:, i * 64:(i + 1) * 64]
            lr_, li_ = a_lr[:, sl], pf("A_lim")[:, sl]
            TT("dve", W(0), lr_, lr_, ALU.mult, [a_lr], [w])
            TT("dve", W(2), li_, li_, ALU.mult, RD, [w])
            TT("dve", W(0), W(0), W(2), ALU.add, [w], [w])
            RECIP(W(0), W(0), [w], [w])
            TS("dve", W(1), mr[:, 64:128], -1.0, ALU.add, [mr], [w])
            TT("dve", W(2), W(1), lr_, ALU.mult, [w, a_lr], [w])
            TT("dve", W(7), mi[:, 64:128], li_, ALU.mult, [mi] + RD, [w])
            TT("dve", W(2), W(2), W(7), ALU.add, [w], [w])
            TT("dve", W(3), W(2), W(0), ALU.mult, [w], [w])
            TT("dve", W(2), mi[:, 64:128], lr_, ALU.mult, [mi, a_lr], [w])
            TT("dve", W(7), W(1), li_, ALU.mult, [w] + RD, [w])
            TT("dve", W(2), W(2), W(7), ALU.subtract, [w], [w])
            TT("dve", W(4), W(2), W(0), ALU.mult, [w], [w])
            bre_, bim_ = pf("A_bre")[:, sl], pf("A_bim")[:, sl]
            TT("dve", W(5), W(3), bre_, ALU.mult, [w] + RD, [w])
            TT("dve", W(7), W(4), bim_, ALU.mult, [w] + RD, [w])
            TT("dve", W(5), W(5), W(7), ALU.subtract, [w], [w])
            TT("dve", W(6), W(3), bim_, ALU.mult, [w] + RD, [w])
            TT("dve", W(7), W(4), bre_, ALU.mult, [w] + RD, [w])
            TT("dve", W(6), W(6), W(7), ALU.add, [w], [w])
            p1r = falloc(); p1i = falloc(); t = falloc()
            bbr_b = bc(W(5).unsqueeze(1), [128, 8, 64]); bbi_b = bc(W(6).unsqueeze(1), [128, 8, 64])
            TT("dve", p1r.v3(64), mr.v3(64), bbr_b, ALU.mult, [mr, w], [p1r])
            TT("dve", t.v3(64), mi.v3(64), bbi_b, ALU.mult, [mi, w], [t])
            TT("dve", p1r[:, :], p1r[:, :], t[:, :], ALU.subtract, [p1r, t], [p1r])
            TT("dve", p1i.v3(64), mr.v3(64), bbi_b, ALU.mult, [mr, w], [p1i])
            TT("dve", t.v3(64), mi.v3(64), bbr_b, ALU.mult, [mi, w], [t])
            TT("dve", p1i[:, :], p1i[:, :], t[:, :], ALU.add, [p1i, t], [p1i])
            free(w); free(mr); free(mi)
            for ri, src in ((0, p1r), (1, p1i)):
                for gp in range(2):
                    TS("dve", d["L1"][:, j, ri, :, gp * 64:(gp + 1) * 64], src.v3(64), cs("mgp%d" % gp), ALU.mult, [src, "cst"], ["s5m"])
            kc = falloc()
            t2 = falloc()
            acr = falloc(2); aci = falloc(2)
            ocr, oci = _PRM_OFF["A_cre"][0], _PRM_OFF["A_cim"][0]
            P.dma("sp", acr[:, 0:1024], prm_d[l, :, ocr + j * 1024:ocr + (j + 1) * 1024], writes=[acr], chan="acr")
            P.dma("sp", aci[:, 0:1024], prm_d[l, :, oci + j * 1024:oci + (j + 1) * 1024], writes=[aci], chan="aci")
            for ho in range(16):
                cr_b = bc(acr[:, ho * 64:(ho + 1) * 64].unsqueeze(1), [128, 8, 64])
                ci_b = bc(aci[:, ho * 64:(ho + 1) * 64].unsqueeze(1), [128, 8, 64])
                TT("dve", t.v3(64), p1r.v3(64), cr_b, ALU.mult, [p1r, acr], [t])
                TT("dve", t2.v3(64), p1i.v3(64), ci_b, ALU.mult, [p1i, aci], [t2])
                TT("dve", t[:, :], t[:, :], t2[:, :], ALU.subtract, [t, t2], [t])
                P.op("dve", lambda e, ho=ho, kc=kc, t=t: e.tensor_reduce(out=kc[:, 0:128].rearrange("p (a b) -> p a b", b=16)[:, :, ho], in_=t.v3(64),
                                                                         axis=AX.X, op=ALU.add), [t], [kc])
            free(t2); free(p1r); free(p1i); free(acr); free(aci)
            klf = falloc(2)
            for g in range(8):
                o_, _w = _CST_OFF["mg8"]
                TS("dve", klf[:, 0:1024].rearrange("p (a b) -> p a b", b=128)[:, :, g * 16:(g + 1) * 16],
                   kc[:, 0:128].rearrange("p (a b) -> p a b", b=16), cst[:, o_ + g:o_ + g + 1], ALU.mult, [kc, "cst"], [klf])
            STT(klf[:, 0:128], cs("ident"), sp[:, SPC["s5d"] + j:SPC["s5d"] + j + 1], klf[:, 0:128], ALU.mult, ALU.add, ["cst", SPK, klf], [klf])
            CP("dve", d["KL"][:, j, :, :], klf[:, 0:1024].rearrange("p (a b) -> p a b", b=128), [klf], ["s5m"])
            free(kc); free(klf); free(t)
        free(a_lr); free(a_dt); free(a_e1); free(a_th)

        b_ = falloc()
        Bc = lambda i: b_[:, i * 8:(i + 1) * 8]
        TS("dve", Bc(0), pf("B_lre"), -1e-4, ALU.min, RD, [b_])
        ACT(Bc(1), pf("B_ldt"), AF.Exp, RD, [b_])
        TT("dve", Bc(2), Bc(0), Bc(1), ALU.mult, [b_], [b_])
        TT("dve", Bc(3), pf("B_lim"), Bc(1), ALU.mult, RD + [b_], [b_])
        TS("dve", Bc(3), Bc(3), 1.0 / TWO_PI, ALU.mult, [b_], [b_])
        u = falloc(); me = falloc(); cc = falloc(); ss = falloc()
        tauB_b = bc(cs("tauB").unsqueeze(1), [128, 8, 8])
        TT("dve", u[:, 0:64].rearrange("p (a b) -> p a b", b=8), bc(Bc(3).unsqueeze(2), [128, 8, 8]), tauB_b, ALU.mult, [b_, "cst"], [u])
        TT("dve", me[:, 0:64].rearrange("p (a b) -> p a b", b=8), bc(Bc(2).unsqueeze(2), [128, 8, 8]), tauB_b, ALU.mult, [b_, "cst"], [me])
        ACT(me[:, 0:64], me[:, 0:64], AF.Exp, [me], [me])
        trig(u[:, 0:64], 64, cc[:, 0:64], ss[:, 0:64], u, cc, ss)
        TT("dve", cc[:, 0:64], cc[:, 0:64], me[:, 0:64], ALU.mult, [cc, me], [cc])
        TT("dve", ss[:, 0:64], ss[:, 0:64], me[:, 0:64], ALU.mult, [ss, me], [ss])
        for qh in range(2):
            cr_b = bc(pf("B_cre")[:, qh * 64:(qh + 1) * 64].rearrange("p (q h) -> p q h", h=16).unsqueeze(2), [128, 4, 8, 16])
            ci_b = bc(pf("B_cim")[:, qh * 64:(qh + 1) * 64].rearrange("p (q h) -> p q h", h=16).unsqueeze(2), [128, 4, 8, 16])
            mr_b = bc(cc[:, qh * 32:(qh + 1) * 32].rearrange("p (q t) -> p q t", t=8).unsqueeze(3), [128, 4, 8, 16])
            mi_b = bc(ss[:, qh * 32:(qh + 1) * 32].rearrange("p (q t) -> p q t", t=8).unsqueeze(3), [128, 4, 8, 16])
            c1 = falloc(); c2 = falloc()
            v4 = lambda b: b[:, :].rearrange("p (q t h) -> p q t h", t=8, h=16)
            TT("dve", v4(c1), cr_b, mr_b, ALU.mult, RD + [cc], [c1])
            TT("dve", v4(c2), ci_b, mi_b, ALU.mult, RD + [ss], [c2])
            TT("dve", c1[:, :], c1[:, :], c2[:, :], ALU.subtract, [c1, c2], [c1])
            for gp in range(2):
                TS("dve", d["L3"][:, 0, qh * 4:(qh + 1) * 4, :, gp * 16:(gp + 1) * 16], v4(c1), cs("mB%d" % gp), ALU.mult, [c1, "cst"], ["s5m"])
            TT("dve", v4(c1), cr_b, mi_b, ALU.mult, RD + [ss], [c1])
            TT("dve", v4(c2), ci_b, mr_b, ALU.mult, RD + [cc], [c2])
            TT("dve", c1[:, :], c1[:, :], c2[:, :], ALU.add, [c1, c2], [c1])
            for gp in range(2):
                TS("dve", d["L3"][:, 1, qh * 4:(qh + 1) * 4, :, gp * 16:(gp + 1) * 16], v4(c1), cs("mB%d" % gp), ALU.mult, [c1, "cst"], ["s5m"],
                   s2=-1.0, op1=ALU.mult)
            free(c1); free(c2)
        ACT(d["r8"][:, :], Bc(2), AF.Exp, [b_], [("r8", l)], scale=8.0)
        TT("dve", d["rtab"][:, :].rearrange("p (q c) -> p q c", c=64), bc(d["r8"][:, :].unsqueeze(2), [128, 8, 64]),
           cs("segm").rearrange("p (q c) -> p q c", c=64), ALU.mult, [("r8", l), "cst"], [("rtab", l)])
        TS("dve", Bc(4), Bc(3), 8.0, ALU.mult, [b_], [b_], s2=64.0, op1=ALU.add)
        CP("dve", sb_int[:, 0:8], Bc(4), [b_], ["sb_int"])
        CP("dve", Bc(5), sb_int[:, 0:8], ["sb_int"], [b_])
        TT("dve", Bc(4), Bc(4), Bc(5), ALU.subtract, [b_], [b_])
        TS("dve", Bc(5), Bc(4), 0.5, ALU.is_gt, [b_], [b_])
        TT("dve", Bc(4), Bc(4), Bc(5), ALU.subtract, [b_], [b_])
        TS("dve", Bc(5), Bc(4), -0.5, ALU.is_lt, [b_], [b_])
        TT("dve", Bc(4), Bc(4), Bc(5), ALU.add, [b_], [b_])
        TT("dve", u.v3(64), bc(Bc(4).unsqueeze(2), [128, 8, 64]), bc(cs("cidx").unsqueeze(1), [128, 8, 64]), ALU.mult, [b_, "cst"], [u])
        trig(u[:, :], 512, d["cosR"][:, :], d["sinR"][:, :], u, ("cosR", l), ("sinR", l))
        free(u); free(me); free(cc); free(ss); free(b_)

    def prologue_taps(l):
        d = L[l]
        if "KL" in tap_out:
            tf = falloc(4)
            CP("dve", tf[:, 0:2048], s5m[:, 8192:10240], ["s5m"], [tf])
            TAP("KL", l, tf[:, 0:2048], [tf]); free(tf)
        if "L1" in tap_out:
            tf = falloc(8)
            CP("dve", tf[:, 0:4096], s5m[:, 0:4096], ["s5m"], [tf])
            TAP("L1", l, tf[:, 0:4096], [tf]); free(tf)
        if "L3" in tap_out:
            tf = falloc(8)
            CP("dve", tf[:, 0:4096], s5m[:, 4096:8192], ["s5m"], [tf])
            TAP("L3", l, tf[:, 0:4096], [tf]); free(tf)
        if "rot" in tap_out:
            TAP("rot", (l, 0), d["cosR"][:, :], [("cosR", l)])
            TAP("rot", (l, 1), d["sinR"][:, :], [("sinR", l)])
            TAP("rot", (l, 2), d["rtab"][:, :], [("rtab", l)])

    for l in layers:
        prologue_layer(l)
        prologue_taps(l)
        P.dma("sp", s5m_d[l], s5m[:, :], reads=["s5m"], writes=[("s5m_d", l)], chan="s5m_st")
    free(prm_sb)

    tile_list = [(s, ti, l) for s in range(n_seq) for ti in range(n_tiles) for l in layers]
    chunk_seq = [(l, c) for (s, ti, l) in tile_list for c in range(NCHUNK)]
    ring_state = dict(issued=0, used=0)

    def issue_next():
        n = ring_state["issued"]
        if n >= len(chunk_seq):
            return
        l, c = chunk_seq[n]
        slot = n % NRING
        ci = l * NCHUNK + c
        P.dma("sp", ring[slot][:, :, :].rearrange("p k n -> p (k n)"), wsc[ci], reads=[("wsc", ci)], writes=[("ring", slot)], chan=("ring", slot))
        ring_state["issued"] += 1

    def get_chunk(l, c):
        n = ring_state["used"]
        assert chunk_seq[n] == (l, c), (chunk_seq[n], l, c)
        while ring_state["issued"] <= n:
            issue_next()
        return n % NRING

    def release_chunk():
        ring_state["used"] += 1
        while ring_state["issued"] < min(len(chunk_seq), ring_state["used"] + NRING):
            issue_next()

    for _ in range(NRING):
        issue_next()

    def rmsnorm_to_h(l, gcol):
        sp = L[l]["sp"]
        for k in range(8):
            sq = balloc()
            ACT(sq[:, :], xs[:, k, :], AF.Square, [("xs", k)], [sq])
            MM([mm(banks[7][:, :], ones_bf[:, :], sq[:, :], start=(k == 0), stop=(k == 7))], [sq, "ones_bf"], [BK[7]])
            free(sq)
        rs = falloc()
        ACT(rs[:, :], banks[7][:, :], AF.Sqrt, [BK[7]], [rs], scale=1.0 / DM, bias=EPS)
        RECIP(rs[:, :], rs[:, :], [rs], [rs])
        for k in range(8):
            STT(hbuf[:, k, :], xs[:, k, :], sp[:, gcol + k:gcol + k + 1], rs[:, :], ALU.mult, ALU.mult, [("xs", k), ("sp", l), rs], [("h", k)])
        free(rs)

    HK = [("h", k) for k in range(8)]

    def proj_fm(slot, m, bank, split=False):
        if split:
            for k in range(8):
                MM([mm(banks[bank][:, :], ring[slot][:, k, m * 128:(m + 1) * 128], hbuf[:, k, :], start=(k == 0), stop=(k == 7))],
                   [("ring", slot), HK[k]], [BK[bank]])
            return
        MM([mm(banks[bank][:, :], ring[slot][:, k, m * 128:(m + 1) * 128], hbuf[:, k, :], start=(k == 0), stop=(k == 7)) for k in range(8)],
           [("ring", slot)] + HK, [BK[bank]])

    def layer_tile(s, ti, l):
        d = L[l]
        sp = d["sp"]
        SPK = ("sp", l)
        first = (ti == 0)
        t0 = ti * NT
        if l == layers[0]:
            for k in range(8):
                P.dma("act", xs[:, k, :], xT[s, k * 128:(k + 1) * 128, t0:t0 + NT], writes=[("xs", k)], chan=("xs", k))
        if first:
            P.op("pool", lambda e: e.memset(d["zbuf"][:, :, 0:2], 0.0), writes=[("zbuf", l)])
            P.op("pool", lambda e: e.memset(d["Er"][:, :, 0:1], 0.0), writes=[("Er", l)])
            P.op("pool", lambda e: e.memset(d["Ei"][:, :, 0:1], 0.0), writes=[("Ei", l)])
            for j in range(2):
                P.op("pool", lambda e, j=j: e.memset(d["S"][j][:, :], 0.0), writes=[("S", l, j)])
        P.dma("act", s5m[:, :], s5m_d[l], reads=[("s5m_d", l)], writes=["s5m"], chan="s5m_ld")
        rmsnorm_to_h(l, SPC["gmix"])
        CHK("norm1")
        Y = [falloc() for _ in range(8)]
        YN = [balloc() for _ in range(8)]

        def group_norm(gI, ssb):
            for i in range(2):
                sq = balloc()
                ACT(sq[:, :], Y[2 * gI + i][:, :], AF.Square, [Y[2 * gI + i]], [sq])
                MM([mm(banks[ssb][:, :], ones_bf[:, :], sq[:, :], start=(i == 0), stop=(i == 1))], [sq, "ones_bf"], [BK[ssb]])
                free(sq)
            rs = falloc()
            ACT(rs[:, :], banks[ssb][:, :], AF.Sqrt, [BK[ssb]], [rs], scale=1.0 / 256, bias=EPS)
            RECIP(rs[:, :], rs[:, :], [rs], [rs])
            for i in range(2):
                k = 2 * gI + i
                STT(YN[k][:, :], Y[k][:, :], sp[:, SPC["ggn"] + k:SPC["ggn"] + k + 1], rs[:, :], ALU.mult, ALU.mult, [Y[k], SPK, rs], [YN[k]])
            free(rs)
            free(Y[2 * gI]); free(Y[2 * gI + 1])

        def chainX():
            slot = get_chunk(l, 0)
            hsb = [falloc() for _ in range(2)]
            bsb = [falloc() for _ in range(2)]
            for j in range(2):
                proj_fm(slot, j, j, split=(j == 0))
                CP("act", hsb[j][:, :], banks[j][:, :], [BK[j]], [hsb[j]])
            for j in range(2):
                proj_fm(slot, 2 + j, j)
                CP("act", bsb[j][:, :], banks[j][:, :], [BK[j]], [bsb[j]])
            release_chunk()
            slot = get_chunk(l, 1)
            zb = d["zbuf"]
            for j in range(2):
                proj_fm(slot, j, j)
                TT("dve", zb[:, j, 2:NT + 2], banks[j][:, :], hsb[j][:, :], ALU.mult, [BK[j], hsb[j]], [("zbuf", l)])
            ubf = balloc(2)
            for j in range(2):
                proj_fm(slot, 2 + j, j)
                CP("act", ubf[:, j * NT:(j + 1) * NT].rearrange("p (t c) -> p t c", c=NC5), banks[j][:, :].rearrange("p (c t) -> p t c", t=TS5),
                   [BK[j]], [ubf])
            release_chunk()
            yield
            for j in range(2):
                acc = falloc()
                cw = lambda i: sp[:, SPC["convw"] + j * 3 + i:SPC["convw"] + j * 3 + i + 1]
                TS("pool", acc[:, :], zb[:, j, 2:NT + 2], cw(2), ALU.mult, [("zbuf", l), SPK], [acc])
                STT(acc[:, :], zb[:, j, 1:NT + 1], cw(1), acc[:, :], ALU.mult, ALU.add, [("zbuf", l), SPK, acc], [acc])
                STT(acc[:, :], zb[:, j, 0:NT], cw(0), acc[:, :], ALU.mult, ALU.add, [("zbuf", l), SPK, acc], [acc])
                TT("pool", Y[j][:, :], acc[:, :], bsb[j][:, :], ALU.mult, [acc, bsb[j]], [Y[j]])
                free(acc)
            P.op("pool", lambda e: e.tensor_copy(out=d["zbuf"][:, :, 0:2], in_=d["zbuf"][:, :, NT:NT + 2]), [("zbuf", l)], [("zbuf", l)])
            for b_ in hsb + bsb:
                free(b_)
            yield
            group_norm(0, 1)
            yield
            ut = [ubf[:, j * NT:(j + 1) * NT] for j in range(2)]
            for ri in range(2):
                for qq in range(4):
                    mms = []
                    for j in range(2):
                        q = j * 4 + qq
                        for s_ in range(TS5):
                            mms.append(mm(banks[ri][:, q * NC5:(q + 1) * NC5], d["L1"][qq * 32:(qq + 1) * 32, j, ri, TS5 - 1 - s_, :],
                                          ut[j][qq * 32:(qq + 1) * 32, s_ * NC5:(s_ + 1) * NC5], start=(s_ == 0), stop=(s_ == TS5 - 1), tp=(qq * 32, 0)))
                    MM(mms, [ubf, "s5m"], [BK[ri]], force=True)
            yield
            Wr = falloc(); Wi = falloc(); t1 = falloc(); t2 = falloc()
            cosR, sinR, rtab = d["cosR"], d["sinR"], d["rtab"]
            CK, SK, RK = ("cosR", l), ("sinR", l), ("rtab", l)
            TT("dve", t1[:, :], banks[0][:, :], cosR[:, :], ALU.mult, [BK[0], CK], [t1])
            TT("dve", t2[:, :], banks[1][:, :], sinR[:, :], ALU.mult, [BK[1], SK], [t2])
            TT("pool", Wr[:, :], t1[:, :], t2[:, :], ALU.add, [t1, t2], [Wr])
            TT("dve", t1[:, :], banks[1][:, :], cosR[:, :], ALU.mult, [BK[1], CK], [t1])
            TT("dve", t2[:, :], banks[0][:, :], sinR[:, :], ALU.mult, [BK[0], SK], [t2])
            TT("pool", Wi[:, :], t1[:, :], t2[:, :], ALU.subtract, [t1, t2], [Wi])
            yield
            for (Wx, Ex, ek) in ((Wr, d["Er"], ("Er", l)), (Wi, d["Ei"], ("Ei", l))):
                TT("dve", t1[:, 0:8], d["r8"][:, :], Ex[:, :, 0], ALU.mult, [("r8", l), ek], [t1])
                TT("dve", Wx.v3(NC5)[:, :, 0], Wx.v3(NC5)[:, :, 0], t1[:, 0:8], ALU.add, [Wx, t1], [Wx])
            Fr = falloc(); Fi = falloc()
            for (Wx, Fx) in ((Wr, Fr), (Wi, Fi)):
                P.op("dve", lambda e, Wx=Wx, Fx=Fx: e.tensor_tensor_scan(out=Fx[:, :], data0=rtab[:, :], data1=Wx[:, :], initial=0.0,
                                                                        op0=ALU.mult, op1=ALU.add), [RK, Wx], [Fx])
            yield
            TT("dve", t1[:, :], Fr[:, :], cosR[:, :], ALU.mult, [Fr, CK], [t1])
            TT("dve", t2[:, :], Fi[:, :], sinR[:, :], ALU.mult, [Fi, SK], [t2])
            TT("pool", d["Er"][:, :, 1:NC5 + 1], t1.v3(NC5), t2.v3(NC5), ALU.subtract, [t1, t2], [("Er", l)])
            TT("dve", t1[:, :], Fi[:, :], cosR[:, :], ALU.mult, [Fi, CK], [t1])
            TT("dve", t2[:, :], Fr[:, :], sinR[:, :], ALU.mult, [Fr, SK], [t2])
            TT("pool", d["Ei"][:, :, 1:NC5 + 1], t1.v3(NC5), t2.v3(NC5), ALU.add, [t1, t2], [("Ei", l)])
            for b_ in (Wr, Wi, t1, t2, Fr, Fi):
                free(b_)
            Ebf = balloc(2)
            CP("act", Ebf[:, 0:NT].rearrange("p (q c) -> p q c", c=NC5), d["Er"][:, :, 0:NC5], [("Er", l)], [Ebf])
            CP("act", Ebf[:, NT:2 * NT].rearrange("p (q c) -> p q c", c=NC5), d["Ei"][:, :, 0:NC5], [("Ei", l)], [Ebf])
            P.op("pool", lambda e: e.tensor_copy(out=d["Er"][:, :, 0:1], in_=d["Er"][:, :, NC5:NC5 + 1]), [("Er", l)], [("Er", l)])
            P.op("pool", lambda e: e.tensor_copy(out=d["Ei"][:, :, 0:1], in_=d["Ei"][:, :, NC5:NC5 + 1]), [("Ei", l)], [("Ei", l)])
            yield
            E4 = [Ebf[:, ri * NT:(ri + 1) * NT].rearrange("p (q c) -> p q c", c=NC5) for ri in range(2)]
            for j in range(2):
                yb = banks[j]
                mms = []
                for tau in range(TS5):
                    mms.append(mm(yb[:, tau * NC5:NT], d["KL"][:, j, tau, :], ut[j][:, 0:(TS5 - tau) * NC5], start=(tau == 0), stop=False))
                for qq in range(4):
                    q = j * 4 + qq
                    for t_ in range(TS5):
                        for ri in range(2):
                            mms.append(mm(yb[qq * 32:(qq + 1) * 32, t_ * NC5:(t_ + 1) * NC5], d["L3"][:, ri, q, t_, :], E4[ri][:, q, :], start=False,
                                          stop=(qq == 3 and t_ == TS5 - 1 and ri == 1), tp=(0, qq * 32)))
                MM(mms, [ubf, Ebf, "s5m"], [BK[j]])
            yield
            gl = balloc(2)
            for j in range(2):
                ysb = falloc(); sq = falloc()
                ynat = banks[j][:, :].rearrange("p (t c) -> p c t", c=NC5)
                CP("act", ysb.v3(TS5), ynat, [BK[j]], [ysb])
                ACT(sq.v3(TS5), ynat, AF.Square, [BK[j]], [sq])
                TS("dve", sq[:, :], sq[:, :], 0.044715, ALU.mult, [sq], [sq], s2=1.0, op1=ALU.add)
                TT("dve", sq[:, :], sq[:, :], ysb[:, :], ALU.mult, [sq, ysb], [sq])
                ACT(sq[:, :], sq[:, :], AF.Sigmoid, [sq], [sq], scale=1.5957691216057308)
                TT("pool", gl[:, j * NT:(j + 1) * NT], ysb[:, :], sq[:, :], ALU.mult, [ysb, sq], [gl])
                free(ysb); free(sq)
                yield
            free(ubf); free(Ebf)
            for j in range(2):
                for (n, bk) in ((j, 0), (2 + j, 1)):
                    MM([mm(banks[bk][:, :], d["glu"][:, k, n * 128:(n + 1) * 128], gl[:, k * NT:(k + 1) * NT], start=(k == 0), stop=(k == 1)) for k in range(2)],
                       [gl, ("glu", l)], [BK[bk]])
                sg = falloc()
                ACT(sg[:, :], banks[1][:, :], AF.Sigmoid, [BK[1]], [sg])
                TT("dve", Y[2 + j][:, :], banks[0][:, :], sg[:, :], ALU.mult, [BK[0], sg], [Y[2 + j]])
                free(sg)
                yield
            free(gl)
            group_norm(1, 1)

        def chainY():
            slot = get_chunk(l, 2)
            for m in range(3):
                proj_fm(slot, m, 2 + m)
            MM([mm(banks[5][:, blk * 128:(blk + 1) * 128], hbuf[:, k, blk * 128:(blk + 1) * 128], ring[slot][:, k, 384:512], start=(k == 0), stop=(k == 7))
                for blk in range(4) for k in range(8)], [("ring", slot)] + HK, [BK[5]])
            release_chunk()
            kbuf, vbuf = d["kbuf"], d["vbuf"]
            KB, VB = ("kbuf", l), ("vbuf", l)
            qn = balloc(2)
            CP("act", vbuf[:, 1:5, :], banks[5][:, :].rearrange("p (b f) -> p b f", f=128), [BK[5]], [VB])
            yield
            for (bk, gcol, outv, okey, ssb) in ((2, SPC["qgs"], qn[:, 0:NT], qn, 6), (3, SPC["qgs"], qn[:, NT:2 * NT], qn, 7),
                                                (4, SPC["kg"], kbuf[:, 128:128 + NT], KB, 5)):
                sq = balloc(); rs = falloc()
                ACT(sq[:, :], banks[bk][:, :], AF.Square, [BK[bk]], [sq])
                MM([mm(banks[ssb][:, :], bones_bf[:, :], sq[:, :])], [sq, "bones_bf"], [BK[ssb]])
                ACT(rs[:, :], banks[ssb][:, :], AF.Sqrt, [BK[ssb]], [rs], scale=1.0 / 64, bias=EPS)
                RECIP(rs[:, :], rs[:, :], [rs], [rs])
                STT(outv, banks[bk][:, :], sp[:, gcol:gcol + 1], rs[:, :], ALU.mult, ALU.mult, [BK[bk], SPK, rs], [okey])
                free(sq); free(rs)
                yield
            jb_list = list(range(1 if first else 0, 5))
            for jbi, jb in enumerate(jb_list):
                qlo, qhi = max(0, 2 * jb - 2), min(8, 2 * jb + 2)
                off = (qlo - (2 * jb - 2)) * 64
                ncol = (qhi - qlo) * 64
                for kh in range(2):
                    MM([mm(banks[2 + kh][:, r * 256 + off:r * 256 + off + ncol], kbuf[kh * 64:(kh + 1) * 64, jb * 128:(jb + 1) * 128],
                           qn[kh * 64:(kh + 1) * 64, r * NT + qlo * 64:r * NT + qhi * 64]) for r in range(2)], [KB, qn], [BK[2 + kh]])
                pT = balloc(2)
                P.op("pool", lambda e, pT=pT: e.memset(pT[:, :], 0.0), writes=[pT])
                for kh in range(2):
                    for half, (c0, c1) in enumerate(((0, 192), (64, 256))):
                        a, b2 = max(c0, off), min(c1, off + ncol)
                        if b2 <= a:
                            continue
                        src = banks[2 + kh][half * 64:(half + 1) * 64, :].rearrange("p (r c) -> p r c", c=256)[:, :, a:b2]
                        dst = pT[half * 64:(half + 1) * 64, kh * 512:(kh + 1) * 512].rearrange("p (r c) -> p r c", c=256)[:, :, a:b2]
                        ACT(dst, src, AF.Exp, [BK[2 + kh]], [pT])
                for r in range(2):
                    mms_n, mms_d = [], []
                    for kh in range(2):
                        pv = pT[:, kh * 512 + r * 256:kh * 512 + (r + 1) * 256]
                        for part in range(2):
                            pair = jb - 1 + part
                            if pair < 0 or pair > 3:
                                continue
                            first_contrib = (part == 1) or (first and jb == 1)
                            last_contrib = (part == 0) or (jb == 4)
                            if part == 1 and jb == 4:
                                continue
                            cols = slice(pair * 128, (pair + 1) * 128)
                            mms_n.append(mm(banks[4 + r][kh * 64:(kh + 1) * 64, cols], vbuf[:, jb, kh * 64:(kh + 1) * 64], pv[:, part * 128:(part + 1) * 128],
                                            start=first_contrib, stop=last_contrib))
                            mms_d.append(mm(banks[6 + r][kh * 64:(kh + 1) * 64, cols], ones_bf[:, 0:64], pv[:, part * 128:(part + 1) * 128],
                                            start=first_contrib, stop=last_contrib))
                    MM(mms_n, [pT, VB], [BK[4 + r]])
                    MM(mms_d, [pT, "ones_bf"], [BK[6 + r]])
                free(pT)
                yield
            for r in range(2):
                rec = falloc()
                TS("dve", rec[:, :], banks[6 + r][:, :], sp[:, SPC["esink"] + r:SPC["esink"] + r + 1], ALU.add, [BK[6 + r], SPK], [rec])
                RECIP(rec[:, :], rec[:, :], [rec], [rec])
                TT("dve", Y[4 + r][:, :], banks[4 + r][:, :], rec[:, :], ALU.mult, [BK[4 + r], rec], [Y[4 + r]])
                free(rec)
            free(qn)
            P.op("pool", lambda e: e.tensor_copy(out=kbuf[:, 0:128], in_=kbuf[:, NT:NT + 128]), [KB], [KB])
            P.op("pool", lambda e: e.tensor_copy(out=vbuf[:, 0, :], in_=vbuf[:, 4, :]), [VB], [VB])
            yield
            group_norm(2, 2)
            yield
            slot = get_chunk(l, 3)
            for m in range(4):
                proj_fm(slot, m, 2 + m)
            release_chunk()
            qt = balloc(2); qh = balloc(4); kt = balloc(2); kend = balloc(2)
            e3s = []
            for j in range(2):
                sig = falloc(); lg = falloc(); kk = falloc(); B = falloc(); e1 = falloc(); e2 = falloc(); e3 = falloc()
                ACT(sig[:, :], banks[4 + j][:, :], AF.Sigmoid, [BK[4 + j]], [sig])
                TS("dve", sig[:, :], sig[:, :], sp[:, SPC["oml"] + j:SPC["oml"] + j + 1], ALU.mult, [sig, SPK], [sig],
                   s2=sp[:, SPC["lb"] + j:SPC["lb"] + j + 1], op1=ALU.add)
                ACT(lg[:, :], sig[:, :], AF.Ln, [sig], [lg])
                TS("pool", kk[:, :], sig[:, :], -1.0, ALU.mult, [sig], [kk], s2=1.0, op1=ALU.add)
                P.op("dve", lambda e, B=B, lg=lg: e.tensor_tensor_scan(out=B[:, :], data0=cs("segm"), data1=lg[:, :], initial=0.0,
                                                                      op0=ALU.mult, op1=ALU.add), ["cst", lg], [B])
                yield
                ACT(e3[:, :], B[:, :], AF.Exp, [B], [e3])
                TT("dve", lg.v3(64), B.v3(64), bc(B.v3(64)[:, :, 31:32], [128, 8, 64]), ALU.subtract, [B], [lg])
                ACT(e1[:, :], lg[:, :], AF.Exp, [lg], [e1])
                ACT(e2[:, :], lg[:, :], AF.Exp, [lg], [e2], scale=-1.0)
                TT("dve", qt[:, j * NT:(j + 1) * NT], banks[2 + j][:, :], e1[:, :], ALU.mult, [BK[2 + j], e1], [qt])
                for hh in range(2):
                    STT(qh[:, (j * 2 + hh) * NT:(j * 2 + hh + 1) * NT], banks[2 + j][:, :], cs("mB%d" % hh), e3[:, :], ALU.mult, ALU.mult,
                        [BK[2 + j], e3, "cst"], [qh])
                TT("pool", kt[:, j * NT:(j + 1) * NT], kk[:, :], e2[:, :], ALU.mult, [kk, e2], [kt])
                TT("pool", kend[:, j * NT:(j + 1) * NT].rearrange("p (b t) -> p b t", t=64), kt[:, j * NT:(j + 1) * NT].rearrange("p (b t) -> p b t", t=64),
                   bc(e1.v3(64)[:, :, 63:64], [128, 8, 64]), ALU.mult, [kt, e1], [kend])
                e3s.append(e3)
                for b_ in (sig, lg, kk, B, e1, e2):
                    free(b_)
                yield
            b6bf = banks[6][:, :].bitcast(BF16)
            def tr_fn(e):
                ins = None
                for bp in range(4):
                    for j in range(2):
                        ins = e.transpose(out=b6bf[:, (bp * 2 + j) * 128:(bp * 2 + j + 1) * 128], in_=kend[:, j * NT + bp * 128:j * NT + (bp + 1) * 128],
                                          identity=ident_bf[:, :])
                return ins
            P.op("pe", tr_fn, [kend, "ident_bf"], [BK[6]])
            kendT = balloc(2)
            CP("act", kendT[:, :], b6bf, [BK[6]], [kendT])
            free(kend)
            yield
            slot = get_chunk(l, 4)
            hib = (7, 2)
            for half in range(2):
                MM([mm(banks[hib[half]][:, bq * 256:(bq + 1) * 256], hbuf[:, k, (half * 2 + bq) * 128:(half * 2 + bq + 1) * 128], ring[slot][:, k, 0:256],
                       start=(k == 0), stop=(k == 7)) for bq in range(2) for k in range(8)], [("ring", slot)] + HK, [BK[hib[half]]])
            for j in range(2):
                proj_fm(slot, 2 + j, 3 + j)
            release_chunk()
            vT = balloc(2)
            for half in range(2):
                CP("act", vT[:, half * NT:(half + 1) * NT], banks[hib[half]][:, :], [BK[hib[half]]], [vT])
            sgs = []
            for j in range(2):
                sg = falloc()
                ACT(sg[:, :], banks[3 + j][:, :], AF.Silu, [BK[3 + j]], [sg])
                sgs.append(sg)
            yield
            for hp in range(2):
                mms = []
                for j in range(2):
                    for b in range(8):
                        bp, half = b // 2, b % 2
                        mms.append(mm(banks[5 + hp][half * 64:(half + 1) * 64, j * 256 + bp * 64:j * 256 + (bp + 1) * 64],
                                      kt[hp * 64:(hp + 1) * 64, j * NT + b * 64:j * NT + (b + 1) * 64],
                                      qt[hp * 64:(hp + 1) * 64, j * NT + b * 64:j * NT + (b + 1) * 64]))
                MM(mms, [kt, qt], [BK[5 + hp]])
            for hp in range(2):
                for half in range(2):
                    rows = slice(half * 64, (half + 1) * 64)
                    dst = Amz[rows, :].rearrange("p (j hp bp hf t) -> p j hp bp hf t", j=2, hp=2, bp=4, hf=2, t=64)[:, :, hp, :, half, :]
                    src = banks[5 + hp][rows, :].rearrange("p (j bp t) -> p j bp t", j=2, bp=4)
                    o_c, _w = _CST_OFF["cmask"]
                    msk = bc(cst[rows, o_c:o_c + 64].unsqueeze(1).unsqueeze(1), [64, 2, 4, 64])
                    TT("dve", dst, src, msk, ALU.mult, [BK[5 + hp], "cst"], ["Amz"])
            free(kt); free(qt)
            yield
            ub = (7, 2)
            for half in range(2):
                mms = []
                for j in range(2):
                    for hh in range(2):
                        h = 2 * j + hh
                        for bp in range(4):
                            mms.append(mm(banks[ub[half]][hh * 64:(hh + 1) * 64, j * 256 + bp * 64:j * 256 + (bp + 1) * 64],
                                          kendT[half * 64:(half + 1) * 64, bp * 256 + h * 64:bp * 256 + (h + 1) * 64],
                                          vT[half * 64:(half + 1) * 64, bp * 256 + h * 64:bp * 256 + (h + 1) * 64]))
                MM(mms, [kendT, vT], [BK[ub[half]]])
            free(kendT)
            yield
            Sall = [balloc() for _ in range(2)]
            for j in range(2):
                Sa, Sb = d["S"][j], d["S2"][j]
                KA, KB2 = ("S", l, j), ("S2", l, j)
                CP("act", Sall[j][:, 0:64], Sa[:, :], [KA], [Sall[j]])
                for b in range(8):
                    bp, half = b // 2, b % 2
                    src, dst, ks, kd = (Sa, Sb, KA, KB2) if b % 2 == 0 else (Sb, Sa, KB2, KA)
                    STT(dst[:, :], src[:, :], e3s[j][:, b * 64 + 63:b * 64 + 64], banks[ub[half]][:, j * 256 + bp * 64:j * 256 + (bp + 1) * 64], ALU.mult, ALU.add,
                        [ks, e3s[j], BK[ub[half]]], [kd])
                    if b < 7:
                        CP("act", Sall[j][:, (b + 1) * 64:(b + 2) * 64], dst[:, :], [kd], [Sall[j]])
                yield
            for e3 in e3s:
                free(e3)
            for j in range(2):
                mms = []
                for hh in range(2):
                    h = 2 * j + hh
                    for b in range(8):
                        bp = b // 2
                        o_ = banks[5 + j][hh * 64:(hh + 1) * 64, b * 64:(b + 1) * 64]
                        mms.append(mm(o_, Sall[j][:, b * 64:(b + 1) * 64],
                                      qh[:, (j * 2 + hh) * NT + b * 64:(j * 2 + hh) * NT + (b + 1) * 64], start=True, stop=False))
                        mms.append(mm(o_, vT[:, bp * 256 + h * 64:bp * 256 + (h + 1) * 64],
                                      Amz[:, (h * 8 + b) * 64:(h * 8 + b + 1) * 64], start=False, stop=True))
                MM(mms, [Sall[j], qh, vT, "Amz"], [BK[5 + j]])
            free(Sall[0]); free(Sall[1]); free(qh); free(vT)
            yield
            for j in range(2):
                sq = balloc(); rs = falloc()
                ACT(sq[:, :], banks[5 + j][:, :], AF.Square, [BK[5 + j]], [sq])
                MM([mm(banks[3][:, :], bones_bf[:, :], sq[:, :])], [sq, "bones_bf"], [BK[3]])
                ACT(rs[:, :], banks[3][:, :], AF.Sqrt, [BK[3]], [rs], scale=1.0 / 64, bias=EPS)
                RECIP(rs[:, :], rs[:, :], [rs], [rs])
                STT(rs[:, :], banks[5 + j][:, :], sp[:, SPC["og"]:SPC["og"] + 1], rs[:, :], ALU.mult, ALU.mult, [BK[5 + j], SPK, rs], [rs])
                TT("pool", Y[6 + j][:, :], rs[:, :], sgs[j][:, :], ALU.mult, [rs, sgs[j]], [Y[6 + j]])
                free(sq); free(rs); free(sgs[j])
                yield
            group_norm(3, 3)

        gens = [chainX(), chainY()]
        while gens:
            for g in list(gens):
                try:
                    next(g)
                except StopIteration:
                    gens.remove(g)
        CHK("hgrn")
        for c in range(2):
            slot = get_chunk(l, 5 + c)
            for m in range(4):
                MM([mm(banks[m][:, :], ring[slot][:, k, m * 128:(m + 1) * 128], YN[k][:, :], start=(k == 0), stop=(k == 7)) for k in range(8)],
                   [("ring", slot)] + YN, [BK[m]])
            release_chunk()
            for m in range(4):
                k = c * 4 + m
                TT("dve", xs[:, k, :], xs[:, k, :], banks[m][:, :], ALU.add, [("xs", k), BK[m]], [("xs", k)])
        for y_ in YN:
            free(y_)
        if "xmid" in tap_out and (s, ti) == taps.get("_ysel_tile", (0, 0)):
            for k in range(8):
                TAP("xmid", (l, k), xs[:, k, :], [("xs", k)])

        CHK("gn")
        rmsnorm_to_h(l, SPC["gffn"])
        hid = balloc(32)
        HIDK = hid.keys_
        for c in range(8):
            slot = get_chunk(l, 7 + c)
            for m in range(4):
                bk = (c * 4 + m) % 4
                proj_fm(slot, m, bk, split=(c == 0 and m == 0))
                r_ = falloc()
                ACT(r_[:, :], banks[bk][:, :], AF.Relu, [BK[bk]], [r_])
                idx = c * 4 + m
                TT("pool", hid[:, idx * NT:(idx + 1) * NT], r_[:, :], r_[:, :], ALU.mult, [r_], [HIDK[idx]])
                free(r_)
            release_chunk()
        for cg in range(2):
            for kg in range(4):
                slot = get_chunk(l, 15 + cg * 4 + kg)
                for m in range(4):
                    MM([mm(banks[4 + m][:, :], ring[slot][:, k, m * 128:(m + 1) * 128], hid[:, (kg * 8 + k) * NT:(kg * 8 + k + 1) * NT],
                           start=(kg == 0 and k == 0), stop=(kg == 3 and k == 7)) for k in range(8)],
                       [("ring", slot)] + HIDK[kg * 8:(kg + 1) * 8], [BK[4 + m]])
                release_chunk()
            for m in range(4):
                k = cg * 4 + m
                TT("dve", xs[:, k, :], xs[:, k, :], banks[4 + m][:, :], ALU.add, [("xs", k), BK[4 + m]], [("xs", k)])
        free(hid)
        if "xout" in tap_out and (s, ti) == taps.get("_ysel_tile", (0, 0)):
            for k in range(8):
                TAP("xout", (l, k), xs[:, k, :], [("xs", k)])
        if l == layers[-1]:
            outs = []
            for k in range(8):
                outs.append(P.dma("sp", oT[s, k * 128:(k + 1) * 128, t0:t0 + NT], xs[:, k, :], reads=[("xs", k)], writes=[("oT", s, ti, k)], chan=("out", k)))
            return outs
        return []

    all_out = []
    try:
        CHK("prologue")
        for (s, ti, l) in tile_list:
            all_out += layer_tile(s, ti, l)
    except StopBuild:
        pass
    if list_schedule:
        P.schedule()
    P.emit(final_deps=all_out + tap_dmas)
    return nc, P


_CACHE = {}


def prepare_inputs(inputs):
    wch, glu = _build_weights(inputs)
    prm = np.stack([_build_params(inputs, l) for l in range(DEPTH)], axis=0)
    cst = _build_consts()
    x = np.asarray(inputs["x"])
    in_maps = []
    for c in range(NCORES):
        xc = x[c * SEQ_PER_CORE:(c + 1) * SEQ_PER_CORE]
        xT = np.ascontiguousarray(xc.transpose(0, 2, 1))
        in_maps.append({"xT": xT, "wch": wch, "glu": glu, "prm": prm, "cst": cst})
    return in_maps


def kernel(**inputs):
    in_maps = prepare_inputs(inputs)
    if "nc" not in _CACHE:
        _CACHE["nc"] = build_program()[0]
    res = run_bass_kernel_spmd(_CACHE["nc"], in_maps, core_ids=list(range(NCORES)))
    outs = []
    for c in range(NCORES):
        oT = res.results[c]["oT"]
        outs.append(np.ascontiguousarray(oT.transpose(0, 2, 1)))
    return np.concatenate(outs, axis=0).astype(np.float32)
```

```python
import contextlib
import numpy as np
import concourse.bass as bass
import concourse.mybir as mybir
from concourse.bass_utils import run_bass_kernel_spmd

F32 = mybir.dt.float32
BF16 = mybir.dt.bfloat16
I32 = mybir.dt.int32
ALU = mybir.AluOpType
AF = mybir.ActivationFunctionType
AX = mybir.AxisListType

NCORES = 8
SEQ_PER_CORE = 4
SEQ = 2048
NT = 512
TILES_PER_SEQ = SEQ // NT
DM = 1024
DEPTH = 2
NCHUNK = 23
EPS = 1e-6
TS5 = 8
NC5 = NT // TS5
TWO_PI = 6.283185307179586

ENGS = ("pe", "act", "dve", "pool", "sp")
SEM_CAP = 1000


class Op:
    __slots__ = ("eng", "fn", "deps", "is_dma", "chan", "needs_inc", "sem", "val", "all_deps", "cost", "seq", "dma_us")

    def __init__(self, eng, fn, is_dma=False, chan=None):
        self.eng = eng
        self.fn = fn
        self.deps = []
        self.is_dma = is_dma
        self.chan = chan
        self.needs_inc = is_dma
        self.sem = None
        self.val = None
        self.all_deps = []
        self.cost = 0.3
        self.dma_us = 0.0
        self.seq = 0


def _keys(items):
    out = []
    for it in items:
        if hasattr(it, "keys_"):
            out.extend(it.keys_)
        else:
            out.append(it)
    return out


class Prog:
    def __init__(self, nc, same_engine_sync=True):
        self.nc = nc
        self.ops = {e: [] for e in ENGS}
        self.last_writer = {}
        self.readers = {}
        self.same_engine_sync = same_engine_sync
        self.nops = 0

    def schedule(self):
        allops = [o for e in ENGS for o in self.ops[e]]
        allops.sort(key=lambda o: o.seq)
        idx = {id(o): i for i, o in enumerate(allops)}
        n = len(allops)
        succ = [[] for _ in range(n)]
        ndep = [0] * n
        for i, o in enumerate(allops):
            ndep[i] = len(o.all_deps)
            for d in o.all_deps:
                succ[idx[id(d)]].append(i)

        def lat_of(o):
            return (2.0 + o.dma_us) if o.is_dma else o.cost

        def edge(o, c):
            return 0.06 if (c.eng == o.eng and not o.is_dma) else 0.25

        bl = [0.0] * n
        for i in range(n - 1, -1, -1):
            o = allops[i]
            m = 0.0
            for j in succ[i]:
                v = bl[j] + edge(o, allops[j])
                if v > m:
                    m = v
            bl[i] = lat_of(o) + m
        ready_t = [0.0] * n
        ready = {e: [] for e in ENGS}
        for i, o in enumerate(allops):
            if ndep[i] == 0:
                ready[o.eng].append(i)
        eng_free = {e: 0.0 for e in ENGS}
        new_order = {e: [] for e in ENGS}
        finish_max = 0.0
        nsched = 0
        while nsched < n:
            best_e, best_t = None, None
            for e in ENGS:
                if ready[e]:
                    t = max(eng_free[e], min(ready_t[i] for i in ready[e]))
                    if best_t is None or t < best_t:
                        best_e, best_t = e, t
            assert best_e is not None
            e, t = best_e, best_t
            cands = [i for i in ready[e] if ready_t[i] <= t + 1e-6]
            i = max(cands, key=lambda i_: (bl[i_], -i_))
            ready[e].remove(i)
            o = allops[i]
            start = max(eng_free[e], ready_t[i])
            if o.is_dma:
                eng_free[e] = start + 0.06
                fin = start + 2.0 + o.dma_us
            else:
                eng_free[e] = start + o.cost
                fin = start + o.cost
            finish_max = max(finish_max, fin)
            new_order[e].append(o)
            nsched += 1
            for j in succ[i]:
                c = allops[j]
                r = fin + edge(o, c)
                if r > ready_t[j]:
                    ready_t[j] = r
                ndep[j] -= 1
                if ndep[j] == 0:
                    ready[c.eng].append(j)
        self.ops = new_order
        self.est_us = finish_max

    def op(self, eng, fn, reads=(), writes=(), is_dma=False, chan=None, force=False, cost=0.3, dma_us=0.0):
        reads = _keys(reads)
        writes = _keys(writes)
        bank_reads = [k for k in reads if isinstance(k, tuple) and k and k[0] == "bank"]
        if bank_reads:
            reads = [k for k in reads if k not in bank_reads]
            writes = list(writes) + bank_reads
        o = Op(eng, fn, is_dma, chan)
        o.cost = cost
        o.dma_us = dma_us
        o.seq = self.nops
        self.nops += 1
        deps = {}
        for k in reads:
            w = self.last_writer.get(k)
            if w is not None:
                deps[id(w)] = w
        for k in writes:
            w = self.last_writer.get(k)
            if w is not None:
                deps[id(w)] = w
            for r in self.readers.get(k, ()):
                deps[id(r)] = r
        o.all_deps = list(deps.values())
        for d in deps.values():
            if (not d.is_dma) and d.eng == eng and ((eng == "pe" and not force) or not self.same_engine_sync):
                continue
            d.needs_inc = True
            o.deps.append(d)
        for k in reads:
            self.readers.setdefault(k, []).append(o)
        for k in writes:
            self.last_writer[k] = o
            self.readers[k] = []
        self.ops[eng].append(o)
        return o

    def dma(self, eng, out, in_, reads=(), writes=(), chan=None, **kw):
        nbytes = 1
        for d_ in out.shape:
            nbytes *= d_
        nbytes *= mybir.dt.size(out.dtype)
        return self.op(eng, lambda e: e.dma_start(out=out, in_=in_, **kw), reads, list(writes) + [("chan", chan)], is_dma=True, chan=chan,
                       dma_us=nbytes / 150e3)

    def emit(self, final_deps=()):
        nc = self.nc
        stack = contextlib.ExitStack()
        eng_sems = {e: [] for e in ENGS}
        eng_cnt = {e: 0 for e in ENGS}
        chan_sem, chan_cnt = {}, {}
        nsem = 0
        for e in ENGS:
            for o in self.ops[e]:
                if o.is_dma:
                    if o.chan not in chan_sem or chan_cnt[o.chan] + 16 > SEM_CAP:
                        chan_sem[o.chan] = stack.enter_context(nc.semaphore(f"c{nsem}"))
                        nsem += 1
                        chan_cnt[o.chan] = 0
                    chan_cnt[o.chan] += 16
                    o.sem, o.val = chan_sem[o.chan], chan_cnt[o.chan]
                elif o.needs_inc:
                    if eng_cnt[e] % SEM_CAP == 0:
                        eng_sems[e].append(stack.enter_context(nc.semaphore(f"e{nsem}")))
                        nsem += 1
                    eng_cnt[e] += 1
                    o.sem, o.val = eng_sems[e][-1], (eng_cnt[e] - 1) % SEM_CAP + 1
        self.nsem = nsem
        final_deps = list(final_deps)

        def run_engine(ename, eng):
            waited = {}

            def wait_for(d):
                key = id(d.sem)
                if waited.get(key, 0) >= d.val:
                    return
                eng.wait_ge(d.sem, d.val)
                waited[key] = d.val

            for o in self.ops[ename]:
                for d in o.deps:
                    wait_for(d)
                ins = o.fn(eng)
                if o.needs_inc:
                    ins.then_inc(o.sem, 16 if o.is_dma else 1)
            if ename == "sp":
                for d in final_deps:
                    wait_for(d)

        with nc.Block() as block:
            @block.tensor
            def _(e):
                run_engine("pe", e)

            @block.scalar
            def _(e):
                run_engine("act", e)

            @block.vector
            def _(e):
                run_engine("dve", e)

            @block.gpsimd
            def _(e):
                run_engine("pool", e)

            @block.sync
            def _(e):
                run_engine("sp", e)
        stack.close()


def _col8(v):
    return np.ascontiguousarray(v.reshape(8, 128).T)


def _col2(v):
    return np.ascontiguousarray(v.reshape(2, 128).T)


def _qperm():
    idx = np.zeros(256, np.int64)
    for r in range(2):
        for kh in range(2):
            for d in range(64):
                idx[r * 128 + kh * 64 + d] = (kh * 2 + r) * 64 + d
    return idx


_PRM_FIELDS = [
    ("gmix", 8), ("gffn", 8), ("ggn", 8), ("convw", 6), ("s5d", 2), ("qg", 1), ("kg", 1), ("sink", 2),
    ("hlb0", 2), ("hlb1", 2), ("og", 1),
    ("A_lre", 128), ("A_lim", 128), ("A_ldt", 2), ("A_bre", 128), ("A_bim", 128),
    ("A_cre", 2048), ("A_cim", 2048),
    ("B_lre", 8), ("B_lim", 8), ("B_ldt", 8), ("B_cre", 128), ("B_cim", 128),
]
_PRM_OFF = {}
_o = 0
for _n, _w in _PRM_FIELDS:
    _PRM_OFF[_n] = (_o, _w)
    _o += _w
NPRM = _o

_CST_FIELDS = [
    ("ident", 128), ("bones", 128),
    ("mgp0", 1), ("mgp1", 1), ("mB0", 1), ("mB1", 1),
    ("mg8", 8), ("tauA", 8), ("tauB", 8), ("cidx", 64),
    ("segm", 512), ("cmask", 64),
]
_CST_OFF = {}
_o = 0
for _n, _w in _CST_FIELDS:
    _CST_OFF[_n] = (_o, _w)
    _o += _w
NCST = _o


def _build_consts():
    c = np.zeros((128, NCST), np.float32)

    def put(name, arr):
        o, w = _CST_OFF[name]
        c[:, o:o + w] = np.asarray(arr, np.float32).reshape(128, w)

    p = np.arange(128)
    put("ident", np.eye(128))
    bo = np.zeros((128, 128))
    bo[:64, :64] = 1
    bo[64:, 64:] = 1
    put("bones", bo)
    put("mgp0", ((p // 16) % 2 == 0)[:, None])
    put("mgp1", ((p // 16) % 2 == 1)[:, None])
    put("mB0", (p < 64)[:, None])
    put("mB1", (p >= 64)[:, None])
    put("mg8", (p[:, None] // 16) == np.arange(8)[None, :])
    put("tauA", np.tile(np.arange(8.0)[None], (128, 1)))
    put("tauB", np.tile(np.arange(1.0, 9.0)[None], (128, 1)))
    put("cidx", np.tile(np.arange(1.0, 65.0)[None], (128, 1)))
    seg = np.ones((8, 64))
    seg[:, 0] = 0
    put("segm", np.tile(seg.reshape(1, 512), (128, 1)))
    s = (p % 64)[:, None]
    t = np.arange(64)[None, :]
    put("cmask", (s <= t))
    return c


def _build_params(inp, l):
    prm = np.zeros((128, NPRM), np.float32)

    def put(name, arr):
        o, w = _PRM_OFF[name]
        prm[:, o:o + w] = np.asarray(arr, np.float32).reshape(128, w)

    qp = _qperm()
    put("gmix", _col8(inp["norm_mix"][l]))
    put("gffn", _col8(inp["norm_ffn"][l]))
    gn = np.array(inp["group_norm"][l])
    gn[512:768] = np.array(inp["group_norm"][l])[512:768][qp]
    put("ggn", _col8(gn))
    cw = np.asarray(inp["conv_w"][l])
    put("convw", np.stack([_col2(cw[i]) for i in range(3)], axis=2).reshape(128, 6))
    put("s5d", _col2(np.asarray(inp["s5_d"][l])))
    put("qg", np.tile(np.asarray(inp["attn_q_norm"][l]), 2)[:, None])
    put("kg", np.tile(np.asarray(inp["attn_k_norm"][l]), 2)[:, None])
    sk = np.asarray(inp["attn_sinks"][l])
    put("sink", np.stack([np.repeat(sk[[0 * 2 + r, 1 * 2 + r]], 64) for r in range(2)], axis=1))
    put("hlb0", _col2(np.asarray(inp["hg_lower_bounds"][0])))
    put("hlb1", _col2(np.asarray(inp["hg_lower_bounds"][1])))
    put("og", np.tile(np.asarray(inp["hg_out_norm"][l]), 2)[:, None])
    lre, lim, ldt = (np.asarray(inp[k][l]) for k in ("s5_lam_re", "s5_lam_im", "s5_log_dt"))
    bre, bim = np.asarray(inp["s5_b_re"][l]), np.asarray(inp["s5_b_im"][l])
    cre, cim = np.asarray(inp["s5_c_re"][l]), np.asarray(inp["s5_c_im"][l])
    put("A_lre", np.repeat(lre.reshape(2, 8, 1, 64), 16, axis=2).transpose(1, 2, 0, 3).reshape(128, 128))
    put("A_lim", np.repeat(lim.reshape(2, 8, 1, 64), 16, axis=2).transpose(1, 2, 0, 3).reshape(128, 128))
    put("A_ldt", np.repeat(ldt.reshape(2, 8, 1), 16, axis=2).transpose(1, 2, 0).reshape(128, 2))
    put("A_bre", bre.reshape(2, 8, 64, 16).transpose(1, 3, 0, 2).reshape(128, 128))
    put("A_bim", bim.reshape(2, 8, 64, 16).transpose(1, 3, 0, 2).reshape(128, 128))
    put("A_cre", np.repeat(cre.reshape(2, 8, 1, 16, 64), 16, axis=2).transpose(1, 2, 0, 3, 4).reshape(128, 2048))
    put("A_cim", np.repeat(cim.reshape(2, 8, 1, 16, 64), 16, axis=2).transpose(1, 2, 0, 3, 4).reshape(128, 2048))
    put("B_lre", lre.reshape(8, 2, 64).transpose(1, 2, 0).reshape(128, 8))
    put("B_lim", lim.reshape(8, 2, 64).transpose(1, 2, 0).reshape(128, 8))
    put("B_ldt", np.repeat(ldt.reshape(8, 2, 1), 64, axis=2).transpose(1, 2, 0).reshape(128, 8))
    put("B_cre", cre.reshape(8, 2, 16, 64).transpose(1, 3, 0, 2).reshape(128, 128))
    put("B_cim", cim.reshape(8, 2, 16, 64).transpose(1, 3, 0, 2).reshape(128, 128))
    return prm


def _chunks_kn(W):
    K, N = W.shape
    out = []
    for cg in range(N // 512):
        for kg in range(K // 1024):
            blk = W[kg * 1024:(kg + 1) * 1024, cg * 512:(cg + 1) * 512]
            out.append(np.ascontiguousarray(blk.reshape(8, 128, 512).transpose(1, 0, 2)).reshape(128, 4096))
    return out


def _build_weights(inp):
    qp = _qperm()
    chunks = []
    for l in range(DEPTH):
        w_in = np.asarray(inp["w_in"][l])
        win = np.array(w_in)
        win[:, 1024:1280] = w_in[:, 1024:1280][:, qp]
        w_out = np.asarray(inp["w_out"][l])
        wout = np.array(w_out)
        wout[512:768, :] = w_out[512:768, :][qp, :]
        chunks += _chunks_kn(win)
        chunks += _chunks_kn(wout)
        chunks += _chunks_kn(np.asarray(inp["w_ff1"][l]))
        chunks += _chunks_kn(np.asarray(inp["w_ff2"][l]))
    wch = np.stack(chunks, axis=0).astype(np.float32)
    glu = np.stack([np.asarray(inp["s5_w_glu"][l]).reshape(2, 128, 512).transpose(1, 0, 2) for l in range(DEPTH)], axis=0)
    return wch, np.ascontiguousarray(glu, np.float32)


class Buf:
    def __init__(self, ap2d, keys):
        self.a = ap2d
        self.keys_ = keys

    def __getitem__(self, k):
        return self.a[k]

    def v3(self, b):
        return self.a.rearrange("p (a b) -> p a b", b=b)


def bc(ap, shape):
    return ap.broadcast_to(list(shape))


def build_program(n_seq=SEQ_PER_CORE, n_tiles=TILES_PER_SEQ, layers=(0, 1), taps=None, same_engine_sync=True,
                  stop_after=None, skip_prologue=False, list_schedule=True):
    taps = taps or {}

    class StopBuild(Exception):
        pass

    def CHK(name):
        if stop_after == name:
            raise StopBuild()
    nc = bass.Bass("TRN2", target_bir_lowering=False)
    P = Prog(nc, same_engine_sync=same_engine_sync)
    NL = DEPTH

    xT = nc.dram_tensor("xT", [SEQ_PER_CORE, DM, SEQ], F32, kind="ExternalInput").ap()
    wch = nc.dram_tensor("wch", [NL * NCHUNK, 128, 4096], F32, kind="ExternalInput").ap()
    glu_d = nc.dram_tensor("glu", [NL, 128, 2, 512], F32, kind="ExternalInput").ap()
    prm_d = nc.dram_tensor("prm", [NL, 128, NPRM], F32, kind="ExternalInput").ap()
    cst_d = nc.dram_tensor("cst", [128, NCST], F32, kind="ExternalInput").ap()
    oT = nc.dram_tensor("oT", [SEQ_PER_CORE, DM, SEQ], F32, kind="ExternalOutput").ap()
    wsc = nc.dram_tensor("wsc", [NL * NCHUNK, 128, 4096], BF16).ap()
    tap_out = {}
    for name, shape in taps.items():
        if name.startswith("_"):
            continue
        tap_out[name] = nc.dram_tensor("tap_" + name, list(shape), F32, kind="ExternalOutput").ap()
    s5m_d = nc.dram_tensor("s5m_d", [NL, 128, 10240], BF16).ap()
    tap_dmas = []

    def sb(name, shape, dt=F32):
        return nc.alloc_sbuf_tensor("s_" + name, list(shape), dt)

    NRING = 4
    ring = [sb(f"ring{i}", [128, 8, 512], BF16) for i in range(NRING)]
    xs = sb("xs", [128, 8, NT], F32)
    hbuf = sb("hbuf", [128, 8, NT], BF16)
    cst = sb("cst", [128, NCST], F32)
    NF, NB = 23, 32
    farena = sb("farena", [128, NF * NT], F32)
    barena = sb("barena", [128, NB * NT], BF16)
    fmap = [False] * NF
    bmap = [False] * NB

    stamp = {"fp": [0] * NF, "bp": [0] * NB}
    clock = [0]

    def _alloc(arena, amap, tag, n):
        best, best_key = None, None
        for i in range(len(amap) - n + 1):
            if not any(amap[i:i + n]):
                key = max(stamp[tag][i:i + n])
                if best is None or key < best_key:
                    best, best_key = i, key
        if best is None:
            raise RuntimeError(f"arena {tag} exhausted")
        i = best
        for j in range(i, i + n):
            amap[j] = True
        b = Buf(arena[:, i * NT:(i + n) * NT], [(tag, j) for j in range(i, i + n)])
        b.rng = (i, n)
        return b

    def falloc(n=1):
        return _alloc(farena, fmap, "fp", n)

    def balloc(n=1):
        return _alloc(barena, bmap, "bp", n)

    def free(b):
        i, n = b.rng
        amap = fmap if b.keys_[0][0] == "fp" else bmap
        clock[0] += 1
        for j in range(i, i + n):
            assert amap[j]
            amap[j] = False
            stamp[b.keys_[0][0]][j] = clock[0]

    banks = [nc.alloc_psum_tensor(f"bank{i}", [128, 512], F32) for i in range(8)]
    BK = [("bank", i) for i in range(8)]

    def cs(name):
        o, w = _CST_OFF[name]
        return cst[:, o:o + w]

    ident_bf = sb("ident_bf", [128, 128], BF16)
    bones_bf = sb("bones_bf", [128, 128], BF16)
    ones_bf = sb("ones_bf", [128, 128], BF16)
    sb_int = sb("sb_int", [128, 512], I32)
    Amz = sb("Amz", [128, 2048], BF16)

    s5m = sb("s5m", [128, 10240], BF16)
    L1v = s5m[:, 0:4096].rearrange("p (j r t c) -> p j r t c", j=2, r=2, t=8)
    L3v = s5m[:, 4096:8192].rearrange("p (r q t c) -> p r q t c", r=2, q=8, t=8)
    KLv = s5m[:, 8192:10240].rearrange("p (j t c) -> p j t c", j=2, t=8)
    L = []
    for l in range(NL):
        d = dict(
            sp=sb(f"sp{l}", [128, 48], F32),
            glu=sb(f"glu{l}", [128, 2, 512], BF16),
            L1=L1v,
            L3=L3v,
            KL=KLv,
            cosR=sb(f"cosR{l}", [128, 512], F32),
            sinR=sb(f"sinR{l}", [128, 512], F32),
            rtab=sb(f"rtab{l}", [128, 512], F32),
            r8=sb(f"r8_{l}", [128, 8], F32),
            zbuf=sb(f"zbuf{l}", [128, 2, NT + 2], F32),
            Er=sb(f"Er{l}", [128, 8, NC5 + 1], F32),
            Ei=sb(f"Ei{l}", [128, 8, NC5 + 1], F32),
            kbuf=sb(f"kbuf{l}", [128, 128 + NT], BF16),
            vbuf=sb(f"vbuf{l}", [128, 5, 128], BF16),
            S=[sb(f"S{l}_{j}", [128, 64], F32) for j in range(2)],
            S2=[sb(f"S2{l}_{j}", [128, 64], F32) for j in range(2)],
        )
        L.append(d)
    SPC = dict(gmix=0, gffn=8, ggn=16, convw=24, s5d=30, qgs=32, kg=33, esink=34, lb=36, oml=38, og=40)

    def _nfree(ap):
        n = 1
        for d_ in ap.shape[1:]:
            n *= d_
        return n

    def _cost(eng, ap):
        n = _nfree(ap)
        if eng == "act":
            return 0.15 + n * 0.00083
        if eng == "pool":
            return 0.2 + n * 0.0021
        return 0.12 + n * 0.00104

    def ACT(out, in_, func, reads, writes, scale=1.0, bias=None):
        if bias is None:
            return P.op("act", lambda e: e.activation(out=out, in_=in_, func=func, scale=scale), reads, writes, cost=_cost("act", out))
        return P.op("act", lambda e: e.activation(out=out, in_=in_, func=func, scale=scale, bias=bias), reads, writes, cost=_cost("act", out))

    def TT(eng, out, a, b, op, reads, writes):
        return P.op(eng, lambda e: e.tensor_tensor(out=out, in0=a, in1=b, op=op), reads, writes, cost=_cost(eng, out))

    def TS(eng, out, a, s1, op0, reads, writes, s2=None, op1=None):
        if op1 is None:
            return P.op(eng, lambda e: e.tensor_scalar(out=out, in0=a, scalar1=s1, scalar2=None, op0=op0), reads, writes, cost=_cost(eng, out))
        return P.op(eng, lambda e: e.tensor_scalar(out=out, in0=a, scalar1=s1, scalar2=s2, op0=op0, op1=op1), reads, writes, cost=_cost(eng, out))

    def STT(out, in0, scalar, in1, op0, op1, reads, writes):
        return P.op("dve", lambda e: e.scalar_tensor_tensor(out=out, in0=in0, scalar=scalar, in1=in1, op0=op0, op1=op1), reads, writes,
                    cost=_cost("dve", out))

    def CP(eng, out, in_, reads, writes):
        if eng == "act":
            return P.op("act", lambda e: e.activation(out=out, in_=in_, func=AF.Copy), reads, writes, cost=_cost("act", out))
        return P.op(eng, lambda e: e.tensor_copy(out=out, in_=in_), reads, writes, cost=_cost(eng, out))

    def RECIP(out, in_, reads, writes):
        return P.op("dve", lambda e: e.reciprocal(out=out, in_=in_), reads, writes, cost=_cost("dve", out))

    def MM(mms, reads, writes, force=False):
        def fn(e):
            ins = None
            for m in mms:
                if m.get("tp") is not None:
                    ins = e.matmul(m["out"], lhsT=m["lhsT"], rhs=m["rhs"], start=m["start"], stop=m["stop"], skip_group_check=True,
                                   tile_position=m["tp"])
                else:
                    ins = e.matmul(m["out"], lhsT=m["lhsT"], rhs=m["rhs"], start=m["start"], stop=m["stop"], skip_group_check=True)
            return ins
        c_ = 0.0
        for m in mms:
            c_ += max(_nfree(m["rhs"]), 64) / 2400.0 + 0.045
        return P.op("pe", fn, reads, writes, force=force, cost=c_)

    def mm(out, lhsT, rhs, start=True, stop=True, tp=None):
        return dict(out=out, lhsT=lhsT, rhs=rhs, start=start, stop=stop, tp=tp)

    def TAP(name, idx, src_ap, reads):
        if name in tap_out:
            o = P.dma("sp", tap_out[name][idx], src_ap, reads=reads, writes=[("tap", name, idx)], chan=("tap", name))
            tap_dmas.append(o)

    P.dma("sp", cst[:], cst_d, writes=["cst"], chan="cst")
    CP("dve", ident_bf[:], cs("ident"), ["cst"], ["ident_bf"])
    CP("dve", bones_bf[:], cs("bones"), ["cst"], ["bones_bf"])
    P.op("pool", lambda e: e.memset(ones_bf[:], 1.0), writes=["ones_bf"])
    P.op("pool", lambda e: e.memset(Amz[:, :], 0.0), writes=["Amz"])

    for l in layers:
        for c in range(NCHUNK):
            ci = l * NCHUNK + c
            P.dma("pool", wsc[ci].rearrange("p (a b) -> p a b", b=2048), wch[ci].rearrange("p (a b) -> p a b", b=2048),
                  writes=[("wsc", ci)], chan=("wcast", ci % 4))

    prm_sb = falloc(2)
    _ac_lo = _PRM_OFF["A_cre"][0]
    _ac_hi = _PRM_OFF["A_cim"][0] + 2048
    NSMALL = NPRM - 4096
    assert NSMALL <= 2 * NT

    def prologue_layer(l):
        d = L[l]
        sp = d["sp"]
        SPK = ("sp", l)
        P.dma("sp", prm_sb[:, 0:_ac_lo], prm_d[l, :, 0:_ac_lo], writes=[prm_sb], chan="prm")
        P.dma("sp", prm_sb[:, _ac_lo:NSMALL], prm_d[l, :, _ac_hi:NPRM], writes=[prm_sb], chan="prm2")

        def pf(name, a=0, b=None):
            o, w = _PRM_OFF[name]
            if o >= _ac_hi:
                o -= 4096
            b = w if b is None else b
            return prm_sb[:, o + a:o + b]

        RD = [prm_sb, "cst"]
        for nm in ("gmix", "gffn", "ggn", "convw", "s5d", "kg", "og"):
            w = _PRM_OFF[nm][1]
            CP("dve", sp[:, SPC[nm]:SPC[nm] + w], pf(nm), RD, [SPK])
        TS("dve", sp[:, SPC["qgs"]:SPC["qgs"] + 1], pf("qg"), 0.125, ALU.mult, RD, [SPK])
        ACT(sp[:, SPC["esink"]:SPC["esink"] + 2], pf("sink"), AF.Exp, RD, [SPK])
        if l == 0:
            P.op("pool", lambda e: e.memset(sp[:, SPC["lb"]:SPC["lb"] + 2], 0.0), writes=[SPK])
        else:
            tmp = falloc()
            TT("dve", tmp[:, 0:2], pf("hlb1"), pf("hlb0"), ALU.subtract, RD, [tmp])
            ACT(sp[:, SPC["lb"]:SPC["lb"] + 2], tmp[:, 0:2], AF.Sigmoid, [tmp], [SPK])
            free(tmp)
        TS("dve", sp[:, SPC["oml"]:SPC["oml"] + 2], sp[:, SPC["lb"]:SPC["lb"] + 2], -1.0, ALU.mult, [SPK], [SPK], s2=1.0, op1=ALU.add)
        gl32 = falloc(2)
        P.dma("sp", gl32[:, 0:1024].rearrange("p (k n) -> p k n", n=512), glu_d[l], writes=[gl32], chan="glu")
        CP("dve", d["glu"][:, 0, :], gl32[:, 0:512], [gl32], [("glu", l)])
        CP("dve", d["glu"][:, 1, :], gl32[:, 512:1024], [gl32], [("glu", l)])
        free(gl32)

        def trig(u, n, cos_out, sin_out, ub, cb, sbk, shape3=None):
            t1 = falloc(); t2 = falloc()
            for (shift, outv, ob) in ((0.0, sin_out, sbk), (0.25, cos_out, cb)):
                TS("dve", t1[:, 0:n], u, shift + 64.0, ALU.add, [ub], [t1])
                CP("dve", sb_int[:, 0:n], t1[:, 0:n], [t1], ["sb_int"])
                CP("dve", t2[:, 0:n], sb_int[:, 0:n], ["sb_int"], [t2])
                TT("dve", t1[:, 0:n], t1[:, 0:n], t2[:, 0:n], ALU.subtract, [t1, t2], [t1])
                TS("dve", t2[:, 0:n], t1[:, 0:n], 0.5, ALU.is_gt, [t1], [t2])
                TT("dve", t1[:, 0:n], t1[:, 0:n], t2[:, 0:n], ALU.subtract, [t1, t2], [t1])
                TS("dve", t2[:, 0:n], t1[:, 0:n], -0.5, ALU.is_lt, [t1], [t2])
                TT("dve", t1[:, 0:n], t1[:, 0:n], t2[:, 0:n], ALU.add, [t1, t2], [t1])
                ACT(outv, t1[:, 0:n], AF.Sin, [t1], [ob], scale=TWO_PI)
            free(t1); free(t2)
            return None

        a_lr = falloc(); a_dt = falloc(); a_e1 = falloc(); a_th = falloc()
        TS("dve", a_lr[:, 0:128], pf("A_lre"), -1e-4, ALU.min, RD, [a_lr])
        ACT(a_dt[:, 0:2], pf("A_ldt"), AF.Exp, RD, [a_dt])
        for j in range(2):
            sl = slice(j * 64, (j + 1) * 64)
            TS("dve", a_e1[:, sl], a_lr[:, sl], a_dt[:, j:j + 1], ALU.mult, [a_lr, a_dt], [a_e1])
            TS("dve", a_th[:, sl], pf("A_lim")[:, sl], a_dt[:, j:j + 1], ALU.mult, RD + [a_dt], [a_th], s2=1.0 / TWO_PI, op1=ALU.mult)
        for j in range(2):
            sl = slice(j * 64, (j + 1) * 64)
            u = falloc(); me = falloc(); cc = falloc(); ss = falloc(); mr = falloc(); mi = falloc()
            u3, me3 = u.v3(64), me.v3(64)
            tau_b = bc(cs("tauA").unsqueeze(2), [128, 8, 64])
            TT("dve", u3, bc(a_th[:, sl].unsqueeze(1), [128, 8, 64]), tau_b, ALU.mult, [a_th, "cst"], [u])
            TT("dve", me3, bc(a_e1[:, sl].unsqueeze(1), [128, 8, 64]), tau_b, ALU.mult, [a_e1, "cst"], [me])
            ACT(me[:, :], me[:, :], AF.Exp, [me], [me])
            trig(u[:, :], 512, cc[:, :], ss[:, :], u, cc, ss)
            TT("dve", mr[:, :], me[:, :], cc[:, :], ALU.mult, [me, cc], [mr])
            TT("dve", mi[:, :], me[:, :], ss[:, :], ALU.mult, [me, ss], [mi])
            free(u); free(me); free(cc); free(ss)
            w = falloc()
            W = lambda i: w[:, i * 64:(i + 1) * 64]
            lr_, li_ = a_lr[:, sl], pf("A_lim")[:, sl]
            TT("dve", W(0), lr_, lr_, ALU.mult, [a_lr], [w])
            TT("dve", W(2), li_, li_, ALU.mult, RD, [w])
            TT("dve", W(0), W(0), W(2), ALU.add, [w], [w])
            RECIP(W(0), W(0), [w], [w])
            TS("dve", W(1), mr[:, 64:128], -1.0, ALU.add, [mr], [w])
            TT("dve", W(2), W(1), lr_, ALU.mult, [w, a_lr], [w])
            TT("dve", W(7), mi[:, 64:128], li_, ALU.mult, [mi] + RD, [w])
            TT("dve", W(2), W(2), W(7), ALU.add, [w], [w])
            TT("dve", W(3), W(2), W(0), ALU.mult, [w], [w])
            TT("dve", W(2), mi[:, 64:128], lr_, ALU.mult, [mi, a_lr], [w])
            TT("dve", W(7), W(1), li_, ALU.mult, [w] + RD, [w])
            TT("dve", W(2), W(2), W(7), ALU.subtract, [w], [w])
            TT("dve", W(4), W(2), W(0), ALU.mult, [w], [w])
            bre_, bim_ = pf("A_bre")[:, sl], pf("A_bim")[:, sl]
            TT("dve", W(5), W(3), bre_, ALU.mult, [w] + RD, [w])
            TT("dve", W(7), W(4), bim_, ALU.mult, [w] + RD, [w])
            TT("dve", W(5), W(5), W(7), ALU.subtract, [w], [w])
            TT("dve", W(6), W(3), bim_, ALU.mult, [w] + RD, [w])
            TT("dve", W(7), W(4), bre_, ALU.mult, [w] + RD, [w])
            TT("dve", W(6), W(6), W(7), ALU.add, [w], [w])
            p1r = falloc(); p1i = falloc(); t = falloc()
            bbr_b = bc(W(5).unsqueeze(1), [128, 8, 64]); bbi_b = bc(W(6).unsqueeze(1), [128, 8, 64])
            TT("dve", p1r.v3(64), mr.v3(64), bbr_b, ALU.mult, [mr, w], [p1r])
            TT("dve", t.v3(64), mi.v3(64), bbi_b, ALU.mult, [mi, w], [t])
            TT("dve", p1r[:, :], p1r[:, :], t[:, :], ALU.subtract, [p1r, t], [p1r])
            TT("dve", p1i.v3(64), mr.v3(64), bbi_b, ALU.mult, [mr, w], [p1i])
            TT("dve", t.v3(64), mi.v3(64), bbr_b, ALU.mult, [mi, w], [t])
            TT("dve", p1i[:, :], p1i[:, :], t[:, :], ALU.add, [p1i, t], [p1i])
            free(w); free(mr); free(mi)
            for ri, src in ((0, p1r), (1, p1i)):
                for gp in range(2):
                    TS("dve", d["L1"][:, j, ri, :, gp * 64:(gp + 1) * 64], src.v3(64), cs("mgp%d" % gp), ALU.mult, [src, "cst"], ["s5m"])
            kc = falloc()
            t2 = falloc()
            acr = falloc(2); aci = falloc(2)
            ocr, oci = _PRM_OFF["A_cre"][0], _PRM_OFF["A_cim"][0]
            P.dma("sp", acr[:, 0:1024], prm_d[l, :, ocr + j * 1024:ocr + (j + 1) * 1024], writes=[acr], chan="acr")
            P.dma("sp", aci[:, 0:1024], prm_d[l, :, oci + j * 1024:oci + (j + 1) * 1024], writes=[aci], chan="aci")
            for ho in range(16):
                cr_b = bc(acr[:, ho * 64:(ho + 1) * 64].unsqueeze(1), [128, 8, 64])
                ci_b = bc(aci[:, ho * 64:(ho + 1) * 64].unsqueeze(1), [128, 8, 64])
                TT("dve", t.v3(64), p1r.v3(64), cr_b, ALU.mult, [p1r, acr], [t])
                TT("dve", t2.v3(64), p1i.v3(64), ci_b, ALU.mult, [p1i, aci], [t2])
                TT("dve", t[:, :], t[:, :], t2[:, :], ALU.subtract, [t, t2], [t])
                P.op("dve", lambda e, ho=ho, kc=kc, t=t: e.tensor_reduce(out=kc[:, 0:128].rearrange("p (a b) -> p a b", b=16)[:, :, ho], in_=t.v3(64),
                                                                         axis=AX.X, op=ALU.add), [t], [kc])
            free(t2); free(p1r); free(p1i); free(acr); free(aci)
            klf = falloc(2)
            for g in range(8):
                o_, _w = _CST_OFF["mg8"]
                TS("dve", klf[:, 0:1024].rearrange("p (a b) -> p a b", b=128)[:, :, g * 16:(g + 1) * 16],
                   kc[:, 0:128].rearrange("p (a b) -> p a b", b=16), cst[:, o_ + g:o_ + g + 1], ALU.mult, [kc, "cst"], [klf])
            STT(klf[:, 0:128], cs("ident"), sp[:, SPC["s5d"] + j:SPC["s5d"] + j + 1], klf[:, 0:128], ALU.mult, ALU.add, ["cst", SPK, klf], [klf])
            CP("dve", d["KL"][:, j, :, :], klf[:, 0:1024].rearrange("p (a b) -> p a b", b=128), [klf], ["s5m"])
            free(kc); free(klf); free(t)
        free(a_lr); free(a_dt); free(a_e1); free(a_th)

        b_ = falloc()
        Bc = lambda i: b_[:, i * 8:(i + 1) * 8]
        TS("dve", Bc(0), pf("B_lre"), -1e-4, ALU.min, RD, [b_])
        ACT(Bc(1), pf("B_ldt"), AF.Exp, RD, [b_])
        TT("dve", Bc(2), Bc(0), Bc(1), ALU.mult, [b_], [b_])
        TT("dve", Bc(3), pf("B_lim"), Bc(1), ALU.mult, RD + [b_], [b_])
        TS("dve", Bc(3), Bc(3), 1.0 / TWO_PI, ALU.mult, [b_], [b_])
        u = falloc(); me = falloc(); cc = falloc(); ss = falloc()
        tauB_b = bc(cs("tauB").unsqueeze(1), [128, 8, 8])
        TT("dve", u[:, 0:64].rearrange("p (a b) -> p a b", b=8), bc(Bc(3).unsqueeze(2), [128, 8, 8]), tauB_b, ALU.mult, [b_, "cst"], [u])
        TT("dve", me[:, 0:64].rearrange("p (a b) -> p a b", b=8), bc(Bc(2).unsqueeze(2), [128, 8, 8]), tauB_b, ALU.mult, [b_, "cst"], [me])
        ACT(me[:, 0:64], me[:, 0:64], AF.Exp, [me], [me])
        trig(u[:, 0:64], 64, cc[:, 0:64], ss[:, 0:64], u, cc, ss)
        TT("dve", cc[:, 0:64], cc[:, 0:64], me[:, 0:64], ALU.mult, [cc, me], [cc])
        TT("dve", ss[:, 0:64], ss[:, 0:64], me[:, 0:64], ALU.mult, [ss, me], [ss])
        for qh in range(2):
            cr_b = bc(pf("B_cre")[:, qh * 64:(qh + 1) * 64].rearrange("p (q h) -> p q h", h=16).unsqueeze(2), [128, 4, 8, 16])
            ci_b = bc(pf("B_cim")[:, qh * 64:(qh + 1) * 64].rearrange("p (q h) -> p q h", h=16).unsqueeze(2), [128, 4, 8, 16])
            mr_b = bc(cc[:, qh * 32:(qh + 1) * 32].rearrange("p (q t) -> p q t", t=8).unsqueeze(3), [128, 4, 8, 16])
            mi_b = bc(ss[:, qh * 32:(qh + 1) * 32].rearrange("p (q t) -> p q t", t=8).unsqueeze(3), [128, 4, 8, 16])
            c1 = falloc(); c2 = falloc()
            v4 = lambda b: b[:, :].rearrange("p (q t h) -> p q t h", t=8, h=16)
            TT("dve", v4(c1), cr_b, mr_b, ALU.mult, RD + [cc], [c1])
            TT("dve", v4(c2), ci_b, mi_b, ALU.mult, RD + [ss], [c2])
            TT("dve", c1[:, :], c1[:, :], c2[:, :], ALU.subtract, [c1, c2], [c1])
            for gp in range(2):
                TS("dve", d["L3"][:, 0, qh * 4:(qh + 1) * 4, :, gp * 16:(gp + 1) * 16], v4(c1), cs("mB%d" % gp), ALU.mult, [c1, "cst"], ["s5m"])
            TT("dve", v4(c1), cr_b, mi_b, ALU.mult, RD + [ss], [c1])
            TT("dve", v4(c2), ci_b, mr_b, ALU.mult, RD + [cc], [c2])
            TT("dve", c1[:, :], c1[:, :], c2[:, :], ALU.add, [c1, c2], [c1])
            for gp in range(2):
                TS("dve", d["L3"][:, 1, qh * 4:(qh + 1) * 4, :, gp * 16:(gp + 1) * 16], v4(c1), cs("mB%d" % gp), ALU.mult, [c1, "cst"], ["s5m"],
                   s2=-1.0, op1=ALU.mult)
            free(c1); free(c2)
        ACT(d["r8"][:, :], Bc(2), AF.Exp, [b_], [("r8", l)], scale=8.0)
        TT("dve", d["rtab"][:, :].rearrange("p (q c) -> p q c", c=64), bc(d["r8"][:, :].unsqueeze(2), [128, 8, 64]),
           cs("segm").rearrange("p (q c) -> p q c", c=64), ALU.mult, [("r8", l), "cst"], [("rtab", l)])
        TS("dve", Bc(4), Bc(3), 8.0, ALU.mult, [b_], [b_], s2=64.0, op1=ALU.add)
        CP("dve", sb_int[:, 0:8], Bc(4), [b_], ["sb_int"])
        CP("dve", Bc(5), sb_int[:, 0:8], ["sb_int"], [b_])
        TT("dve", Bc(4), Bc(4), Bc(5), ALU.subtract, [b_], [b_])
        TS("dve", Bc(5), Bc(4), 0.5, ALU.is_gt, [b_], [b_])
        TT("dve", Bc(4), Bc(4), Bc(5), ALU.subtract, [b_], [b_])
        TS("dve", Bc(5), Bc(4), -0.5, ALU.is_lt, [b_], [b_])
        TT("dve", Bc(4), Bc(4), Bc(5), ALU.add, [b_], [b_])
        TT("dve", u.v3(64), bc(Bc(4).unsqueeze(2), [128, 8, 64]), bc(cs("cidx").unsqueeze(1), [128, 8, 64]), ALU.mult, [b_, "cst"], [u])
        trig(u[:, :], 512, d["cosR"][:, :], d["sinR"][:, :], u, ("cosR", l), ("sinR", l))
        free(u); free(me); free(cc); free(ss); free(b_)

    def prologue_taps(l):
        d = L[l]
        if "KL" in tap_out:
            tf = falloc(4)
            CP("dve", tf[:, 0:2048], s5m[:, 8192:10240], ["s5m"], [tf])
            TAP("KL", l, tf[:, 0:2048], [tf]); free(tf)
        if "L1" in tap_out:
            tf = falloc(8)
            CP("dve", tf[:, 0:4096], s5m[:, 0:4096], ["s5m"], [tf])
            TAP("L1", l, tf[:, 0:4096], [tf]); free(tf)
        if "L3" in tap_out:
            tf = falloc(8)
            CP("dve", tf[:, 0:4096], s5m[:, 4096:8192], ["s5m"], [tf])
            TAP("L3", l, tf[:, 0:4096], [tf]); free(tf)
        if "rot" in tap_out:
            TAP("rot", (l, 0), d["cosR"][:, :], [("cosR", l)])
            TAP("rot", (l, 1), d["sinR"][:, :], [("sinR", l)])
            TAP("rot", (l, 2), d["rtab"][:, :], [("rtab", l)])

    for l in layers:
        prologue_layer(l)
        prologue_taps(l)
        P.dma("sp", s5m_d[l], s5m[:, :], reads=["s5m"], writes=[("s5m_d", l)], chan="s5m_st")
    free(prm_sb)

    tile_list = [(s, ti, l) for s in range(n_seq) for ti in range(n_tiles) for l in layers]
    chunk_seq = [(l, c) for (s, ti, l) in tile_list for c in range(NCHUNK)]
    ring_state = dict(issued=0, used=0)

    def issue_next():
        n = ring_state["issued"]
        if n >= len(chunk_seq):
            return
        l, c = chunk_seq[n]
        slot = n % NRING
        ci = l * NCHUNK + c
        P.dma("sp", ring[slot][:, :, :].rearrange("p k n -> p (k n)"), wsc[ci], reads=[("wsc", ci)], writes=[("ring", slot)], chan=("ring", slot))
        ring_state["issued"] += 1

    def get_chunk(l, c):
        n = ring_state["used"]
        assert chunk_seq[n] == (l, c), (chunk_seq[n], l, c)
        while ring_state["issued"] <= n:
            issue_next()
        return n % NRING

    def release_chunk():
        ring_state["used"] += 1
        while ring_state["issued"] < min(len(chunk_seq), ring_state["used"] + NRING):
            issue_next()

    for _ in range(NRING):
        issue_next()

    def rmsnorm_to_h(l, gcol):
        sp = L[l]["sp"]
        for k in range(8):
            sq = balloc()
            ACT(sq[:, :], xs[:, k, :], AF.Square, [("xs", k)], [sq])
            MM([mm(banks[7][:, :], ones_bf[:, :], sq[:, :], start=(k == 0), stop=(k == 7))], [sq, "ones_bf"], [BK[7]])
            free(sq)
        rs = falloc()
        ACT(rs[:, :], banks[7][:, :], AF.Sqrt, [BK[7]], [rs], scale=1.0 / DM, bias=EPS)
        RECIP(rs[:, :], rs[:, :], [rs], [rs])
        for k in range(8):
            STT(hbuf[:, k, :], xs[:, k, :], sp[:, gcol + k:gcol + k + 1], rs[:, :], ALU.mult, ALU.mult, [("xs", k), ("sp", l), rs], [("h", k)])
        free(rs)

    HK = [("h", k) for k in range(8)]

    def proj_fm(slot, m, bank, split=False):
        if split:
            for k in range(8):
                MM([mm(banks[bank][:, :], ring[slot][:, k, m * 128:(m + 1) * 128], hbuf[:, k, :], start=(k == 0), stop=(k == 7))],
                   [("ring", slot), HK[k]], [BK[bank]])
            return
        MM([mm(banks[bank][:, :], ring[slot][:, k, m * 128:(m + 1) * 128], hbuf[:, k, :], start=(k == 0), stop=(k == 7)) for k in range(8)],
           [("ring", slot)] + HK, [BK[bank]])

    def layer_tile(s, ti, l):
        d = L[l]
        sp = d["sp"]
        SPK = ("sp", l)
        first = (ti == 0)
        t0 = ti * NT
        if l == layers[0]:
            for k in range(8):
                P.dma("act", xs[:, k, :], xT[s, k * 128:(k + 1) * 128, t0:t0 + NT], writes=[("xs", k)], chan=("xs", k))
        if first:
            P.op("pool", lambda e: e.memset(d["zbuf"][:, :, 0:2], 0.0), writes=[("zbuf", l)])
            P.op("pool", lambda e: e.memset(d["Er"][:, :, 0:1], 0.0), writes=[("Er", l)])
            P.op("pool", lambda e: e.memset(d["Ei"][:, :, 0:1], 0.0), writes=[("Ei", l)])
            for j in range(2):
                P.op("pool", lambda e, j=j: e.memset(d["S"][j][:, :], 0.0), writes=[("S", l, j)])
        P.dma("act", s5m[:, :], s5m_d[l], reads=[("s5m_d", l)], writes=["s5m"], chan="s5m_ld")
        rmsnorm_to_h(l, SPC["gmix"])
        CHK("norm1")
        Y = [falloc() for _ in range(8)]
        YN = [balloc() for _ in range(8)]

        def group_norm(gI, ssb):
            for i in range(2):
                sq = balloc()
                ACT(sq[:, :], Y[2 * gI + i][:, :], AF.Square, [Y[2 * gI + i]], [sq])
                MM([mm(banks[ssb][:, :], ones_bf[:, :], sq[:, :], start=(i == 0), stop=(i == 1))], [sq, "ones_bf"], [BK[ssb]])
                free(sq)
            rs = falloc()
            ACT(rs[:, :], banks[ssb][:, :], AF.Sqrt, [BK[ssb]], [rs], scale=1.0 / 256, bias=EPS)
            RECIP(rs[:, :], rs[:, :], [rs], [rs])
            for i in range(2):
                k = 2 * gI + i
                STT(YN[k][:, :], Y[k][:, :], sp[:, SPC["ggn"] + k:SPC["ggn"] + k + 1], rs[:, :], ALU.mult, ALU.mult, [Y[k], SPK, rs], [YN[k]])
            free(rs)
            free(Y[2 * gI]); free(Y[2 * gI + 1])

        def chainX():
            slot = get_chunk(l, 0)
            hsb = [falloc() for _ in range(2)]
            bsb = [falloc() for _ in range(2)]
            for j in range(2):
                proj_fm(slot, j, j, split=(j == 0))
                CP("act", hsb[j][:, :], banks[j][:, :], [BK[j]], [hsb[j]])
            for j in range(2):
                proj_fm(slot, 2 + j, j)
                CP("act", bsb[j][:, :], banks[j][:, :], [BK[j]], [bsb[j]])
            release_chunk()
            slot = get_chunk(l, 1)
            zb = d["zbuf"]
            for j in range(2):
                proj_fm(slot, j, j)
                TT("dve", zb[:, j, 2:NT + 2], banks[j][:, :], hsb[j][:, :], ALU.mult, [BK[j], hsb[j]], [("zbuf", l)])
            ubf = balloc(2)
            for j in range(2):
                proj_fm(slot, 2 + j, j)
                CP("act", ubf[:, j * NT:(j + 1) * NT].rearrange("p (t c) -> p t c", c=NC5), banks[j][:, :].rearrange("p (c t) -> p t c", t=TS5),
                   [BK[j]], [ubf])
            release_chunk()
            yield
            for j in range(2):
                acc = falloc()
                cw = lambda i: sp[:, SPC["convw"] + j * 3 + i:SPC["convw"] + j * 3 + i + 1]
                TS("pool", acc[:, :], zb[:, j, 2:NT + 2], cw(2), ALU.mult, [("zbuf", l), SPK], [acc])
                STT(acc[:, :], zb[:, j, 1:NT + 1], cw(1), acc[:, :], ALU.mult, ALU.add, [("zbuf", l), SPK, acc], [acc])
                STT(acc[:, :], zb[:, j, 0:NT], cw(0), acc[:, :], ALU.mult, ALU.add, [("zbuf", l), SPK, acc], [acc])
                TT("pool", Y[j][:, :], acc[:, :], bsb[j][:, :], ALU.mult, [acc, bsb[j]], [Y[j]])
                free(acc)
            P.op("pool", lambda e: e.tensor_copy(out=d["zbuf"][:, :, 0:2], in_=d["zbuf"][:, :, NT:NT + 2]), [("zbuf", l)], [("zbuf", l)])
            for b_ in hsb + bsb:
                free(b_)
            yield
            group_norm(0, 1)
            yield
            ut = [ubf[:, j * NT:(j + 1) * NT] for j in range(2)]
            for ri in range(2):
                for qq in range(4):
                    mms = []
                    for j in range(2):
                        q = j * 4 + qq
                        for s_ in range(TS5):
                            mms.append(mm(banks[ri][:, q * NC5:(q + 1) * NC5], d["L1"][qq * 32:(qq + 1) * 32, j, ri, TS5 - 1 - s_, :],
                                          ut[j][qq * 32:(qq + 1) * 32, s_ * NC5:(s_ + 1) * NC5], start=(s_ == 0), stop=(s_ == TS5 - 1), tp=(qq * 32, 0)))
                    MM(mms, [ubf, "s5m"], [BK[ri]], force=True)
            yield
            Wr = falloc(); Wi = falloc(); t1 = falloc(); t2 = falloc()
            cosR, sinR, rtab = d["cosR"], d["sinR"], d["rtab"]
            CK, SK, RK = ("cosR", l), ("sinR", l), ("rtab", l)
            TT("dve", t1[:, :], banks[0][:, :], cosR[:, :], ALU.mult, [BK[0], CK], [t1])
            TT("dve", t2[:, :], banks[1][:, :], sinR[:, :], ALU.mult, [BK[1], SK], [t2])
            TT("pool", Wr[:, :], t1[:, :], t2[:, :], ALU.add, [t1, t2], [Wr])
            TT("dve", t1[:, :], banks[1][:, :], cosR[:, :], ALU.mult, [BK[1], CK], [t1])
            TT("dve", t2[:, :], banks[0][:, :], sinR[:, :], ALU.mult, [BK[0], SK], [t2])
            TT("pool", Wi[:, :], t1[:, :], t2[:, :], ALU.subtract, [t1, t2], [Wi])
            yield
            for (Wx, Ex, ek) in ((Wr, d["Er"], ("Er", l)), (Wi, d["Ei"], ("Ei", l))):
                TT("dve", t1[:, 0:8], d["r8"][:, :], Ex[:, :, 0], ALU.mult, [("r8", l), ek], [t1])
                TT("dve", Wx.v3(NC5)[:, :, 0], Wx.v3(NC5)[:, :, 0], t1[:, 0:8], ALU.add, [Wx, t1], [Wx])
            Fr = falloc(); Fi = falloc()
            for (Wx, Fx) in ((Wr, Fr), (Wi, Fi)):
                P.op("dve", lambda e, Wx=Wx, Fx=Fx: e.tensor_tensor_scan(out=Fx[:, :], data0=rtab[:, :], data1=Wx[:, :], initial=0.0,
                                                                        op0=ALU.mult, op1=ALU.add), [RK, Wx], [Fx])
            yield
            TT("dve", t1[:, :], Fr[:, :], cosR[:, :], ALU.mult, [Fr, CK], [t1])
            TT("dve", t2[:, :], Fi[:, :], sinR[:, :], ALU.mult, [Fi, SK], [t2])
            TT("pool", d["Er"][:, :, 1:NC5 + 1], t1.v3(NC5), t2.v3(NC5), ALU.subtract, [t1, t2], [("Er", l)])
            TT("dve", t1[:, :], Fi[:, :], cosR[:, :], ALU.mult, [Fi, CK], [t1])
            TT("dve", t2[:, :], Fr[:, :], sinR[:, :], ALU.mult, [Fr, SK], [t2])
            TT("pool", d["Ei"][:, :, 1:NC5 + 1], t1.v3(NC5), t2.v3(NC5), ALU.add, [t1, t2], [("Ei", l)])
            for b_ in (Wr, Wi, t1, t2, Fr, Fi):
                free(b_)
            Ebf = balloc(2)
            CP("act", Ebf[:, 0:NT].rearrange("p (q c) -> p q c", c=NC5), d["Er"][:, :, 0:NC5], [("Er", l)], [Ebf])
            CP("act", Ebf[:, NT:2 * NT].rearrange("p (q c) -> p q c", c=NC5), d["Ei"][:, :, 0:NC5], [("Ei", l)], [Ebf])
            P.op("pool", lambda e: e.tensor_copy(out=d["Er"][:, :, 0:1], in_=d["Er"][:, :, NC5:NC5 + 1]), [("Er", l)], [("Er", l)])
            P.op("pool", lambda e: e.tensor_copy(out=d["Ei"][:, :, 0:1], in_=d["Ei"][:, :, NC5:NC5 + 1]), [("Ei", l)], [("Ei", l)])
            yield
            E4 = [Ebf[:, ri * NT:(ri + 1) * NT].rearrange("p (q c) -> p q c", c=NC5) for ri in range(2)]
            for j in range(2):
                yb = banks[j]
                mms = []
                for tau in range(TS5):
                    mms.append(mm(yb[:, tau * NC5:NT], d["KL"][:, j, tau, :], ut[j][:, 0:(TS5 - tau) * NC5], start=(tau == 0), stop=False))
                for qq in range(4):
                    q = j * 4 + qq
                    for t_ in range(TS5):
                        for ri in range(2):
                            mms.append(mm(yb[qq * 32:(qq + 1) * 32, t_ * NC5:(t_ + 1) * NC5], d["L3"][:, ri, q, t_, :], E4[ri][:, q, :], start=False,
                                          stop=(qq == 3 and t_ == TS5 - 1 and ri == 1), tp=(0, qq * 32)))
                MM(mms, [ubf, Ebf, "s5m"], [BK[j]])
            yield
            gl = balloc(2)
            for j in range(2):
                ysb = falloc(); sq = falloc()
                ynat = banks[j][:, :].rearrange("p (t c) -> p c t", c=NC5)
                CP("act", ysb.v3(TS5), ynat, [BK[j]], [ysb])
                ACT(sq.v3(TS5), ynat, AF.Square, [BK[j]], [sq])
                TS("dve", sq[:, :], sq[:, :], 0.044715, ALU.mult, [sq], [sq], s2=1.0, op1=ALU.add)
                TT("dve", sq[:, :], sq[:, :], ysb[:, :], ALU.mult, [sq, ysb], [sq])
                ACT(sq[:, :], sq[:, :], AF.Sigmoid, [sq], [sq], scale=1.5957691216057308)
                TT("pool", gl[:, j * NT:(j + 1) * NT], ysb[:, :], sq[:, :], ALU.mult, [ysb, sq], [gl])
                free(ysb); free(sq)
                yield
            free(ubf); free(Ebf)
            for j in range(2):
                for (n, bk) in ((j, 0), (2 + j, 1)):
                    MM([mm(banks[bk][:, :], d["glu"][:, k, n * 128:(n + 1) * 128], gl[:, k * NT:(k + 1) * NT], start=(k == 0), stop=(k == 1)) for k in range(2)],
                       [gl, ("glu", l)], [BK[bk]])
                sg = falloc()
                ACT(sg[:, :], banks[1][:, :], AF.Sigmoid, [BK[1]], [sg])
                TT("dve", Y[2 + j][:, :], banks[0][:, :], sg[:, :], ALU.mult, [BK[0], sg], [Y[2 + j]])
                free(sg)
                yield
            free(gl)
            group_norm(1, 1)

        def chainY():
            slot = get_chunk(l, 2)
            for m in range(3):
                proj_fm(slot, m, 2 + m)
            MM([mm(banks[5][:, blk * 128:(blk + 1) * 128], hbuf[:, k, blk * 128:(blk + 1) * 128], ring[slot][:, k, 384:512], start=(k == 0), stop=(k == 7))
                for blk in range(4) for k in range(8)], [("ring", slot)] + HK, [BK[5]])
            release_chunk()
            kbuf, vbuf = d["kbuf"], d["vbuf"]
            KB, VB = ("kbuf", l), ("vbuf", l)
            qn = balloc(2)
            CP("act", vbuf[:, 1:5, :], banks[5][:, :].rearrange("p (b f) -> p b f", f=128), [BK[5]], [VB])
            yield
            for (bk, gcol, outv, okey, ssb) in ((2, SPC["qgs"], qn[:, 0:NT], qn, 6), (3, SPC["qgs"], qn[:, NT:2 * NT], qn, 7),
                                                (4, SPC["kg"], kbuf[:, 128:128 + NT], KB, 5)):
                sq = balloc(); rs = falloc()
                ACT(sq[:, :], banks[bk][:, :], AF.Square, [BK[bk]], [sq])
                MM([mm(banks[ssb][:, :], bones_bf[:, :], sq[:, :])], [sq, "bones_bf"], [BK[ssb]])
                ACT(rs[:, :], banks[ssb][:, :], AF.Sqrt, [BK[ssb]], [rs], scale=1.0 / 64, bias=EPS)
                RECIP(rs[:, :], rs[:, :], [rs], [rs])
                STT(outv, banks[bk][:, :], sp[:, gcol:gcol + 1], rs[:, :], ALU.mult, ALU.mult, [BK[bk], SPK, rs], [okey])
                free(sq); free(rs)
                yield
            jb_list = list(range(1 if first else 0, 5))
            for jbi, jb in enumerate(jb_list):
                qlo, qhi = max(0, 2 * jb - 2), min(8, 2 * jb + 2)
                off = (qlo - (2 * jb - 2)) * 64
                ncol = (qhi - qlo) * 64
                for kh in range(2):
                    MM([mm(banks[2 + kh][:, r * 256 + off:r * 256 + off + ncol], kbuf[kh * 64:(kh + 1) * 64, jb * 128:(jb + 1) * 128],
                           qn[kh * 64:(kh + 1) * 64, r * NT + qlo * 64:r * NT + qhi * 64]) for r in range(2)], [KB, qn], [BK[2 + kh]])
                pT = balloc(2)
                P.op("pool", lambda e, pT=pT: e.memset(pT[:, :], 0.0), writes=[pT])
                for kh in range(2):
                    for half, (c0, c1) in enumerate(((0, 192), (64, 256))):
                        a, b2 = max(c0, off), min(c1, off + ncol)
                        if b2 <= a:
                            continue
                        src = banks[2 + kh][half * 64:(half + 1) * 64, :].rearrange("p (r c) -> p r c", c=256)[:, :, a:b2]
                        dst = pT[half * 64:(half + 1) * 64, kh * 512:(kh + 1) * 512].rearrange("p (r c) -> p r c", c=256)[:, :, a:b2]
                        ACT(dst, src, AF.Exp, [BK[2 + kh]], [pT])
                for r in range(2):
                    mms_n, mms_d = [], []
                    for kh in range(2):
                        pv = pT[:, kh * 512 + r * 256:kh * 512 + (r + 1) * 256]
                        for part in range(2):
                            pair = jb - 1 + part
                            if pair < 0 or pair > 3:
                                continue
                            first_contrib = (part == 1) or (first and jb == 1)
                            last_contrib = (part == 0) or (jb == 4)
                            if part == 1 and jb == 4:
                                continue
                            cols = slice(pair * 128, (pair + 1) * 128)
                            mms_n.append(mm(banks[4 + r][kh * 64:(kh + 1) * 64, cols], vbuf[:, jb, kh * 64:(kh + 1) * 64], pv[:, part * 128:(part + 1) * 128],
                                            start=first_contrib, stop=last_contrib))
                            mms_d.append(mm(banks[6 + r][kh * 64:(kh + 1) * 64, cols], ones_bf[:, 0:64], pv[:, part * 128:(part + 1) * 128],
                                            start=first_contrib, stop=last_contrib))
                    MM(mms_n, [pT, VB], [BK[4 + r]])
                    MM(mms_d, [pT, "ones_bf"], [BK[6 + r]])
                free(pT)
                yield
            for r in range(2):
                rec = falloc()
                TS("dve", rec[:, :], banks[6 + r][:, :], sp[:, SPC["esink"] + r:SPC["esink"] + r + 1], ALU.add, [BK[6 + r], SPK], [rec])
                RECIP(rec[:, :], rec[:, :], [rec], [rec])
                TT("dve", Y[4 + r][:, :], banks[4 + r][:, :], rec[:, :], ALU.mult, [BK[4 + r], rec], [Y[4 + r]])
                free(rec)
            free(qn)
            P.op("pool", lambda e: e.tensor_copy(out=kbuf[:, 0:128], in_=kbuf[:, NT:NT + 128]), [KB], [KB])
            P.op("pool", lambda e: e.tensor_copy(out=vbuf[:, 0, :], in_=vbuf[:, 4, :]), [VB], [VB])
            yield
            group_norm(2, 2)
            yield
            slot = get_chunk(l, 3)
            for m in range(4):
                proj_fm(slot, m, 2 + m)
            release_chunk()
            qt = balloc(2); qh = balloc(4); kt = balloc(2); kend = balloc(2)
            e3s = []
            for j in range(2):
                sig = falloc(); lg = falloc(); kk = falloc(); B = falloc(); e1 = falloc(); e2 = falloc(); e3 = falloc()
                ACT(sig[:, :], banks[4 + j][:, :], AF.Sigmoid, [BK[4 + j]], [sig])
                TS("dve", sig[:, :], sig[:, :], sp[:, SPC["oml"] + j:SPC["oml"] + j + 1], ALU.mult, [sig, SPK], [sig],
                   s2=sp[:, SPC["lb"] + j:SPC["lb"] + j + 1], op1=ALU.add)
                ACT(lg[:, :], sig[:, :], AF.Ln, [sig], [lg])
                TS("pool", kk[:, :], sig[:, :], -1.0, ALU.mult, [sig], [kk], s2=1.0, op1=ALU.add)
                P.op("dve", lambda e, B=B, lg=lg: e.tensor_tensor_scan(out=B[:, :], data0=cs("segm"), data1=lg[:, :], initial=0.0,
                                                                      op0=ALU.mult, op1=ALU.add), ["cst", lg], [B])
                yield
                ACT(e3[:, :], B[:, :], AF.Exp, [B], [e3])
                TT("dve", lg.v3(64), B.v3(64), bc(B.v3(64)[:, :, 31:32], [128, 8, 64]), ALU.subtract, [B], [lg])
                ACT(e1[:, :], lg[:, :], AF.Exp, [lg], [e1])
                ACT(e2[:, :], lg[:, :], AF.Exp, [lg], [e2], scale=-1.0)
                TT("dve", qt[:, j * NT:(j + 1) * NT], banks[2 + j][:, :], e1[:, :], ALU.mult, [BK[2 + j], e1], [qt])
                for hh in range(2):
                    STT(qh[:, (j * 2 + hh) * NT:(j * 2 + hh + 1) * NT], banks[2 + j][:, :], cs("mB%d" % hh), e3[:, :], ALU.mult, ALU.mult,
                        [BK[2 + j], e3, "cst"], [qh])
                TT("pool", kt[:, j * NT:(j + 1) * NT], kk[:, :], e2[:, :], ALU.mult, [kk, e2], [kt])
                TT("pool", kend[:, j * NT:(j + 1) * NT].rearrange("p (b t) -> p b t", t=64), kt[:, j * NT:(j + 1) * NT].rearrange("p (b t) -> p b t", t=64),
                   bc(e1.v3(64)[:, :, 63:64], [128, 8, 64]), ALU.mult, [kt, e1], [kend])
                e3s.append(e3)
                for b_ in (sig, lg, kk, B, e1, e2):
                    free(b_)
                yield
            b6bf = banks[6][:, :].bitcast(BF16)
            def tr_fn(e):
                ins = None
                for bp in range(4):
                    for j in range(2):
                        ins = e.transpose(out=b6bf[:, (bp * 2 + j) * 128:(bp * 2 + j + 1) * 128], in_=kend[:, j * NT + bp * 128:j * NT + (bp + 1) * 128],
                                          identity=ident_bf[:, :])
                return ins
            P.op("pe", tr_fn, [kend, "ident_bf"], [BK[6]])
            kendT = balloc(2)
            CP("act", kendT[:, :], b6bf, [BK[6]], [kendT])
            free(kend)
            yield
            slot = get_chunk(l, 4)
            hib = (7, 2)
            for half in range(2):
                MM([mm(banks[hib[half]][:, bq * 256:(bq + 1) * 256], hbuf[:, k, (half * 2 + bq) * 128:(half * 2 + bq + 1) * 128], ring[slot][:, k, 0:256],
                       start=(k == 0), stop=(k == 7)) for bq in range(2) for k in range(8)], [("ring", slot)] + HK, [BK[hib[half]]])
            for j in range(2):
                proj_fm(slot, 2 + j, 3 + j)
            release_chunk()
            vT = balloc(2)
            for half in range(2):
                CP("act", vT[:, half * NT:(half + 1) * NT], banks[hib[half]][:, :], [BK[hib[half]]], [vT])
            sgs = []
            for j in range(2):
                sg = falloc()
                ACT(sg[:, :], banks[3 + j][:, :], AF.Silu, [BK[3 + j]], [sg])
                sgs.append(sg)
            yield
            for hp in range(2):
                mms = []
                for j in range(2):
                    for b in range(8):
                        bp, half = b // 2, b % 2
                        mms.append(mm(banks[5 + hp][half * 64:(half + 1) * 64, j * 256 + bp * 64:j * 256 + (bp + 1) * 64],
                                      kt[hp * 64:(hp + 1) * 64, j * NT + b * 64:j * NT + (b + 1) * 64],
                                      qt[hp * 64:(hp + 1) * 64, j * NT + b * 64:j * NT + (b + 1) * 64]))
                MM(mms, [kt, qt], [BK[5 + hp]])
            for hp in range(2):
                for half in range(2):
                    rows = slice(half * 64, (half + 1) * 64)
                    dst = Amz[rows, :].rearrange("p (j hp bp hf t) -> p j hp bp hf t", j=2, hp=2, bp=4, hf=2, t=64)[:, :, hp, :, half, :]
                    src = banks[5 + hp][rows, :].rearrange("p (j bp t) -> p j bp t", j=2, bp=4)
                    o_c, _w = _CST_OFF["cmask"]
                    msk = bc(cst[rows, o_c:o_c + 64].unsqueeze(1).unsqueeze(1), [64, 2, 4, 64])
                    TT("dve", dst, src, msk, ALU.mult, [BK[5 + hp], "cst"], ["Amz"])
            free(kt); free(qt)
            yield
            ub = (7, 2)
            for half in range(2):
                mms = []
                for j in range(2):
                    for hh in range(2):
                        h = 2 * j + hh
                        for bp in range(4):
                            mms.append(mm(banks[ub[half]][hh * 64:(hh + 1) * 64, j * 256 + bp * 64:j * 256 + (bp + 1) * 64],
                                          kendT[half * 64:(half + 1) * 64, bp * 256 + h * 64:bp * 256 + (h + 1) * 64],
                                          vT[half * 64:(half + 1) * 64, bp * 256 + h * 64:bp * 256 + (h + 1) * 64]))
                MM(mms, [kendT, vT], [BK[ub[half]]])
            free(kendT)
            yield
            Sall = [balloc() for _ in range(2)]
            for j in range(2):
                Sa, Sb = d["S"][j], d["S2"][j]
                KA, KB2 = ("S", l, j), ("S2", l, j)
                CP("act", Sall[j][:, 0:64], Sa[:, :], [KA], [Sall[j]])
                for b in range(8):
                    bp, half = b // 2, b % 2
                    src, dst, ks, kd = (Sa, Sb, KA, KB2) if b % 2 == 0 else (Sb, Sa, KB2, KA)
                    STT(dst[:, :], src[:, :], e3s[j][:, b * 64 + 63:b * 64 + 64], banks[ub[half]][:, j * 256 + bp * 64:j * 256 + (bp + 1) * 64], ALU.mult, ALU.add,
                        [ks, e3s[j], BK[ub[half]]], [kd])
                    if b < 7:
                        CP("act", Sall[j][:, (b + 1) * 64:(b + 2) * 64], dst[:, :], [kd], [Sall[j]])
                yield
            for e3 in e3s:
                free(e3)
            for j in range(2):
                mms = []
                for hh in range(2):
                    h = 2 * j + hh
                    for b in range(8):
                        bp = b // 2
                        o_ = banks[5 + j][hh * 64:(hh + 1) * 64, b * 64:(b + 1) * 64]
                        mms.append(mm(o_, Sall[j][:, b * 64:(b + 1) * 64],
                                      qh[:, (j * 2 + hh) * NT + b * 64:(j * 2 + hh) * NT + (b + 1) * 64], start=True, stop=False))
                        mms.append(mm(o_, vT[:, bp * 256 + h * 64:bp * 256 + (h + 1) * 64],
                                      Amz[:, (h * 8 + b) * 64:(h * 8 + b + 1) * 64], start=False, stop=True))
                MM(mms, [Sall[j], qh, vT, "Amz"], [BK[5 + j]])
            free(Sall[0]); free(Sall[1]); free(qh); free(vT)
            yield
            for j in range(2):
                sq = balloc(); rs = falloc()
                ACT(sq[:, :], banks[5 + j][:, :], AF.Square, [BK[5 + j]], [sq])
                MM([mm(banks[3][:, :], bones_bf[:, :], sq[:, :])], [sq, "bones_bf"], [BK[3]])
                ACT(rs[:, :], banks[3][:, :], AF.Sqrt, [BK[3]], [rs], scale=1.0 / 64, bias=EPS)
                RECIP(rs[:, :], rs[:, :], [rs], [rs])
                STT(rs[:, :], banks[5 + j][:, :], sp[:, SPC["og"]:SPC["og"] + 1], rs[:, :], ALU.mult, ALU.mult, [BK[5 + j], SPK, rs], [rs])
                TT("pool", Y[6 + j][:, :], rs[:, :], sgs[j][:, :], ALU.mult, [rs, sgs[j]], [Y[6 + j]])
                free(sq); free(rs); free(sgs[j])
                yield
            group_norm(3, 3)

        gens = [chainX(), chainY()]
        while gens:
            for g in list(gens):
                try:
                    next(g)
                except StopIteration:
                    gens.remove(g)
        CHK("hgrn")
        for c in range(2):
            slot = get_chunk(l, 5 + c)
            for m in range(4):
                MM([mm(banks[m][:, :], ring[slot][:, k, m * 128:(m + 1) * 128], YN[k][:, :], start=(k == 0), stop=(k == 7)) for k in range(8)],
                   [("ring", slot)] + YN, [BK[m]])
            release_chunk()
            for m in range(4):
                k = c * 4 + m
                TT("dve", xs[:, k, :], xs[:, k, :], banks[m][:, :], ALU.add, [("xs", k), BK[m]], [("xs", k)])
        for y_ in YN:
            free(y_)
        if "xmid" in tap_out and (s, ti) == taps.get("_ysel_tile", (0, 0)):
            for k in range(8):
                TAP("xmid", (l, k), xs[:, k, :], [("xs", k)])

        CHK("gn")
        rmsnorm_to_h(l, SPC["gffn"])
        hid = balloc(32)
        HIDK = hid.keys_
        for c in range(8):
            slot = get_chunk(l, 7 + c)
            if c == 0:
                for k in range(8):
                    MM([mm(banks[m][:, :], ring[slot][:, k, m * 128:(m + 1) * 128], hbuf[:, k, :], start=(k == 0), stop=(k == 7)) for m in range(4)],
                       [("ring", slot), HK[k]], [BK[m] for m in range(4)])
            for m in range(4):
                bk = (c * 4 + m) % 4
                if c > 0:
                    proj_fm(slot, m, bk)
                r_ = falloc()
                ACT(r_[:, :], banks[bk][:, :], AF.Relu, [BK[bk]], [r_])
                idx = c * 4 + m
                TT("pool", hid[:, idx * NT:(idx + 1) * NT], r_[:, :], r_[:, :], ALU.mult, [r_], [HIDK[idx]])
                free(r_)
            release_chunk()
        for cg in range(2):
            for kg in range(4):
                slot = get_chunk(l, 15 + cg * 4 + kg)
                for m in range(4):
                    MM([mm(banks[4 + m][:, :], ring[slot][:, k, m * 128:(m + 1) * 128], hid[:, (kg * 8 + k) * NT:(kg * 8 + k + 1) * NT],
                           start=(kg == 0 and k == 0), stop=(kg == 3 and k == 7)) for k in range(8)],
                       [("ring", slot)] + HIDK[kg * 8:(kg + 1) * 8], [BK[4 + m]])
                release_chunk()
            for m in range(4):
                k = cg * 4 + m
                TT("dve", xs[:, k, :], xs[:, k, :], banks[4 + m][:, :], ALU.add, [("xs", k), BK[4 + m]], [("xs", k)])
        free(hid)
        if "xout" in tap_out and (s, ti) == taps.get("_ysel_tile", (0, 0)):
            for k in range(8):
                TAP("xout", (l, k), xs[:, k, :], [("xs", k)])
        if l == layers[-1]:
            outs = []
            for k in range(8):
                outs.append(P.dma("sp", oT[s, k * 128:(k + 1) * 128, t0:t0 + NT], xs[:, k, :], reads=[("xs", k)], writes=[("oT", s, ti, k)], chan=("out", k)))
            return outs
        return []

    all_out = []
    try:
        CHK("prologue")
        for (s, ti, l) in tile_list:
            all_out += layer_tile(s, ti, l)
    except StopBuild:
        pass
    if list_schedule:
        P.schedule()
    P.emit(final_deps=all_out + tap_dmas)
    return nc, P


_CACHE = {}


def prepare_inputs(inputs):
    wch, glu = _build_weights(inputs)
    prm = np.stack([_build_params(inputs, l) for l in range(DEPTH)], axis=0)
    cst = _build_consts()
    x = np.asarray(inputs["x"])
    in_maps = []
    for c in range(NCORES):
        xc = x[c * SEQ_PER_CORE:(c + 1) * SEQ_PER_CORE]
        xT = np.ascontiguousarray(xc.transpose(0, 2, 1))
        in_maps.append({"xT": xT, "wch": wch, "glu": glu, "prm": prm, "cst": cst})
    return in_maps


def kernel(**inputs):
    in_maps = prepare_inputs(inputs)
    if "nc" not in _CACHE:
        _CACHE["nc"] = build_program()[0]
    res = run_bass_kernel_spmd(_CACHE["nc"], in_maps, core_ids=list(range(NCORES)))
    outs = []
    for c in range(NCORES):
        oT = res.results[c]["oT"]
        outs.append(np.ascontiguousarray(oT.transpose(0, 2, 1)))
    return np.concatenate(outs, axis=0).astype(np.float32)
```
